# Optimizing a Trainium2 kernel written in Bass

```python
import jax
import jax.numpy as jnp
from jax import lax
import numpy as np

D_MODEL = 1024
BATCH = 8
SEQ = 2048
DEPTH = 2

CHUNK = 64
MIX_WIDTH = D_MODEL
M_WIDTH = MIX_WIDTH // 2
M_HEADS = 4
M_HD = M_WIDTH // M_HEADS
M_CONV = 4
R_WIDTH = MIX_WIDTH - M_WIDTH
R_HD = 64
R_HEADS = R_WIDTH // R_HD
R_DECAY_LR = 64
R_A_LR = 64
R_G_LR = 128
M_SIZES = (M_WIDTH, M_WIDTH, M_WIDTH, M_WIDTH, M_HEADS, M_HEADS)
R_SIZES = (R_WIDTH, R_WIDTH, R_WIDTH, R_DECAY_LR, R_A_LR, R_G_LR)
M_COLS = sum(M_SIZES)
R_COLS = sum(R_SIZES)
IN_COLS = M_COLS + R_COLS
X_HEADS = 4
X_HD = D_MODEL // X_HEADS
N_MEM = 256
N_EXPERTS = 32
TOP_K = 4
D_FF = D_MODEL
SWIGLU_LIMIT = 7.0
SWIGLU_ALPHA = 1.702
MOE_BLOCK = 128
LN_EPS = 1e-5
HEAD_NORM_EPS = 1e-5
RWKV_GN_EPS = 64e-5
DN_ALPHA = (2 * DEPTH) ** 0.25
DN_BETA = (8 * DEPTH) ** -0.25

kernel_name = 'hybrid_mlstm_rwkv7_moe_encoder'


def split_cols(p, sizes):
    cuts = [int(c) for c in np.cumsum(sizes)[:-1]]
    return jnp.split(p, cuts, axis=-1)


def layer_norm(x, g, b):
    xf = x.astype(jnp.float32)
    mu = jnp.mean(xf, axis=-1, keepdims=True)
    var = jnp.mean(jnp.square(xf - mu), axis=-1, keepdims=True)
    y = (xf - mu) * lax.rsqrt(var + LN_EPS) * g.astype(jnp.float32) + b.astype(jnp.float32)
    return y.astype(x.dtype)


def head_standardize(x, eps):
    mu = jnp.mean(x, axis=-1, keepdims=True)
    var = jnp.mean(jnp.square(x - mu), axis=-1, keepdims=True)
    return (x - mu) * lax.rsqrt(var + eps)


def causal_dwconv(x, w, b):
    k_w, c = w.shape
    y = lax.conv_general_dilated(x, w[:, None, :].astype(x.dtype), window_strides=(1,),
                                 padding=[(k_w - 1, 0)],
                                 dimension_numbers=('NWC', 'WIO', 'NWC'),
                                 feature_group_count=c)
    return y + b


def token_shift(p, mu):
    prev = jnp.pad(p, ((0, 0), (1, 0), (0, 0)))[:, :-1]
    return p + (prev - p) * mu


def mlstm_chunkwise(q, k, v, i_pre, f_pre):
    f32 = jnp.float32
    B, S, H, D = q.shape
    NC = S // CHUNK
    blk = lambda t: t.astype(f32).reshape(B, NC, CHUNK, H, D).transpose(0, 3, 1, 2, 4)
    gblk = lambda t: t.astype(f32).reshape(B, NC, CHUNK, H).transpose(0, 3, 1, 2)
    q, k, v = blk(q), blk(k) * (D ** -0.5), blk(v)
    ig = gblk(i_pre)
    b = jnp.cumsum(jax.nn.log_sigmoid(gblk(f_pre)), axis=-1)
    b_last = b[..., -1]
    src = b_last[..., None] - b + ig
    m_loc = jnp.max(src, axis=-1)
    w_src = jnp.exp(src - m_loc[..., None])
    C_loc = jnp.einsum('bhcsv,bhcsk->bhcvk', v * w_src[..., None], k)
    n_loc = jnp.einsum('bhcs,bhcsk->bhck', w_src, k)

    def step(carry, inp):
        C, n, m = carry
        C_l, n_l, m_l, bl = inp
        m_new = jnp.maximum(bl + m, m_l)
        a = jnp.exp(bl + m - m_new)
        c = jnp.exp(m_l - m_new)
        C_new = a[..., None, None] * C + c[..., None, None] * C_l
        n_new = a[..., None] * n + c[..., None] * n_l
        return (C_new, n_new, m_new), (C, n, m)

    init = (jnp.zeros((B, H, D, D), f32), jnp.zeros((B, H, D), f32), jnp.zeros((B, H), f32))
    xs = (jnp.moveaxis(C_loc, 2, 0), jnp.moveaxis(n_loc, 2, 0),
          jnp.moveaxis(m_loc, 2, 0), jnp.moveaxis(b_last, 2, 0))
    _, (C_prev, n_prev, m_prev) = lax.scan(step, init, xs)
    C_prev = jnp.moveaxis(C_prev, 0, 2)
    n_prev = jnp.moveaxis(n_prev, 0, 2)
    m_prev = jnp.moveaxis(m_prev, 0, 2)

    g_inter = b + m_prev[..., None]
    causal = jnp.tril(jnp.ones((CHUNK, CHUNK), dtype=bool))
    d_mat = jnp.where(causal, b[..., :, None] - b[..., None, :] + ig[..., None, :], -jnp.inf)
    m_t = jnp.maximum(g_inter, jnp.max(d_mat, axis=-1))
    s = jnp.einsum('bhctd,bhcsd->bhcts', q, k) * jnp.exp(d_mat - m_t[..., None])
    w_inter = jnp.exp(g_inter - m_t)
    num = jnp.einsum('bhcts,bhcsv->bhctv', s, v) + w_inter[..., None] * jnp.einsum('bhcvk,bhctk->bhctv', C_prev, q)
    den = jnp.sum(s, axis=-1) + w_inter * jnp.einsum('bhck,bhctk->bhct', n_prev, q)
    h = num / jnp.maximum(jnp.abs(den), jnp.exp(-m_t))[..., None]
    return h.transpose(0, 2, 3, 1, 4).reshape(B, S, H, D)


def rwkv7_scan(r, w_log, k, v, kk, a):
    f32 = jnp.float32
    B, S, H, D = r.shape
    decay = jnp.exp(-jnp.exp(w_log.astype(f32)))

    def step(state, inp):
        r_t, d_t, k_t, v_t, kk_t, a_t = inp
        sa = jnp.einsum('bhvk,bhk->bhv', state, -kk_t)
        state = (state * d_t[:, :, None, :] + sa[..., None] * (kk_t * a_t)[:, :, None, :]
                 + v_t[..., None] * k_t[:, :, None, :])
        y = jnp.einsum('bhvk,bhk->bhv', state, r_t)
        return state, y

    xs = tuple(t.astype(f32).transpose(1, 0, 2, 3) for t in (r, decay, k, v, kk, a))
    _, y = lax.scan(step, jnp.zeros((B, H, D, D), f32), xs)
    return y.transpose(1, 0, 2, 3)


def hybrid_mixer(h, w_in, m_conv_w, m_conv_b, m_ig_b, m_fg_b, m_norm_g, r_mu, r_w0, r_w2,
                 r_a0, r_a2, r_g2, r_kk, r_ka, r_rk, r_gn_g, r_gn_b, w_out):
    f32 = jnp.float32
    B, S, _ = h.shape
    proj = h @ w_in
    m_cols, r_cols = proj[..., :M_COLS], proj[..., M_COLS:]

    mq, mk, mv, mo, mi, mf = split_cols(m_cols, M_SIZES)
    qk = jax.nn.silu(causal_dwconv(jnp.concatenate([mq, mk], axis=-1), m_conv_w, m_conv_b))
    mq, mk = qk[..., :M_WIDTH], qk[..., M_WIDTH:]
    hm = mlstm_chunkwise(mq.reshape(B, S, M_HEADS, M_HD), mk.reshape(B, S, M_HEADS, M_HD),
                         mv.reshape(B, S, M_HEADS, M_HD), mi + m_ig_b, mf + m_fg_b)
    hm = head_standardize(hm, HEAD_NORM_EPS).reshape(B, S, M_WIDTH) * m_norm_g
    hm = hm * jax.nn.sigmoid(mo.astype(f32))

    rr, rk, rv, rwl, ral, rgl = split_cols(token_shift(r_cols, r_mu).astype(f32), R_SIZES)
    w_log = -jax.nn.softplus(-(r_w0 + jnp.tanh(rwl) @ r_w2)) - 0.5
    a = jax.nn.sigmoid(r_a0 + ral @ r_a2)
    g = jax.nn.sigmoid(rgl) @ r_g2
    kk = (rk * r_kk).reshape(B, S, R_HEADS, R_HD)
    kk = kk / jnp.maximum(jnp.sqrt(jnp.sum(jnp.square(kk), axis=-1, keepdims=True)), 1e-12)
    rk = rk * (1.0 + (a - 1.0) * r_ka)
    r4, k4, v4, a4, w4 = [t.reshape(B, S, R_HEADS, R_HD) for t in (rr, rk, rv, a, w_log)]
    y = rwkv7_scan(r4, w4, k4, v4, kk, a4)
    y = head_standardize(y, RWKV_GN_EPS).reshape(B, S, R_WIDTH) * r_gn_g + r_gn_b
    bonus = jnp.sum(r4 * k4 * r_rk.reshape(R_HEADS, R_HD), axis=-1, keepdims=True) * v4
    hr = (y + bonus.reshape(B, S, R_WIDTH)) * g

    mixed = jnp.concatenate([hm, hr], axis=-1).astype(h.dtype)
    return mixed @ w_out


def cross_attention(h, mem, wq, wkv, wo):
    B, S, _ = h.shape
    M = mem.shape[1]
    q = (h @ wq).reshape(B, S, X_HEADS, X_HD)
    kv = (mem @ wkv).reshape(B, M, 2, X_HEADS, X_HD)
    k, v = kv[:, :, 0], kv[:, :, 1]
    s = jnp.einsum('bshd,bmhd->bhsm', q, k).astype(jnp.float32) * (X_HD ** -0.5)
    p = jax.nn.softmax(s, axis=-1).astype(h.dtype)
    o = jnp.einsum('bhsm,bmhd->bshd', p, v).reshape(B, S, X_HEADS * X_HD)
    return o @ wo


def moe_ffn(h, wr, br, w1, b1, w2, b2):
    B, S, Dm = h.shape
    xt = h.reshape(-1, Dm)
    N = xt.shape[0]
    logits = (xt @ wr).astype(jnp.float32) + br.astype(jnp.float32)
    top_v, top_i = lax.top_k(logits, TOP_K)
    gates = jax.nn.softmax(top_v, axis=-1)
    n_assign = N * TOP_K
    e_flat = top_i.reshape(-1)
    tok_flat = jnp.arange(n_assign) // TOP_K
    order = jnp.argsort(e_flat)
    e_sorted = e_flat[order]
    tok_sorted = tok_flat[order]
    gate_sorted = gates.reshape(-1)[order]
    counts = jnp.bincount(e_flat, length=N_EXPERTS)
    start = jnp.cumsum(counts) - counts
    padded = (counts + MOE_BLOCK - 1) // MOE_BLOCK * MOE_BLOCK
    pad_end = jnp.cumsum(padded)
    pad_start = pad_end - padded
    dest = pad_start[e_sorted] + (jnp.arange(n_assign) - start[e_sorted])
    n_rows = ((n_assign + MOE_BLOCK - 1) // MOE_BLOCK + N_EXPERTS) * MOE_BLOCK
    n_blocks = n_rows // MOE_BLOCK
    buf = jnp.zeros((n_rows, Dm), h.dtype).at[dest].set(xt[tok_sorted])
    block_e = jnp.minimum(jnp.searchsorted(pad_end, jnp.arange(n_blocks) * MOE_BLOCK, side='right'),
                          N_EXPERTS - 1)

    def expert_block(args):
        xb, e = args
        hu = xb @ w1[e] + b1[e]
        gate, up = hu[:, :D_FF], hu[:, D_FF:]
        gate = jnp.minimum(gate, SWIGLU_LIMIT)
        up = jnp.clip(up, -SWIGLU_LIMIT, SWIGLU_LIMIT)
        glu = gate * jax.nn.sigmoid(gate * SWIGLU_ALPHA)
        return ((up + 1.0) * glu) @ w2[e] + b2[e]

    y_buf = lax.map(expert_block, (buf.reshape(n_blocks, MOE_BLOCK, Dm), block_e)).reshape(n_rows, Dm)
    y = y_buf[dest] * gate_sorted[:, None].astype(y_buf.dtype)
    out = jnp.zeros((N, Dm), y.dtype).at[tok_sorted].add(y)
    return out.reshape(B, S, Dm).astype(h.dtype)


def setup_inputs(seed: int = 0) -> dict:
    key = jax.random.key(seed)
    ks = iter(jax.random.split(key, 64))
    f32 = jnp.float32
    L = DEPTH

    def nrm(shape, scale):
        return jax.random.normal(next(ks), shape, f32) * scale

    return {
        'x': nrm((BATCH, SEQ, D_MODEL), 1.0),
        'mem': nrm((BATCH, N_MEM, D_MODEL), 1.0),
        'ln0_g': 1.0 + nrm((D_MODEL,), 0.02),
        'ln0_b': nrm((D_MODEL,), 0.02),
        'w_in': nrm((L, D_MODEL, IN_COLS), D_MODEL ** -0.5),
        'm_conv_w': nrm((L, M_CONV, 2 * M_WIDTH), M_CONV ** -0.5),
        'm_conv_b': nrm((L, 2 * M_WIDTH), 0.02),
        'm_ig_b': nrm((L, M_HEADS), 0.1),
        'm_fg_b': jnp.linspace(3.0, 6.0, M_HEADS, dtype=f32)[None] + nrm((L, M_HEADS), 0.1),
        'm_norm_g': 1.0 + nrm((L, M_WIDTH), 0.02),
        'r_mu': jax.random.uniform(next(ks), (L, R_COLS), f32),
        'r_w0': jnp.linspace(-6.0, -1.0, R_WIDTH, dtype=f32)[None] + nrm((L, R_WIDTH), 0.1),
        'r_w2': nrm((L, R_DECAY_LR, R_WIDTH), 0.1),
        'r_a0': nrm((L, R_WIDTH), 0.1),
        'r_a2': nrm((L, R_A_LR, R_WIDTH), R_A_LR ** -0.5),
        'r_g2': nrm((L, R_G_LR, R_WIDTH), R_G_LR ** -0.5),
        'r_kk': 0.85 + nrm((L, R_WIDTH), 0.05),
        'r_ka': 1.0 + nrm((L, R_WIDTH), 0.05),
        'r_rk': nrm((L, R_WIDTH), 0.1),
        'r_gn_g': 1.0 + nrm((L, R_WIDTH), 0.02),
        'r_gn_b': nrm((L, R_WIDTH), 0.02),
        'w_out': nrm((L, MIX_WIDTH, D_MODEL), MIX_WIDTH ** -0.5 * DN_BETA),
        'ln1_g': 1.0 + nrm((L, D_MODEL), 0.02),
        'ln1_b': nrm((L, D_MODEL), 0.02),
        'x_wq': nrm((L, D_MODEL, D_MODEL), D_MODEL ** -0.5),
        'x_wkv': nrm((L, D_MODEL, 2 * D_MODEL), D_MODEL ** -0.5),
        'x_wo': nrm((L, D_MODEL, D_MODEL), D_MODEL ** -0.5 * DN_BETA),
        'ln2_g': 1.0 + nrm((L, D_MODEL), 0.02),
        'ln2_b': nrm((L, D_MODEL), 0.02),
        'moe_wr': nrm((L, D_MODEL, N_EXPERTS), D_MODEL ** -0.5),
        'moe_br': nrm((L, N_EXPERTS), 0.01),
        'moe_w1': nrm((L, N_EXPERTS, D_MODEL, 2 * D_FF), D_MODEL ** -0.5),
        'moe_b1': nrm((L, N_EXPERTS, 2 * D_FF), 0.01),
        'moe_w2': nrm((L, N_EXPERTS, D_FF, D_MODEL), D_FF ** -0.5 * DN_BETA),
        'moe_b2': nrm((L, N_EXPERTS, D_MODEL), 0.01),
        'ln3_g': 1.0 + nrm((L, D_MODEL), 0.02),
        'ln3_b': nrm((L, D_MODEL), 0.02),
    }


def reference(x, mem, ln0_g, ln0_b, w_in, m_conv_w, m_conv_b, m_ig_b, m_fg_b, m_norm_g,
              r_mu, r_w0, r_w2, r_a0, r_a2, r_g2, r_kk, r_ka, r_rk, r_gn_g, r_gn_b, w_out,
              ln1_g, ln1_b, x_wq, x_wkv, x_wo, ln2_g, ln2_b, moe_wr, moe_br, moe_w1, moe_b1,
              moe_w2, moe_b2, ln3_g, ln3_b):
    h = layer_norm(x, ln0_g, ln0_b)
    for l in range(DEPTH):
        mix = hybrid_mixer(h, w_in[l], m_conv_w[l], m_conv_b[l], m_ig_b[l], m_fg_b[l], m_norm_g[l],
                           r_mu[l], r_w0[l], r_w2[l], r_a0[l], r_a2[l], r_g2[l], r_kk[l], r_ka[l],
                           r_rk[l], r_gn_g[l], r_gn_b[l], w_out[l])
        h = layer_norm(DN_ALPHA * h + mix, ln1_g[l], ln1_b[l])
        h = layer_norm(DN_ALPHA * h + cross_attention(h, mem, x_wq[l], x_wkv[l], x_wo[l]),
                       ln2_g[l], ln2_b[l])
        h = layer_norm(DN_ALPHA * h + moe_ffn(h, moe_wr[l], moe_br[l], moe_w1[l], moe_b1[l],
                                              moe_w2[l], moe_b2[l]),
                       ln3_g[l], ln3_b[l])
    return h
```

```python
import numpy as np
from contextlib import ExitStack, contextmanager
import concourse.bass as bass
import concourse.mybir as mybir
from concourse.bass_utils import run_bass_kernel_spmd

F32 = mybir.dt.float32
BF16 = mybir.dt.bfloat16
AF = mybir.ActivationFunctionType
ALU = mybir.AluOpType
AX = mybir.AxisListType

S = 2048
D = 1024
NT = S // 128
DEPTH = 2
M_W = 512
M_H = 4
M_HD = 128
R_W = 512
R_H = 8
R_HD = 64
IN_COLS = 3848
M_COLS = 2056
R_COLS = 1792
NM = 256
NE = 32
DFF = 1024
DN_ALPHA = float((2 * DEPTH) ** 0.25)
LN_EPS = 1e-5
MOE_SPARSE = True
MOE_BLK = 384
MOE_NB = (S * 4 + MOE_BLK - 1) // MOE_BLK + NE

WEIGHT_SPECS = [
    ('ln0_g', (1024,)), ('ln0_b', (1024,)), ('w_in', (2, 1024, 3848)), ('m_conv_w', (2, 4, 1024)),
    ('m_conv_b', (2, 1024)), ('m_ig_b', (2, 4)), ('m_fg_b', (2, 4)), ('m_norm_g', (2, 512)),
    ('r_mu', (2, 1792)), ('r_w0', (2, 512)), ('r_w2', (2, 64, 512)), ('r_a0', (2, 512)),
    ('r_a2', (2, 64, 512)), ('r_g2', (2, 128, 512)), ('r_kk', (2, 512)), ('r_ka', (2, 512)),
    ('r_rk', (2, 512)), ('r_gn_g', (2, 512)), ('r_gn_b', (2, 512)), ('w_out', (2, 1024, 1024)),
    ('ln1_g', (2, 1024)), ('ln1_b', (2, 1024)), ('x_wq', (2, 1024, 1024)), ('x_wkv', (2, 1024, 2048)),
    ('x_wo', (2, 1024, 1024)), ('ln2_g', (2, 1024)), ('ln2_b', (2, 1024)), ('moe_wr', (2, 1024, 32)),
    ('moe_br', (2, 32)), ('moe_w1', (2, 32, 1024, 2048)), ('moe_b1', (2, 32, 2048)),
    ('moe_w2', (2, 32, 1024, 1024)), ('moe_b2', (2, 32, 1024)), ('ln3_g', (2, 1024)), ('ln3_b', (2, 1024)),
]


class T:
    _n = 0

    def __init__(self, t, key=None, excl=False):
        self.t = t
        self.excl = excl
        T._n += 1
        self.key = key if key is not None else ('t', T._n)

    def __getitem__(self, idx):
        return self.t[idx]


class K:
    def __init__(self, nc):
        self.nc = nc
        self.es = ExitStack()
        self.engs = {'pe': nc.tensor, 'act': nc.scalar, 'dve': nc.vector, 'pool': nc.gpsimd, 'sp': nc.sync}
        self.sems = {n: self.es.enter_context(nc.semaphore('s_' + n)) for n in self.engs}
        self.cnt = {n: 0 for n in self.engs}
        self.seen = {n: {} for n in self.engs}
        self.res = {}
        self.dsem = {}
        self.ssem = {}
        self.sempool = []
        self.allsems = {('e', n): (self.sems[n], 0) for n in self.engs}
        self.stack = [self.es]
        self.uid = 0

    @contextmanager
    def phase(self):
        es = ExitStack()
        self.stack.append(es)
        try:
            yield
        finally:
            self.barrier()
            self.stack.pop()
            es.close()

    def name(self, p):
        self.uid += 1
        return f"{p}_{self.uid}"

    def sb(self, shape, dt=F32, name='sb'):
        return T(self.stack[-1].enter_context(self.nc.sbuf_tensor(self.name(name), list(shape), dt)))

    def ps(self, shape, dt=F32, name='ps'):
        return T(self.stack[-1].enter_context(self.nc.psum_tensor(self.name(name), list(shape), dt)), excl=True)

    def dram(self, name, shape, dt=F32, kind='Internal'):
        return T(self.nc.dram_tensor(name, list(shape), dt, kind=kind).ap())

    def _deps(self, reads, writes):
        deps = []
        for r in reads:
            e = self.res.get(r.key)
            if e:
                if e[0]:
                    deps.append(e[0])
                deps.extend(e[2].values())
        for w in writes:
            e = self.res.get(w.key)
            if e:
                if e[0]:
                    deps.append(e[0])
                deps.extend(e[1].values())
                deps.extend(e[2].values())
        return deps

    def _wait(self, en, deps):
        best = {}
        for (sk, sh, v) in deps:
            if en == 'pe' and sk == ('e', 'pe'):
                continue
            if self.seen[en].get(sk, 0) >= v:
                continue
            if sk not in best or best[sk][1] < v:
                best[sk] = (sh, v)
        for sk, (sh, v) in best.items():
            self.engs[en].wait_ge(sh, v)
            self.seen[en][sk] = v

    def _record(self, ev, reads, writes, stream=False):
        for r in reads:
            e = self.res.setdefault(r.key, [None, {}, {}])
            old = e[1].get(ev[0])
            if old is None or old[2] < ev[2]:
                e[1][ev[0]] = ev
        for w in writes:
            if stream:
                self.res.setdefault(w.key, [None, {}, {}])[2][ev[0]] = ev
            else:
                self.res[w.key] = [ev, {}, {}]

    NSTREAM = 4

    def _dsem(self, key, stream):
        def new():
            if self.sempool:
                return self.sempool.pop()
            return [self.es.enter_context(self.nc.semaphore(self.name('sd'))), 0]
        if not stream:
            if key not in self.dsem:
                self.dsem[key] = new()
            return self.dsem[key], None
        st = self.ssem.setdefault(key, [[], 0])
        if len(st[0]) < self.NSTREAM:
            st[0].append(new())
        ds = st[0][st[1] % len(st[0])] if len(st[0]) == self.NSTREAM else st[0][-1]
        st[1] += 1
        prev = (('d', id(ds)), ds[0], ds[1]) if ds[1] > 0 else None
        return ds, prev

    def op(self, en, fn, reads=(), writes=(), inc=True):
        writes = list(writes) + [r for r in reads if r.excl]
        reads = [r for r in reads if not r.excl]
        self._wait(en, self._deps(reads, writes))
        ins = fn(self.engs[en])
        sk = ('e', en)
        ev = (sk, self.sems[en], self.cnt[en] + 1)
        if inc:
            self.cnt[en] += 1
            ins.then_inc(self.sems[en], 1)
            self.allsems[sk] = (self.sems[en], self.cnt[en])
        self._record(ev, reads, writes)
        return ins

    def dma(self, en, out, in_, reads, write, stream=False, **kw):
        ds, prev = self._dsem(write.key, stream)
        deps = self._deps(reads, [] if stream else [write])
        if prev is not None:
            deps.append(prev)
        self._wait(en, deps)
        ds[1] += 16
        self.engs[en].dma_start(out=out, in_=in_, **kw).then_inc(ds[0], 16)
        sk = ('d', id(ds))
        ev = (sk, ds[0], ds[1])
        self.allsems[sk] = (ds[0], ds[1])
        self._record(ev, reads, [write], stream=stream)

    def idma(self, out_t, out_ap, in_t, in_ap, idx_t, idx_ap, scatter=False, bounds=None, stream=False, extra_reads=()):
        en = 'pool'
        reads = [in_t, idx_t] + list(extra_reads)
        ds, prev = self._dsem(out_t.key, stream)
        deps = self._deps(reads, [] if stream else [out_t])
        if prev is not None:
            deps.append(prev)
        self._wait(en, deps)
        ds[1] += 16
        off = bass.IndirectOffsetOnAxis(ap=idx_ap, axis=0)
        kw = {}
        if bounds is not None:
            kw = dict(bounds_check=bounds, oob_is_err=False)
        if scatter:
            ins = self.nc.gpsimd.indirect_dma_start(out=out_ap, out_offset=off, in_=in_ap, in_offset=None, **kw)
        else:
            ins = self.nc.gpsimd.indirect_dma_start(out=out_ap, out_offset=None, in_=in_ap, in_offset=off, **kw)
        ins.then_inc(ds[0], 16)
        sk = ('d', id(ds))
        ev = (sk, ds[0], ds[1])
        self.allsems[sk] = (ds[0], ds[1])
        self._record(ev, reads, [out_t], stream=stream)

    def barrier(self):
        for en in self.engs:
            deps = [(sk, sh, v) for sk, (sh, v) in self.allsems.items() if v > 0 and sk != ('e', en)]
            self._wait(en, deps)
        self.res = {}
        for ds in list(self.dsem.values()) + [d for st in self.ssem.values() for d in st[0]]:
            self.sempool.append(ds)
            self.allsems.pop(('d', id(ds)), None)
        self.dsem = {}
        self.ssem = {}

    def mm(self, out, lhsT, rhs, reads, writes, start=True, stop=True, inc=True):
        return self.op('pe', lambda e: e.matmul(out, lhsT, rhs, start=start, stop=stop), reads, writes, inc)

    def tr(self, out, in_, ident, reads, writes, inc=True):
        return self.op('pe', lambda e: e.transpose(out, in_, ident), reads, writes, inc)

    def act(self, out, in_, func, reads, writes, bias=0.0, scale=1.0, accum_out=None):
        if accum_out is None:
            return self.op('act', lambda e: e.activation(out, in_, func, bias=bias, scale=scale), reads, writes)
        return self.op('act', lambda e: e.activation(out, in_, func, bias=bias, scale=scale,
                                                     accum_out=accum_out), reads, writes)

    def tt(self, en, out, in0, in1, op, reads, writes):
        return self.op(en, lambda e: e.tensor_tensor(out, in0, in1, op), reads, writes)

    def ts(self, en, out, in0, s1, s2, op0, op1, reads, writes):
        if s2 is None:
            return self.op(en, lambda e: e.tensor_scalar(out, in0, s1, None, op0), reads, writes)
        return self.op(en, lambda e: e.tensor_scalar(out, in0, s1, s2, op0, op1), reads, writes)

    def stt(self, en, out, in0, scalar, in1, op0, op1, reads, writes):
        en = 'dve'
        return self.op(en, lambda e: e.scalar_tensor_tensor(out, in0, scalar, in1, op0, op1), reads, writes)

    def copy(self, en, out, in_, reads, writes):
        if en == 'act':
            return self.op('act', lambda e: e.copy(out, in_), reads, writes)
        return self.op(en, lambda e: e.tensor_copy(out, in_), reads, writes)

    def memset(self, en, t, ap, val):
        return self.op(en, lambda e: e.memset(ap, val), (), [t])


class Prog:
    def __init__(self, dbg=(), upto=99):
        self.nc = bass.Bass("TRN2", target_bir_lowering=False)
        self.k = K(self.nc)
        self.dbg = set(dbg)
        self.upto = upto
        nc = self.nc
        self.inp = {}
        self.inp['x'] = T(nc.dram_tensor('x', [S, D], F32, kind='ExternalInput').ap())
        self.inp['mem'] = T(nc.dram_tensor('mem', [NM, D], F32, kind='ExternalInput').ap())
        for n, shp in WEIGHT_SPECS:
            self.inp[n] = T(nc.dram_tensor(n, list(shp), F32, kind='ExternalInput').ap())
        self.out = T(nc.dram_tensor('out', [S, D], F32, kind='ExternalOutput').ap())

    def scratch(self, name, shape, dt=F32):
        kind = 'ExternalOutput' if name in self.dbg else 'Internal'
        return self.k.dram(name, shape, dt, kind=kind)

    def consts(self):
        k = self.k
        self.reg_w = self.nc.gpsimd.to_reg(2 * NE * 1024 - 1)
        self.reg_b = self.nc.gpsimd.to_reg(2 * NE * 16 - 1)
        self.ident = k.sb([128, 128], F32, 'ident')
        k.memset('pool', self.ident, self.ident[:], 1.0)
        k.op('pool', lambda e: e.affine_select(self.ident[:], self.ident[:], [[-1, 128]], ALU.is_equal, 0.0,
                                               base=0, channel_multiplier=1), [self.ident], [self.ident])
        self.identb = k.sb([128, 128], BF16, 'identb')
        k.copy('pool', self.identb[:], self.ident[:], [self.ident], [self.identb])
        self.eps_ln = k.sb([128, 1], F32, 'epsln')
        k.memset('pool', self.eps_ln, self.eps_ln[:], LN_EPS)
        self.one_c = k.sb([128, 1], F32, 'onec')
        k.memset('pool', self.one_c, self.one_c[:], 1.0)
        self.mask_le = k.sb([128, 128], F32, 'maskle')
        k.memset('pool', self.mask_le, self.mask_le[:], 1.0)
        k.op('pool', lambda e: e.affine_select(self.mask_le[:], self.mask_le[:], [[1, 128]], ALU.is_ge, 0.0,
                                               base=0, channel_multiplier=-1), [self.mask_le], [self.mask_le])
        self.pb = [k.ps([128, 512], F32, 'bank') for _ in range(8)]
        self.pbi = 0
        self.hT = None
        self.hT_es = None
        k.barrier()

    def alloc_hT(self):
        k = self.k
        self.hT_es = ExitStack()
        self.hT = T(self.hT_es.enter_context(self.nc.sbuf_tensor(k.name('hT'), [128, 8, S + 1], BF16)))
        k.memset('pool', self.hT, self.hT[:, :, 0:1], 0.0)

    def free_hT(self):
        self.hT_es.close()
        self.hT = None

    def bank(self):
        b = self.pb[self.pbi % 8]
        self.pbi += 1
        return b

    def bc_load(self, dram_t, src_ap, n, name='bc'):
        k = self.k
        t = k.sb([128, n], F32, name)
        k.dma('sp', t[:], src_ap.partition_broadcast(128), [dram_t], t)
        return t

    def ln_tile(self, z, g_bc, b_bc, ti, H, out_dram=None, defer=False):
        k = self.k
        st = k.sb([128, 2, 6], F32, 'lnst')
        mv = k.sb([128, 2], F32, 'lnmv')
        for c in range(2):
            k.op('dve', lambda e, c=c: e.bn_stats(st[:, c, :], z[:, c * 512:(c + 1) * 512]), [z], [st])
        k.op('dve', lambda e: e.bn_aggr(mv[:], st[:].rearrange("p a b -> p (a b)")), [st], [mv])
        rstd = k.sb([128, 1], F32, 'lnr')
        k.act(rstd[:], mv[:, 1:2], AF.Sqrt, [mv, self.eps_ln], [rstd], bias=self.eps_ln[:], scale=1.0)
        k.op('dve', lambda e: e.reciprocal(rstd[:], rstd[:]), [rstd], [rstd])
        k.ts('dve', z[:], z[:], mv[:, 0:1], rstd[:], ALU.subtract, ALU.mult, [z, mv, rstd], [z])
        k.tt('pool', z[:], z[:], g_bc[:], ALU.mult, [z, g_bc], [z])
        k.tt('dve', z[:], z[:], b_bc[:], ALU.add, [z, b_bc], [z])
        tgt = out_dram if out_dram is not None else H
        k.dma('sp', tgt[ti * 128:(ti + 1) * 128, :], z[:], [z], tgt, stream=True)
        if out_dram is None:
            if defer:
                return lambda: self.to_hT(z, ti)
            self.to_hT(z, ti)
        return None

    def to_hT(self, z, ti):
        k = self.k
        for half in range(2):
            b = self.bank()
            for j in range(4):
                c = half * 4 + j
                k.tr(b[:, j * 128:(j + 1) * 128], z[:, c * 128:(c + 1) * 128], self.ident[:], [z, self.ident], [b],
                     inc=(j == 3))
            en = 'act' if half == 0 else 'dve'
            k.copy(en, self.hT[:, half * 4:(half + 1) * 4, 1 + ti * 128:1 + (ti + 1) * 128],
                   b[:].rearrange("p (c t) -> p c t", c=4), [b], [self.hT])

    def phase_ln0(self, H):
        k = self.k
        with k.phase():
            g = self.bc_load(self.inp['ln0_g'], self.inp['ln0_g'][:], D)
            b = self.bc_load(self.inp['ln0_b'], self.inp['ln0_b'][:], D)
            zs = [k.sb([128, D], F32, 'z') for _ in range(2)]
            for ti in range(NT):
                z = zs[ti % 2]
                k.dma('sp', z[:], self.inp['x'][ti * 128:(ti + 1) * 128, :], [self.inp['x']], z)
                self.ln_tile(z, g, b, ti, H)

    def load_w(self, src_t, ap, n, name='w', dt=BF16):
        k = self.k
        wt = k.sb([128, 8, n], dt, name)
        eng = 'pool' if dt != F32 else 'sp'
        k.dma(eng, wt[:], ap.rearrange("(c p) n -> p c n", p=128), [src_t], wt)
        return wt

    def phase_mixproj(self, l, qkT, IF, PT):
        k = self.k
        w_in = self.inp['w_in']
        with k.phase():
            wqk = self.load_w(w_in, w_in[l, :, 0:1024], 1024, 'wqk')
            cw = k.sb([128, 4, 8], F32, 'cw')
            for j in range(4):
                k.dma('sp', cw[:, j, :], self.inp['m_conv_w'][l, j].rearrange("(c p) -> p c", p=128),
                      [self.inp['m_conv_w']], cw, allow_slow_non_contiguous=True)
            cb = k.sb([128, 8], F32, 'cb')
            k.dma('sp', cb[:], self.inp['m_conv_b'][l].rearrange("(c p) -> p c", p=128), [self.inp['m_conv_b']], cb,
                  allow_slow_non_contiguous=True)
            raws = [k.sb([128, S + 3], F32, 'raw') for _ in range(2)]
            accs = [k.sb([128, S], F32, 'acc') for _ in range(2)]
            for r in raws:
                k.memset('pool', r, r[:, 0:3], 0.0)
            for c in range(8):
                raw = raws[c % 2]
                acc = accs[c % 2]
                for tb in range(4):
                    b = self.bank()
                    for kc in range(8):
                        k.mm(b[:], wqk[:, kc, c * 128:(c + 1) * 128], self.hT[:, kc, 1 + tb * 512:1 + (tb + 1) * 512],
                             [wqk, self.hT], [b], start=(kc == 0), stop=(kc == 7), inc=(kc == 7))
                    k.copy('act', raw[:, 3 + tb * 512:3 + (tb + 1) * 512], b[:], [b], [raw])
                en = 'dve' if c % 2 == 0 else 'pool'
                k.ts(en, acc[:], raw[:, 3:3 + S], cw[:, 3, c:c + 1], cb[:, c:c + 1], ALU.mult, ALU.add, [raw, cw, cb], [acc])
                for j in range(3):
                    k.stt(en, acc[:], raw[:, j:j + S], cw[:, j, c:c + 1], acc[:], ALU.mult, ALU.add, [raw, cw, acc], [acc])
                k.act(acc[:], acc[:], AF.Silu, [acc], [acc])
                k.ts(en, qkT[:, c, :], acc[:], (1.0 if c < 4 else float(M_HD ** -0.5)), None, ALU.mult, None, [acc], [qkT])
            wif = self.load_w(w_in, w_in[l, :, 2048:2056], 8, 'wif')
            gb = k.sb([8, 1], F32, 'gb')
            k.dma('sp', gb[0:4, :], self.inp['m_ig_b'][l].rearrange("(a b) -> a b", b=1), [self.inp['m_ig_b']], gb)
            k.dma('sp', gb[4:8, :], self.inp['m_fg_b'][l].rearrange("(a b) -> a b", b=1), [self.inp['m_fg_b']], gb)
            ifs = k.sb([8, S], F32, 'ifs')
            for tb in range(4):
                b = self.bank()
                for kc in range(8):
                    k.mm(b[0:8, :], wif[:, kc, :], self.hT[:, kc, 1 + tb * 512:1 + (tb + 1) * 512],
                         [wif, self.hT], [b], start=(kc == 0), stop=(kc == 7), inc=(kc == 7))
                k.ts('dve', ifs[:, tb * 512:(tb + 1) * 512], b[0:8, :], gb[:, 0:1], None, ALU.add, None, [b, gb], [ifs])
            k.dma('sp', IF[:, :], ifs[:], [ifs], IF)
            mu = self.bc_load(self.inp['r_mu'], self.inp['r_mu'][l], R_COLS, 'mu')
            groups = [(1024, 512, 0, False), (1536, 512, 512, False)]
            off = 0
            while off < R_COLS:
                n = min(512, R_COLS - off)
                groups.append((M_COLS + off, n, 1024 + off, True))
                off += n
            outs = [k.sb([128, 512], F32, 'pto') for _ in range(3)]
            oi = 0
            for (c0, n, o0, shifted) in groups:
                with k.phase():
                    if not shifted:
                        wc = self.load_w(w_in, w_in[l, :, c0:c0 + n], n, 'wg')
                        wp = None
                    else:
                        wf = self.load_w(w_in, w_in[l, :, c0:c0 + n], n, 'wf', dt=F32)
                        wp = k.sb([128, 8, n], BF16, 'wp')
                        wc = k.sb([128, 8, n], BF16, 'wc')
                        tmp = k.sb([128, 8, n], F32, 'wtmp')
                        r0 = c0 - M_COLS
                        for kc in range(8):
                            en = 'dve' if kc % 2 == 0 else 'pool'
                            k.tt(en, tmp[:, kc, :], wf[:, kc, :], mu[:, r0:r0 + n], ALU.mult, [wf, mu], [tmp])
                            k.copy('act', wp[:, kc, :], tmp[:, kc, :], [tmp], [wp])
                            k.tt(en, wc[:, kc, :], wf[:, kc, :], tmp[:, kc, :], ALU.subtract, [wf, tmp], [wc])
                    for ti in range(NT):
                        b = self.bank()
                        nmm = 16 if shifted else 8
                        i = 0
                        for kc in range(8):
                            k.mm(b[:, 0:n], self.hT[:, kc, 1 + ti * 128:1 + (ti + 1) * 128], wc[:, kc, :],
                                 [self.hT, wc], [b], start=(i == 0), stop=(i == nmm - 1), inc=(i == nmm - 1))
                            i += 1
                        if shifted:
                            for kc in range(8):
                                k.mm(b[:, 0:n], self.hT[:, kc, ti * 128:(ti + 1) * 128], wp[:, kc, :],
                                     [self.hT, wp], [b], start=False, stop=(i == nmm - 1), inc=(i == nmm - 1))
                                i += 1
                        o = outs[oi % 3]
                        oi += 1
                        k.copy('act' if ti % 2 == 0 else 'dve', o[:, 0:n], b[:, 0:n], [b], [o])
                        k.dma('sp', PT[ti * 128:(ti + 1) * 128, o0:o0 + n], o[:, 0:n], [o], PT, stream=True)

    def phase_mlstm(self, l, qkT, IF, PT, MIX):
        k = self.k
        NCk = 16
        sc = self.scratch(f'msc{l}', [8, 64])
        with k.phase():
            Gi = k.sb([64, 128], F32, 'Gi')
            Gf = k.sb([64, 128], F32, 'Gf')
            k.dma('sp', Gi[:], IF[0:4, :].rearrange("h (c p) -> (h c) p", p=128), [IF], Gi)
            k.dma('sp', Gf[:], IF[4:8, :].rearrange("h (c p) -> (h c) p", p=128), [IF], Gf)
            ones = k.sb([64, 128], F32, 'ones')
            k.memset('pool', ones, ones[:], 1.0)
            k.act(Gf[:], Gf[:], AF.Exp, [Gf], [Gf], scale=-1.0)
            k.act(Gf[:], Gf[:], AF.Ln, [Gf, self.one_c], [Gf], bias=self.one_c[0:64, :], scale=1.0)
            csp = k.sb([64, 128], F32, 'csp')
            k.op('dve', lambda e: e.tensor_tensor_scan(csp[:], ones[:], Gf[:], 0.0, ALU.mult, ALU.add), [ones, Gf], [csp])
            u = k.sb([64, 128], F32, 'u')
            k.tt('dve', u[:], Gi[:], csp[:], ALU.add, [Gi, csp], [u])
            col = k.sb([64, 2], F32, 'col')
            k.op('dve', lambda e: e.reduce_max(col[:, 0:1], u[:], AX.X), [u], [col])
            k.ts('dve', col[:, 1:2], csp[:, 127:128], -1.0, None, ALU.mult, None, [csp], [col])
            k.dma('sp', sc[0, :].rearrange("(a b) -> a b", b=1), col[:, 0:1], [col], sc)
            k.dma('sp', sc[1, :].rearrange("(a b) -> a b", b=1), col[:, 1:2], [col], sc)
            hm = k.sb([4, 2, 16], F32, 'hm')
            k.dma('sp', hm[:, 0, :], sc[0, :].rearrange("(h c) -> h c", c=16), [sc], hm)
            k.dma('sp', hm[:, 1, :], sc[1, :].rearrange("(h c) -> h c", c=16), [sc], hm)
            mn = k.sb([4, 17], F32, 'mn')
            k.memset('dve', mn, mn[:, 0:1], 0.0)
            k.op('dve', lambda e: e.tensor_tensor_scan(mn[:, 1:17], hm[:, 0, :], hm[:, 1, :], 0.0, ALU.max, ALU.add),
                 [hm], [mn])
            ra = k.sb([4, 2, 16], F32, 'ra')
            k.tt('dve', ra[:, 0, :], mn[:, 0:16], hm[:, 0, :], ALU.max, [mn, hm], [ra])
            k.tt('dve', ra[:, 1, :], mn[:, 0:16], ra[:, 0, :], ALU.subtract, [mn, ra], [ra])
            k.ts('dve', ra[:, 0, :], ra[:, 0, :], -1.0, None, ALU.mult, None, [ra], [ra])
            k.dma('sp', sc[2, :].rearrange("(h c) -> h c", c=16), ra[:, 0, :], [ra], sc)
            k.dma('sp', sc[3, :].rearrange("(h c) -> h c", c=16), ra[:, 1, :], [ra], sc)
            negR = k.sb([64, 1], F32, 'negR')
            k.dma('sp', negR[:], sc[2, :].rearrange("(a b) -> a b", b=1), [sc], negR)
            alpha = k.sb([128, 64], F32, 'alpha')
            k.dma('sp', alpha[:], sc[3, :].partition_broadcast(128), [sc], alpha)
            k.act(alpha[:], alpha[:], AF.Exp, [alpha], [alpha])
            k.act(u[:], u[:], AF.Exp, [u, negR], [u], bias=negR[:], scale=1.0)
            k.act(csp[:], csp[:], AF.Exp, [csp, negR], [csp], bias=negR[:], scale=1.0)
            Etm = k.sb([128, 64], F32, 'Etm')
            Ttm = k.sb([128, 64], F32, 'Ttm')
            b = self.bank()
            k.tr(b[:, 0:64], u[:], self.ident[0:64, 0:64], [u, self.ident], [b])
            k.copy('dve', Etm[:], b[:, 0:64], [b], [Etm])
            b = self.bank()
            k.tr(b[:, 0:64], csp[:], self.ident[0:64, 0:64], [csp, self.ident], [b])
            k.copy('dve', Ttm[:], b[:, 0:64], [b], [Ttm])
            Cst = [k.sb([128, 129], F32, 'Cst') for _ in range(4)]
            for h in range(4):
                k.memset('pool', Cst[h], Cst[h][:], 0.0)
            Csb = [k.sb([128, 129], BF16, 'Csb') for _ in range(4)]
            Vs = [k.sb([128, 129], BF16, 'Vs') for _ in range(4)]
            Sm = [k.sb([128, 128], BF16, 'Sm') for _ in range(4)]
            kTm = [k.sb([128, 128], BF16, 'kTm') for _ in range(4)]
            vts = [k.sb([128, 1024], F32, 'vt') for _ in range(2)]
            hns = [k.sb([128, 512], F32, 'hn') for _ in range(2)]
            sml = [k.sb([128, 16], F32, 'sml') for _ in range(4)]
            for c in range(NCk):
                vt = vts[c % 2]
                hn = hns[c % 2]
                k.dma('sp', vt[:], PT[c * 128:(c + 1) * 128, 0:1024], [PT], vt)
                tsl = slice(c * 128, (c + 1) * 128)
                for h in range(4):
                    hc = h * 16 + c
                    sm = sml[h]
                    k.op('act', lambda e, h=h, hc=hc: e.activation(Vs[h][:, 0:128], vt[:, h * 128:(h + 1) * 128], AF.Copy,
                                                                 scale=Etm[:, hc:hc + 1]), [vt, Etm], [Vs[h]])
                    k.copy('dve', Vs[h][:, 128:129], Etm[:, hc:hc + 1], [Etm], [Vs[h]])
                    b1 = self.bank()
                    k.mm(b1[:, 0:128], qkT[:, 4 + h, tsl], qkT[:, h, tsl], [qkT], [b1])
                    k.tt('dve', Sm[h][:], b1[:, 0:128], self.mask_le[:], ALU.mult, [b1, self.mask_le], [Sm[h]])
                    b2 = self.bank()
                    k.mm(b2[:, 0:128], qkT[:, 4 + h, tsl], self.identb[:], [qkT, self.identb], [b2])
                    k.copy('act', kTm[h][:], b2[:, 0:128], [b2], [kTm[h]])
                    k.ts('dve', Csb[h][:], Cst[h][:], alpha[:, hc:hc + 1], None, ALU.mult, None, [Cst[h], alpha], [Csb[h]])
                    b3 = self.bank()
                    k.mm(b3[:, 0:129], Sm[h][:], Vs[h][:], [Sm[h], Vs[h]], [b3], start=True, stop=False, inc=False)
                    k.mm(b3[:, 0:129], qkT[:, h, tsl], Csb[h][:], [qkT, Csb[h]], [b3], start=False, stop=True)
                    b4 = self.bank()
                    k.mm(b4[:, 0:129], kTm[h][:], Vs[h][:], [kTm[h], Vs[h]], [b4])
                    k.stt('dve', Cst[h][:], Cst[h][:], alpha[:, hc:hc + 1], b4[:, 0:129], ALU.mult, ALU.add,
                          [Cst[h], alpha, b4], [Cst[h]])
                    k.copy('act', sm[:, 12:13], b3[:, 128:129], [b3], [sm])
                    k.stt('dve', sm[:, 13:14], sm[:, 12:13], -1.0, sm[:, 12:13], ALU.mult, ALU.max, [sm], [sm])
                    k.tt('dve', sm[:, 0:1], sm[:, 13:14], Ttm[:, hc:hc + 1], ALU.max, [sm, Ttm], [sm])
                    k.op('dve', lambda e, sm=sm: e.reciprocal(sm[:, 1:2], sm[:, 0:1]), [sm], [sm])
                    hs = hn[:, h * 128:(h + 1) * 128]
                    k.ts('dve', hs, b3[:, 0:128], sm[:, 1:2], None, ALU.mult, None, [b3, sm], [hn])
                    k.op('dve', lambda e, sm=sm, hs=hs: e.bn_stats(sm[:, 2:8], hs), [hn], [sm])
                    k.op('dve', lambda e, sm=sm: e.bn_aggr(sm[:, 8:10], sm[:, 2:8]), [sm], [sm])
                    k.act(sm[:, 10:11], sm[:, 9:10], AF.Sqrt, [sm, self.eps_ln], [sm], bias=self.eps_ln[:], scale=1.0)
                    k.op('dve', lambda e, sm=sm: e.reciprocal(sm[:, 11:12], sm[:, 10:11]), [sm], [sm])
                    k.ts('dve', hs, hs, sm[:, 8:9], sm[:, 11:12], ALU.subtract, ALU.mult, [hn, sm], [hn])
                k.act(vt[:, 512:1024], vt[:, 512:1024], AF.Sigmoid, [vt], [vt])
                k.tt('dve', hn[:], hn[:], vt[:, 512:1024], ALU.mult, [hn, vt], [hn])
                k.dma('sp', MIX[c * 128:(c + 1) * 128, 0:512], hn[:], [hn], MIX, stream=True)

    def phase_rwkv(self, l, PT, MIX):
        k = self.k
        P64 = slice(0, 64)
        with k.phase():
            def bc(n, nm):
                return self.bc_load(self.inp[n], self.inp[n][l], 512, nm)
            w0, a0, kkp, ka, rrk, gng, gnb = (bc('r_w0', 'w0'), bc('r_a0', 'a0'), bc('r_kk', 'kkp'), bc('r_ka', 'ka'),
                                               bc('r_rk', 'rrk'), bc('r_gn_g', 'gng'), bc('r_gn_b', 'gnb'))
            omk = k.sb([128, 512], F32, 'omk')
            k.ts('dve', omk[:], ka[:], -1.0, 1.0, ALU.mult, ALU.add, [ka], [omk])
            w2 = k.sb([64, 512], F32, 'w2')
            a2 = k.sb([64, 512], F32, 'a2')
            g2 = k.sb([128, 512], F32, 'g2')
            k.dma('sp', w2[:], self.inp['r_w2'][l], [self.inp['r_w2']], w2)
            k.dma('sp', a2[:], self.inp['r_a2'][l], [self.inp['r_a2']], a2)
            k.dma('sp', g2[:], self.inp['r_g2'][l], [self.inp['r_g2']], g2)

            def b2(t):
                return t[P64, :].unsqueeze(1).to_broadcast([64, 2, 512])

            def mk_mask(pattern_cm, op, base=0):
                m = k.sb([64, 8, 64], F32, 'msk')
                k.memset('pool', m, m[:], 1.0)
                k.op('pool', lambda e: e.affine_select(m[:, 0, :], m[:, 0, :], [[pattern_cm[0], 64]], op, 0.0,
                                                       base=base, channel_multiplier=pattern_cm[1]), [m], [m])
                for i in range(1, 8):
                    k.copy('pool', m[:, i, :], m[:, 0, :], [m], [m])
                return m
            mSU = mk_mask((1, -1), ALU.is_gt)
            mSL = mk_mask((-1, 1), ALU.is_gt)
            mIU = mk_mask((1, -1), ALU.is_ge)
            I8 = mk_mask((1, -1), ALU.is_equal)
            tri = mIU[:, 0, :]
            ST = k.sb([64, 8, 64], F32, 'ST')
            k.memset('pool', ST, ST[:], 0.0)

            def tm(nm):
                return k.sb([64, 2, 512], F32, nm)

            def pair(f):
                return [f(), f()]
            lw, av, gam, gprev, ginv = tm('lw'), tm('av'), tm('gam'), tm('gprev'), tm('ginv')
            kk, rk2, t1, t2, yv = tm('kk'), tm('rk2'), tm('t1'), tm('t2'), tm('yv')
            lrw = k.sb([64, 128], F32, 'lrw')
            lra = k.sb([64, 128], F32, 'lra')
            lrg = k.sb([128, 128], F32, 'lrg')
            QTb = {q: k.sb([64, 16, 64], BF16, 'QTb' + q) for q in ('a', 'b', 'k', 'r')}
            Mc, Nc, Mn, Nn, Qm = [k.sb([64, 16, 64], BF16, 'nm') for _ in range(5)]
            Wsb = k.sb([64, 8, 64], F32, 'Wsb')
            Usb = k.sb([64, 8, 64], F32, 'Usb')
            xt_p = pair(lambda: k.sb([64, 2, R_COLS], F32, 'xt'))
            gv_p, bt_p, kt_p = pair(lambda: tm('gv')), pair(lambda: tm('bt')), pair(lambda: tm('kt'))
            sm_p = pair(lambda: k.sb([64, 8, 16], F32, 'rsm'))
            gL_p = pair(lambda: k.sb([64, 16], F32, 'gL'))
            QTa_p, QTr_p = pair(lambda: k.sb([64, 16, 64], F32, 'QTa')), pair(lambda: k.sb([64, 16, 64], F32, 'QTr'))
            Pm_p = pair(lambda: k.sb([64, 16, 64], F32, 'Pm'))
            Aak_p, Arb_p, Ark_p = [pair(lambda: k.sb([64, 16, 64], F32, 'am')) for _ in range(3)]

            def h3(ap):
                return ap.rearrange("p j (h c) -> p j h c", c=64)

            def s3(ap16):
                return ap16.rearrange("p (j h) -> p j h", j=2)

            def bch(ap16):
                return s3(ap16).unsqueeze(3).to_broadcast([64, 2, 8, 64])

            def v3(bb):
                return bb[P64, :].rearrange("p (h c) -> p h c", c=64)

            def front(it):
                p = it % 2
                xt, gv, bt, kt, sm, gL = xt_p[p], gv_p[p], bt_p[p], kt_p[p], sm_p[p], gL_p[p]
                QT = {'a': QTa_p[p], 'r': QTr_p[p]}
                Pm, Aak, Arb, Ark = Pm_p[p], Aak_p[p], Arb_p[p], Ark_p[p]
                r0 = it * 128
                rr_ = xt[:, :, 0:512]
                rkx = xt[:, :, 512:1024]
                k.dma('sp', xt[:], PT[r0:r0 + 128, 1024:1024 + R_COLS].rearrange("(j p) c -> p j c", p=64), [PT], xt)
                b = self.bank()
                for j in range(2):
                    k.tr(b[P64, j * 64:(j + 1) * 64], xt[:, j, 1536:1600], self.ident[P64, P64], [xt, self.ident], [b], inc=False)
                    k.tr(b[P64, 128 + j * 64:128 + (j + 1) * 64], xt[:, j, 1600:1664], self.ident[P64, P64],
                         [xt, self.ident], [b], inc=False)
                    k.tr(b[:, 256 + j * 64:256 + (j + 1) * 64], xt[:, j, 1664:1792], self.ident[P64, P64],
                         [xt, self.ident], [b], inc=(j == 1))
                k.act(lrw[:], b[P64, 0:128], AF.Tanh, [b], [lrw])
                k.copy('dve', lra[:], b[P64, 128:256], [b], [lra])
                k.act(lrg[:], b[:, 256:384], AF.Sigmoid, [b], [lrg])
                for j in range(2):
                    js = slice(j * 64, (j + 1) * 64)
                    b = self.bank()
                    k.mm(b[P64, :], lrw[:, js], w2[:], [lrw, w2], [b])
                    k.tt('dve', lw[:, j, :], b[P64, :], w0[P64, :], ALU.add, [b, w0], [lw])
                    b = self.bank()
                    k.mm(b[P64, :], lra[:, js], a2[:], [lra, a2], [b])
                    k.tt('dve', av[:, j, :], b[P64, :], a0[P64, :], ALU.add, [b, a0], [av])
                    b = self.bank()
                    k.mm(b[P64, :], lrg[:, js], g2[:], [lrg, g2], [b])
                    k.copy('act', gv[:, j, :], b[P64, :], [b], [gv])
                k.act(lw[:], lw[:], AF.Sigmoid, [lw], [lw])
                k.ts('dve', lw[:], lw[:], -0.6065306597126334, None, ALU.mult, None, [lw], [lw])
                k.act(av[:], av[:], AF.Sigmoid, [av], [av])
                for j in range(2):
                    b = self.bank()
                    k.mm(b[P64, :], tri, lw[:, j, :], [mIU, lw], [b])
                    k.act(gam[:, j, :], b[P64, :], AF.Exp, [b], [gam])
                    k.act(ginv[:, j, :], b[P64, :], AF.Exp, [b], [ginv], scale=-1.0)
                    k.tt('dve', gprev[:, j, :], b[P64, :], lw[:, j, :], ALU.subtract, [b, lw], [gprev])
                k.act(gprev[:], gprev[:], AF.Exp, [gprev], [gprev])
                b = self.bank()
                for j in range(2):
                    for h in range(8):
                        blk = j * 8 + h
                        k.mm(b[P64, blk:blk + 1], lw[:, j, h * 64:(h + 1) * 64], self.one_c[P64, :], [lw, self.one_c], [b],
                             inc=(blk == 15))
                k.act(gL[:], b[P64, 0:16], AF.Exp, [b], [gL])
                yield
                k.tt('dve', kk[:], rkx, b2(kkp), ALU.mult, [xt, kkp], [kk])
                k.tt('dve', t1[:], kk[:], kk[:], ALU.mult, [kk], [t1])
                k.op('dve', lambda e: e.reduce_sum(s3(sm[:, 0, :]), h3(t1[:]), AX.X), [t1], [sm])
                k.act(sm[:, 1, :], sm[:, 0, :], AF.Sqrt, [sm], [sm])
                k.ts('dve', sm[:, 1, :], sm[:, 1, :], 1e-12, None, ALU.max, None, [sm], [sm])
                k.op('dve', lambda e: e.reciprocal(sm[:, 2, :], sm[:, 1, :]), [sm], [sm])
                k.tt('dve', h3(kk[:]), h3(kk[:]), bch(sm[:, 2, :]), ALU.mult, [kk, sm], [kk])
                k.tt('dve', t1[:], av[:], b2(ka), ALU.mult, [av, ka], [t1])
                k.tt('dve', t1[:], t1[:], b2(omk), ALU.add, [t1, omk], [t1])
                k.tt('dve', rk2[:], rkx, t1[:], ALU.mult, [xt, t1], [rk2])
                k.tt('dve', t1[:], rr_, rk2[:], ALU.mult, [xt, rk2], [t1])
                k.tt('dve', t1[:], t1[:], b2(rrk), ALU.mult, [t1, rrk], [t1])
                k.op('dve', lambda e: e.reduce_sum(s3(sm[:, 3, :]), h3(t1[:]), AX.X), [t1], [sm])
                k.stt('dve', gprev[:], kk[:], -1.0, gprev[:], ALU.mult, ALU.mult, [kk, gprev], [gprev])
                k.tt('dve', bt[:], kk[:], av[:], ALU.mult, [kk, av], [bt])
                k.tt('dve', bt[:], bt[:], ginv[:], ALU.mult, [bt, ginv], [bt])
                k.tt('dve', kt[:], rk2[:], ginv[:], ALU.mult, [rk2, ginv], [kt])
                k.tt('dve', gam[:], rr_, gam[:], ALU.mult, [xt, gam], [gam])
                yield
                ei = 0
                for q, src in (('a', gprev), ('b', bt), ('k', kt), ('r', gam)):
                    for j in range(2):
                        b = self.bank()
                        for h in range(8):
                            k.tr(b[P64, h * 64:(h + 1) * 64], src[:, j, h * 64:(h + 1) * 64], self.ident[P64, P64],
                                 [src, self.ident], [b], inc=(h == 7))
                        k.copy('act' if ei % 2 == 0 else 'dve', QTb[q][:, j * 8:(j + 1) * 8, :], v3(b), [b], [QTb[q]])
                        if q in QT:
                            k.copy('dve' if ei % 2 == 0 else 'act', QT[q][:, j * 8:(j + 1) * 8, :], v3(b), [b], [QT[q]])
                        ei += 1
                    if q == 'b':
                        yield
                yield
                specs = [('b', 'a', mSU, Mc), ('a', 'b', mSL, Nc), ('k', 'a', mSU, Aak), ('b', 'r', mIU, Arb),
                         ('k', 'r', mIU, Ark)]
                for si, (ql, qr, msk, dst) in enumerate(specs):
                    for j in range(2):
                        b = self.bank()
                        for h in range(8):
                            blk = j * 8 + h
                            k.mm(b[P64, h * 64:(h + 1) * 64], QTb[ql][:, blk, :], QTb[qr][:, blk, :], [QTb[ql], QTb[qr]], [b],
                                 inc=(h == 7))
                        k.tt('dve', dst[:, j * 8:(j + 1) * 8, :], v3(b), msk[:], ALU.mult, [b, msk], [dst])
                    if si == 1:
                        yield
                for j in range(2):
                    js = slice(j * 8, (j + 1) * 8)
                    k.tt('dve', Pm[:, js, :], Mc[:, js, :], I8[:], ALU.add, [Mc, I8], [Pm])
                    k.tt('dve', Qm[:, js, :], Nc[:, js, :], I8[:], ALU.add, [Nc, I8], [Qm])
                yield
                mc, ncur, mn, nn = Mc, Nc, Mn, Nn
                for lvl in range(5):
                    last = (lvl == 4)
                    bms, bns, bps, bqs = [], [], [], []
                    for j in range(2):
                        bm = self.bank()
                        for h in range(8):
                            blk = j * 8 + h
                            k.mm(bm[P64, h * 64:(h + 1) * 64], ncur[:, blk, :], mc[:, blk, :], [ncur, mc], [bm], inc=(h == 7))
                        bms.append(bm)
                        if not last:
                            bn = self.bank()
                            for h in range(8):
                                blk = j * 8 + h
                                k.mm(bn[P64, h * 64:(h + 1) * 64], mc[:, blk, :], ncur[:, blk, :], [ncur, mc], [bn],
                                     inc=(h == 7))
                            bns.append(bn)
                    for j in range(2):
                        js = slice(j * 8, (j + 1) * 8)
                        k.copy('act', mn[:, js, :], v3(bms[j]), [bms[j]], [mn])
                        if not last:
                            k.copy('dve', nn[:, js, :], v3(bns[j]), [bns[j]], [nn])
                    for j in range(2):
                        bp = self.bank()
                        for h in range(8):
                            blk = j * 8 + h
                            k.mm(bp[P64, h * 64:(h + 1) * 64], Qm[:, blk, :], mn[:, blk, :], [Qm, mn], [bp], inc=(h == 7))
                        bps.append(bp)
                        if not last:
                            bq = self.bank()
                            for h in range(8):
                                blk = j * 8 + h
                                k.mm(bq[P64, h * 64:(h + 1) * 64], mn[:, blk, :], Qm[:, blk, :], [Qm, mn], [bq],
                                     inc=(h == 7))
                            bqs.append(bq)
                    for j in range(2):
                        js = slice(j * 8, (j + 1) * 8)
                        k.tt('dve', Pm[:, js, :], Pm[:, js, :], v3(bps[j]), ALU.add, [Pm, bps[j]], [Pm])
                        if not last:
                            k.tt('dve', Qm[:, js, :], Qm[:, js, :], v3(bqs[j]), ALU.add, [Qm, bqs[j]], [Qm])
                    mc, mn = mn, mc
                    ncur, nn = nn, ncur
                    yield

            def back(it):
                p = it % 2
                xt, gv, bt, kt, sm, gL = xt_p[p], gv_p[p], bt_p[p], kt_p[p], sm_p[p], gL_p[p]
                QT = {'a': QTa_p[p], 'r': QTr_p[p]}
                Pm, Aak, Arb, Ark = Pm_p[p], Aak_p[p], Arb_p[p], Ark_p[p]
                r0 = it * 128
                rv = xt[:, :, 1024:1536]
                for j in range(2):
                    bw = self.bank()
                    for h in range(8):
                        blk = j * 8 + h
                        hs = slice(h * 64, (h + 1) * 64)
                        k.mm(bw[P64, hs], QT['a'][:, blk, :], ST[:, h, :], [QT['a'], ST], [bw], start=True, stop=False, inc=False)
                        k.mm(bw[P64, hs], Aak[:, blk, :], xt[:, j, 1024 + h * 64:1024 + (h + 1) * 64], [Aak, xt], [bw],
                             start=False, stop=True, inc=(h == 7))
                    k.copy('act', Wsb[:], v3(bw), [bw], [Wsb])
                    yield
                    bu = self.bank()
                    for h in range(8):
                        blk = j * 8 + h
                        k.mm(bu[P64, h * 64:(h + 1) * 64], Pm[:, blk, :], Wsb[:, h, :], [Pm, Wsb], [bu], inc=(h == 7))
                    k.copy('act', Usb[:], v3(bu), [bu], [Usb])
                    yield
                    by = self.bank()
                    bs_ = self.bank()
                    for h in range(8):
                        hs = slice(h * 64, (h + 1) * 64)
                        vh = xt[:, j, 1024 + h * 64:1024 + (h + 1) * 64]
                        k.mm(bs_[P64, hs], bt[:, j, hs], Usb[:, h, :], [bt, Usb], [bs_], start=True, stop=False, inc=False)
                        k.mm(bs_[P64, hs], kt[:, j, hs], vh, [kt, xt], [bs_], start=False, stop=True, inc=(h == 7))
                    for h in range(8):
                        blk = j * 8 + h
                        hs = slice(h * 64, (h + 1) * 64)
                        vh = xt[:, j, 1024 + h * 64:1024 + (h + 1) * 64]
                        k.mm(by[P64, hs], QT['r'][:, blk, :], ST[:, h, :], [QT['r'], ST], [by], start=True, stop=False, inc=False)
                        k.mm(by[P64, hs], Arb[:, blk, :], Usb[:, h, :], [Arb, Usb], [by], start=False, stop=False, inc=False)
                        k.mm(by[P64, hs], Ark[:, blk, :], vh, [Ark, xt], [by], start=False, stop=True, inc=(h == 7))
                    k.tt('dve', ST[:], ST[:], v3(bs_), ALU.add, [ST, bs_], [ST])
                    k.tt('dve', ST[:], ST[:], gL[:, j * 8:(j + 1) * 8].unsqueeze(2).to_broadcast([64, 8, 64]), ALU.mult,
                         [ST, gL], [ST])
                    k.copy('act', yv[:, j, :], by[P64, :], [by], [yv])
                    yield
                y3 = h3(yv[:])
                k.op('dve', lambda e: e.reduce_sum(s3(sm[:, 4, :]), y3, AX.X), [yv], [sm])
                k.ts('dve', sm[:, 4, :], sm[:, 4, :], 1.0 / 64, None, ALU.mult, None, [sm], [sm])
                k.tt('dve', y3, y3, bch(sm[:, 4, :]), ALU.subtract, [yv, sm], [yv])
                k.tt('dve', t2[:], yv[:], yv[:], ALU.mult, [yv], [t2])
                k.op('dve', lambda e: e.reduce_sum(s3(sm[:, 5, :]), h3(t2[:]), AX.X), [t2], [sm])
                k.ts('dve', sm[:, 5, :], sm[:, 5, :], 1.0 / 64, 64e-5, ALU.mult, ALU.add, [sm], [sm])
                k.act(sm[:, 5, :], sm[:, 5, :], AF.Sqrt, [sm], [sm])
                k.op('dve', lambda e: e.reciprocal(sm[:, 6, :], sm[:, 5, :]), [sm], [sm])
                k.tt('dve', y3, y3, bch(sm[:, 6, :]), ALU.mult, [yv, sm], [yv])
                yield
                k.tt('dve', yv[:], yv[:], b2(gng), ALU.mult, [yv, gng], [yv])
                k.tt('dve', yv[:], yv[:], b2(gnb), ALU.add, [yv, gnb], [yv])
                k.tt('dve', h3(t2[:]), h3(rv), bch(sm[:, 3, :]), ALU.mult, [xt, sm], [t2])
                k.tt('dve', yv[:], yv[:], t2[:], ALU.add, [yv, t2], [yv])
                k.tt('dve', yv[:], yv[:], gv[:], ALU.mult, [yv, gv], [yv])
                k.dma('sp', MIX[r0:r0 + 128, 512:1024].rearrange("(j p) c -> p j c", p=64), yv[:], [yv], MIX, stream=True)
                yield

            RWI = 16

            def drive(gens):
                gens = [g for g in gens if g is not None]
                while gens:
                    for g in list(gens):
                        try:
                            next(g)
                        except StopIteration:
                            gens.remove(g)
            drive([front(0)])
            for it in range(RWI):
                drive([back(it), front(it + 1) if it + 1 < RWI else None])

    def resid_ln(self, banks, ti, H, g_bc, b_bc, zs, out_dram=None, defer=False):
        k = self.k
        z = zs[ti % len(zs)]
        k.dma('sp', z[:], H[ti * 128:(ti + 1) * 128, :], [H], z)
        if isinstance(banks, (list, tuple)):
            for hf in range(2):
                k.stt('dve', z[:, hf * 512:(hf + 1) * 512], z[:, hf * 512:(hf + 1) * 512], DN_ALPHA, banks[hf][:],
                      ALU.mult, ALU.add, [z, banks[hf]], [z])
        else:
            k.stt('dve', z[:], z[:], DN_ALPHA, banks[:], ALU.mult, ALU.add, [z, banks], [z])
        return self.ln_tile(z, g_bc, b_bc, ti, H, out_dram=out_dram, defer=defer)

    def phase_outproj(self, l, MIX, H):
        k = self.k
        wo_t = self.inp['w_out']
        with k.phase():
            wo = self.load_w(wo_t, wo_t[l], 1024, 'wo')
            gcol = k.sb([128, 4], F32, 'gcol')
            k.dma('sp', gcol[:], self.inp['m_norm_g'][l].rearrange("(c p) -> p c", p=128), [self.inp['m_norm_g']], gcol,
                  allow_slow_non_contiguous=True)
            for c in range(4):
                k.ts('dve', wo[:, c, :], wo[:, c, :], gcol[:, c:c + 1], None, ALU.mult, None, [wo, gcol], [wo])
            g = self.bc_load(self.inp['ln1_g'], self.inp['ln1_g'][l], D)
            b = self.bc_load(self.inp['ln1_b'], self.inp['ln1_b'][l], D)
            zs = [k.sb([128, D], F32, 'z') for _ in range(3)]
            pend = None
            ms = [k.sb([128, D], F32, 'mx') for _ in range(2)]
            mTs = [k.sb([128, 8, 128], BF16, 'mT') for _ in range(2)]
            for ti in range(NT):
                m = ms[ti % 2]
                mT = mTs[ti % 2]
                k.dma('sp', m[:], MIX[ti * 128:(ti + 1) * 128, :], [MIX], m)
                for half in range(2):
                    bb = self.bank()
                    for j in range(4):
                        c = half * 4 + j
                        k.tr(bb[:, j * 128:(j + 1) * 128], m[:, c * 128:(c + 1) * 128], self.ident[:], [m, self.ident], [bb],
                             inc=(j == 3))
                    k.copy('act', mT[:, half * 4:(half + 1) * 4, :], bb[:].rearrange("p (c t) -> p c t", c=4), [bb], [mT])
                bks = [self.bank(), self.bank()]
                for hf in range(2):
                    for kc in range(8):
                        k.mm(bks[hf][:], mT[:, kc, :], wo[:, kc, hf * 512:(hf + 1) * 512], [mT, wo], [bks[hf]],
                             start=(kc == 0), stop=(kc == 7), inc=(kc == 7))
                if pend is not None:
                    pend()
                pend = self.resid_ln(bks, ti, H, g, b, zs, defer=True)
            pend()

    def phase_xattn(self, l, H):
        k = self.k
        with k.phase():
            wq = self.load_w(self.inp['x_wq'], self.inp['x_wq'][l], 1024, 'wq')
            wkv = self.load_w(self.inp['x_wkv'], self.inp['x_wkv'][l], 2048, 'wkv')
            wo = self.load_w(self.inp['x_wo'], self.inp['x_wo'][l], 1024, 'xwo')
            g = self.bc_load(self.inp['ln2_g'], self.inp['ln2_g'][l], D)
            b = self.bc_load(self.inp['ln2_b'], self.inp['ln2_b'][l], D)
            zs = [k.sb([128, D], F32, 'z') for _ in range(3)]
            pend = None
            memT = k.sb([128, 8, NM], BF16, 'memT')
            for mt in range(2):
                z = zs[mt]
                k.dma('sp', z[:], self.inp['mem'][mt * 128:(mt + 1) * 128, :], [self.inp['mem']], z)
                for half in range(2):
                    bb = self.bank()
                    for j in range(4):
                        c = half * 4 + j
                        k.tr(bb[:, j * 128:(j + 1) * 128], z[:, c * 128:(c + 1) * 128], self.ident[:], [z, self.ident], [bb],
                             inc=(j == 3))
                    k.copy('act', memT[:, half * 4:(half + 1) * 4, mt * 128:(mt + 1) * 128],
                           bb[:].rearrange("p (c t) -> p c t", c=4), [bb], [memT])
            KT = k.sb([128, 8, NM], BF16, 'KT')
            for c in range(8):
                bb = self.bank()
                for kc in range(8):
                    k.mm(bb[:, 0:NM], wkv[:, kc, c * 128:(c + 1) * 128], memT[:, kc, :], [wkv, memT], [bb],
                         start=(kc == 0), stop=(kc == 7), inc=(kc == 7))
                k.copy('act' if c % 2 else 'dve', KT[:, c, :], bb[:, 0:NM], [bb], [KT])
            Vt = k.sb([128, 2, 1024], BF16, 'Vt')
            for mt in range(2):
                for hf in range(2):
                    bb = self.bank()
                    for kc in range(8):
                        k.mm(bb[:], memT[:, kc, mt * 128:(mt + 1) * 128], wkv[:, kc, 1024 + hf * 512:1024 + (hf + 1) * 512],
                             [wkv, memT], [bb], start=(kc == 0), stop=(kc == 7), inc=(kc == 7))
                    k.copy('act' if hf else 'dve', Vt[:, mt, hf * 512:(hf + 1) * 512], bb[:], [bb], [Vt])
            QTs = [k.sb([128, 8, 128], BF16, 'QT') for _ in range(2)]
            OTs = [k.sb([128, 8, 128], BF16, 'OT') for _ in range(2)]
            Pf = [k.sb([128, NM], F32, 'Pf') for _ in range(4)]
            Pn = [k.sb([128, NM], BF16, 'Pn') for _ in range(4)]
            PTt = [k.sb([128, 2, 128], BF16, 'PTt') for _ in range(4)]
            st = [k.sb([128, 4], F32, 'xst') for _ in range(4)]
            scale = float(256 ** -0.5)

            def qproj(ti):
                QT = QTs[ti % 2]
                tsl = slice(1 + ti * 128, 1 + (ti + 1) * 128)
                for half in range(2):
                    bb = self.bank()
                    for j in range(4):
                        c = half * 4 + j
                        for kc in range(8):
                            k.mm(bb[:, j * 128:(j + 1) * 128], wq[:, kc, c * 128:(c + 1) * 128], self.hT[:, kc, tsl],
                                 [wq, self.hT], [bb], start=(kc == 0), stop=(kc == 7), inc=(kc == 7 and j == 3))
                    k.copy('act' if half else 'dve', QT[:, half * 4:(half + 1) * 4, :],
                           bb[:].rearrange("p (c t) -> p c t", c=4), [bb], [QT])

            qproj(0)
            for ti in range(NT):
                QT, OT = QTs[ti % 2], OTs[ti % 2]
                bss = []
                for h in range(4):
                    bs = self.bank()
                    for dc in range(2):
                        k.mm(bs[:, 0:NM], QT[:, 2 * h + dc, :], KT[:, 2 * h + dc, :], [QT, KT], [bs],
                             start=(dc == 0), stop=(dc == 1), inc=(dc == 1))
                    bss.append(bs)
                if ti + 1 < NT:
                    qproj(ti + 1)
                if pend is not None:
                    pend()
                    pend = None
                for h in range(4):
                    s_, bs = st[h], bss[h]
                    k.op('dve', lambda e, s_=s_, bs=bs: e.reduce_max(s_[:, 0:1], bs[:, 0:NM], AX.X), [bs], [s_])
                    k.ts('dve', s_[:, 1:2], s_[:, 0:1], -scale, None, ALU.mult, None, [s_], [s_])
                    k.act(Pf[h][:], bs[:, 0:NM], AF.Exp, [bs, s_], [Pf[h], s_], bias=s_[:, 1:2], scale=scale,
                          accum_out=s_[:, 2:3])
                for h in range(4):
                    s_ = st[h]
                    k.op('dve', lambda e, s_=s_: e.reciprocal(s_[:, 3:4], s_[:, 2:3]), [s_], [s_])
                    k.ts('dve', Pn[h][:], Pf[h][:], s_[:, 3:4], None, ALU.mult, None, [Pf[h], s_], [Pn[h]])
                bts = []
                for h in range(4):
                    bt_ = self.bank()
                    for mc in range(2):
                        k.mm(bt_[:, mc * 128:(mc + 1) * 128], Pn[h][:, mc * 128:(mc + 1) * 128], self.identb[:],
                             [Pn[h], self.identb], [bt_], inc=(mc == 1))
                    bts.append(bt_)
                for h in range(4):
                    k.copy('act' if h % 2 else 'dve', PTt[h][:], bts[h][:, 0:256].rearrange("p (c t) -> p c t", c=2),
                           [bts[h]], [PTt[h]])
                bos = []
                for h in range(4):
                    bo = self.bank()
                    for dc in range(2):
                        c = 2 * h + dc
                        for mc in range(2):
                            k.mm(bo[:, dc * 128:(dc + 1) * 128], Vt[:, mc, c * 128:(c + 1) * 128], PTt[h][:, mc, :],
                                 [Vt, PTt[h]], [bo], start=(mc == 0), stop=(mc == 1), inc=(mc == 1 and dc == 1))
                    bos.append(bo)
                for h in range(4):
                    k.copy('dve' if h % 2 else 'act', OT[:, 2 * h:2 * h + 2, :],
                           bos[h][:, 0:256].rearrange("p (c t) -> p c t", c=2), [bos[h]], [OT])
                bks = [self.bank(), self.bank()]
                for hf in range(2):
                    for kc in range(8):
                        k.mm(bks[hf][:], OT[:, kc, :], wo[:, kc, hf * 512:(hf + 1) * 512], [OT, wo], [bks[hf]],
                             start=(kc == 0), stop=(kc == 7), inc=(kc == 7))
                pend = self.resid_ln(bks, ti, H, g, b, zs, defer=True)
            pend()

    def phase_moe(self, l, H, final_out=None):
        k = self.k
        w1_t, w2_t = self.inp['moe_w1'], self.inp['moe_w2']
        NEX = NE
        DMAONLY = 0
        with k.phase():
            G = k.sb([128, NT, NE], F32, 'G')
            acc = k.sb([128, NT, D], F32, 'acc')
            with k.phase():
                wr = k.sb([128, 8, NE], F32, 'wr')
                k.dma('sp', wr[:], self.inp['moe_wr'][l].rearrange("(c p) n -> p c n", p=128), [self.inp['moe_wr']], wr)
                br = self.bc_load(self.inp['moe_br'], self.inp['moe_br'][l], NE, 'br')
                b2 = k.sb([NE, D], F32, 'b2')
                k.dma('sp', b2[:], self.inp['moe_b2'][l], [self.inp['moe_b2']], b2)
                hts = [k.sb([128, D], F32, 'ht') for _ in range(2)]
                h32s = [k.sb([128, 8, 128], F32, 'h32') for _ in range(2)]
                lgs = [k.sb([128, NE], F32, 'lg') for _ in range(2)]
                m8s = [k.sb([128, 16], F32, 'm8') for _ in range(2)]
                GTs = [k.sb([NE, 128], F32, 'GT') for _ in range(2)]
                for ti in range(NT):
                    ht, h32, lg, m8, GT = hts[ti % 2], h32s[ti % 2], lgs[ti % 2], m8s[ti % 2], GTs[ti % 2]
                    k.dma('sp', ht[:], H[ti * 128:(ti + 1) * 128, :], [H], ht)
                    for half in range(2):
                        bb = self.bank()
                        for j in range(4):
                            c = half * 4 + j
                            k.tr(bb[:, j * 128:(j + 1) * 128], ht[:, c * 128:(c + 1) * 128], self.ident[:], [ht, self.ident],
                                 [bb], inc=(j == 3))
                        k.copy('act', h32[:, half * 4:(half + 1) * 4, :], bb[:].rearrange("p (c t) -> p c t", c=4), [bb], [h32])
                    bl = self.bank()
                    for kc in range(8):
                        k.mm(bl[:, 0:NE], h32[:, kc, :], wr[:, kc, :], [h32, wr], [bl], start=(kc == 0), stop=(kc == 7),
                             inc=(kc == 7))
                    k.tt('dve', lg[:], bl[:, 0:NE], br[:], ALU.add, [bl, br], [lg])
                    k.op('dve', lambda e, m8=m8, lg=lg: e.max(m8[:, 0:8], lg[:]), [lg], [m8])
                    k.ts('dve', m8[:, 8:9], m8[:, 0:1], -1.0, None, ALU.mult, None, [m8], [m8])
                    g_ = G[:, ti, :]
                    k.act(g_, lg[:], AF.Exp, [lg, m8], [G], bias=m8[:, 8:9], scale=1.0)
                    k.ts('dve', lg[:], lg[:], m8[:, 3:4], None, ALU.is_ge, None, [lg, m8], [lg])
                    k.tt('dve', g_, g_, lg[:], ALU.mult, [G, lg], [G])
                    k.op('dve', lambda e, m8=m8, g_=g_: e.reduce_sum(m8[:, 9:10], g_, AX.X), [G], [m8])
                    k.op('dve', lambda e, m8=m8: e.reciprocal(m8[:, 10:11], m8[:, 9:10]), [m8], [m8])
                    k.ts('dve', g_, g_, m8[:, 10:11], None, ALU.mult, None, [G, m8], [G])
                    bg = self.bank()
                    k.tr(bg[0:NE, 0:128], g_, self.ident[:], [G, self.ident], [bg])
                    k.copy('act', GT[:], bg[0:NE, 0:128], [bg], [GT])
                    for hf in range(2):
                        bb = self.bank()
                        k.mm(bb[:], GT[:], b2[:, hf * 512:(hf + 1) * 512], [GT, b2], [bb])
                        k.copy('act' if hf else 'dve', acc[:, ti, hf * 512:(hf + 1) * 512], bb[:], [bb], [acc])
            with k.phase():
                b1a = k.sb([128, NE, 16], F32, 'b1a')
                for e in range(NE):
                    k.dma('sp', b1a[:, e, :], self.inp['moe_b1'][l, e].rearrange("(c p) -> p c", p=128),
                          [self.inp['moe_b1']], b1a, allow_slow_non_contiguous=True)
                actT = k.sb([128, 8, S], BF16, 'actT')
                w1r = [k.sb([128, 8, 2, 128], BF16, 'w1r') for _ in range(4)]
                w2b = k.sb([128, 8, D], BF16, 'w2b')
                g0s = [k.sb([128, 512], F32, 'g0') for _ in range(2)]
                sgs = [k.sb([128, 512], F32, 'sg') for _ in range(2)]
                u0s = [k.sb([128, 512], F32, 'u0') for _ in range(2)]
                pi = 0
                ei = 0
                for e in range(NEX):
                    for p in range(8):
                        w1 = w1r[pi % 4]
                        pi += 1
                        for gu in range(2):
                            c0 = gu * DFF + p * 128
                            k.dma('pool', w1[:, :, gu, :], w1_t[l, e, :, c0:c0 + 128].rearrange("(c p) n -> p c n", p=128),
                                  [w1_t], w1)
                        if p == 0:
                            k.dma('pool', w2b[:], w2_t[l, e].rearrange("(c p) n -> p c n", p=128), [w2_t], w2b)
                        for tb in range(4 if not DMAONLY else 0):
                            tsl = slice(1 + tb * 512, 1 + (tb + 1) * 512)
                            bgp, bup = self.bank(), self.bank()
                            for gu, bb in ((0, bgp), (1, bup)):
                                for kc in range(8):
                                    k.mm(bb[:], w1[:, kc, gu, :], self.hT[:, kc, tsl], [w1, self.hT], [bb],
                                         start=(kc == 0), stop=(kc == 7), inc=(kc == 7))
                            g0, sg, u0 = g0s[ei % 2], sgs[ei % 2], u0s[ei % 2]
                            ei += 1
                            k.act(g0[:], bgp[:], AF.Identity, [bgp, b1a], [g0], bias=b1a[:, e, p:p + 1], scale=1.0)
                            k.ts('dve', g0[:], g0[:], 7.0, None, ALU.min, None, [g0], [g0])
                            k.act(sg[:], g0[:], AF.Sigmoid, [g0], [sg], scale=1.702)
                            k.tt('dve', sg[:], sg[:], g0[:], ALU.mult, [sg, g0], [sg])
                            k.act(u0[:], bup[:], AF.Identity, [bup, b1a], [u0], bias=b1a[:, e, 8 + p:9 + p], scale=1.0)
                            k.ts('dve', u0[:], u0[:], 7.0, -7.0, ALU.min, ALU.max, [u0], [u0])
                            k.stt('dve', actT[:, p, tb * 512:(tb + 1) * 512], u0[:], 1.0, sg[:], ALU.add, ALU.mult,
                                  [u0, sg], [actT])
                    for ti in range(NT if not DMAONLY else 0):
                        bks = [self.bank(), self.bank()]
                        for hf in range(2):
                            for fc in range(8):
                                k.mm(bks[hf][:], actT[:, fc, ti * 128:(ti + 1) * 128], w2b[:, fc, hf * 512:(hf + 1) * 512],
                                     [actT, w2b], [bks[hf]], start=(fc == 0), stop=(fc == 7), inc=(fc == 7))
                        for hf in range(2):
                            k.stt('dve', acc[:, ti, hf * 512:(hf + 1) * 512], bks[hf][:], G[:, ti, e:e + 1],
                                  acc[:, ti, hf * 512:(hf + 1) * 512], ALU.mult, ALU.add, [bks[hf], G, acc], [acc])
            with k.phase():
                g = self.bc_load(self.inp['ln3_g'], self.inp['ln3_g'][l], D)
                b = self.bc_load(self.inp['ln3_b'], self.inp['ln3_b'][l], D)
                zs = [k.sb([128, D], F32, 'z') for _ in range(2)]
                for ti in range(NT):
                    z = zs[ti % 2]
                    k.dma('sp', z[:], H[ti * 128:(ti + 1) * 128, :], [H], z)
                    k.stt('dve', z[:], z[:], DN_ALPHA, acc[:, ti, :], ALU.mult, ALU.add, [z, acc], [z])
                    self.ln_tile(z, g, b, ti, H, out_dram=final_out)

    def phase_moe_sparse(self, l, H, YB, RT, final_out=None):
        k = self.k
        I32 = mybir.dt.int32
        BLK = MOE_BLK
        NTB = BLK // 128
        NB = MOE_NB
        NR = NB * BLK
        BIG = 4.0e6
        w1v = self.inp['moe_w1'][:].rearrange("l e d f -> (l e d) f")
        w2v = self.inp['moe_w2'][:].rearrange("l e f d -> (l e f) d")
        b1v = self.inp['moe_b1'][:].rearrange("l e (c f) -> (l e c) f", f=128)
        with k.phase():
            G = k.sb([128, NT, NE], F32, 'G')
            dsel = k.sb([128, NT, 4], I32, 'dsel')
            gsel = k.sb([128, NT, 4], F32, 'gsel')
            IW = k.sb([128, NB, 8], I32, 'IW')
            IB = k.sb([16, NB], I32, 'IB')
            b2 = k.sb([NE, D], F32, 'b2')
            k.dma('sp', b2[:], self.inp['moe_b2'][l], [self.inp['moe_b2']], b2)
            with k.phase():
                wr = k.sb([128, 8, NE], F32, 'wr')
                k.dma('sp', wr[:], self.inp['moe_wr'][l].rearrange("(c p) n -> p c n", p=128), [self.inp['moe_wr']], wr)
                br = self.bc_load(self.inp['moe_br'], self.inp['moe_br'][l], NE, 'br')
                Mk = k.sb([128, NT, NE], F32, 'Mk')
                rank = k.sb([128, NT, NE], F32, 'rank')
                hts = [k.sb([128, D], F32, 'ht') for _ in range(2)]
                h32s = [k.sb([128, 8, 128], F32, 'h32') for _ in range(2)]
                lgs = [k.sb([128, NE], F32, 'lg') for _ in range(2)]
                m8s = [k.sb([128, 16], F32, 'm8') for _ in range(2)]
                for ti in range(NT):
                    ht, h32, lg, m8 = hts[ti % 2], h32s[ti % 2], lgs[ti % 2], m8s[ti % 2]
                    k.dma('sp', ht[:], H[ti * 128:(ti + 1) * 128, :], [H], ht)
                    for half in range(2):
                        bb = self.bank()
                        for j in range(4):
                            c = half * 4 + j
                            k.tr(bb[:, j * 128:(j + 1) * 128], ht[:, c * 128:(c + 1) * 128], self.ident[:], [ht, self.ident],
                                 [bb], inc=(j == 3))
                        k.copy('act', h32[:, half * 4:(half + 1) * 4, :], bb[:].rearrange("p (c t) -> p c t", c=4), [bb], [h32])
                    bl = self.bank()
                    for kc in range(8):
                        k.mm(bl[:, 0:NE], h32[:, kc, :], wr[:, kc, :], [h32, wr], [bl], start=(kc == 0), stop=(kc == 7),
                             inc=(kc == 7))
                    k.tt('dve', lg[:], bl[:, 0:NE], br[:], ALU.add, [bl, br], [lg])
                    k.op('dve', lambda e, m8=m8, lg=lg: e.max(m8[:, 0:8], lg[:]), [lg], [m8])
                    k.ts('dve', m8[:, 8:9], m8[:, 0:1], -1.0, None, ALU.mult, None, [m8], [m8])
                    g_ = G[:, ti, :]
                    k.act(g_, lg[:], AF.Exp, [lg, m8], [G], bias=m8[:, 8:9], scale=1.0)
                    k.ts('dve', Mk[:, ti, :], lg[:], m8[:, 3:4], None, ALU.is_ge, None, [lg, m8], [Mk])
                    k.tt('dve', g_, g_, Mk[:, ti, :], ALU.mult, [G, Mk], [G])
                    k.op('dve', lambda e, m8=m8, g_=g_: e.reduce_sum(m8[:, 9:10], g_, AX.X), [G], [m8])
                    k.op('dve', lambda e, m8=m8: e.reciprocal(m8[:, 10:11], m8[:, 9:10]), [m8], [m8])
                    k.ts('dve', g_, g_, m8[:, 10:11], None, ALU.mult, None, [G, m8], [G])
                ones = k.sb([128, 128], F32, 'ones')
                k.memset('pool', ones, ones[:], 1.0)
                lst = k.sb([128, 128], F32, 'lst')
                k.memset('pool', lst, lst[:], 1.0)
                k.op('pool', lambda e: e.affine_select(lst[:], lst[:], [[1, 128]], ALU.is_gt, 0.0, base=0,
                                                       channel_multiplier=-1), [lst], [lst])
                for ti in range(NT):
                    bb = self.bank()
                    for tj in range(ti):
                        k.mm(bb[:, 0:NE], ones[:], Mk[:, tj, :], [ones, Mk], [bb], start=(tj == 0), stop=False, inc=False)
                    k.mm(bb[:, 0:NE], lst[:], Mk[:, ti, :], [lst, Mk], [bb], start=(ti == 0), stop=True)
                    k.copy('act' if ti % 2 else 'dve', rank[:, ti, :], bb[:, 0:NE], [bb], [rank])
                bb = self.bank()
                for tj in range(NT):
                    k.mm(bb[:, 0:NE], ones[:], Mk[:, tj, :], [ones, Mk], [bb], start=(tj == 0), stop=(tj == NT - 1),
                         inc=(tj == NT - 1))
                cnt = k.sb([128, NE], F32, 'cnt')
                k.copy('dve', cnt[:], bb[:, 0:NE], [bb], [cnt])
                thr = k.sb([128, 16], F32, 'thr')
                k.op('pool', lambda e: e.iota(thr[:], [[BLK, 16]], base=0, channel_multiplier=0,
                                              allow_small_or_imprecise_dtypes=True), [], [thr])
                cmp_ = k.sb([128, NE, 16], F32, 'cmp')
                k.tt('dve', cmp_[:], cnt[:].unsqueeze(2).to_broadcast([128, NE, 16]),
                     thr[:].unsqueeze(1).to_broadcast([128, NE, 16]), ALU.is_gt, [cnt, thr], [cmp_])
                pad = k.sb([128, 4, NE], F32, 'pad')
                k.op('dve', lambda e: e.reduce_sum(pad[:, 0, :], cmp_[:], AX.X), [cmp_], [pad])
                k.ts('dve', pad[:, 0, :], pad[:, 0, :], float(BLK), None, ALU.mult, None, [pad], [pad])
                k.op('dve', lambda e: e.tensor_tensor_scan(pad[:, 1, :], ones[:, 0:NE], pad[:, 0, :], 0.0, ALU.mult, ALU.add),
                     [ones, pad], [pad])
                k.tt('dve', pad[:, 2, :], pad[:, 1, :], pad[:, 0, :], ALU.subtract, [pad], [pad])
                bth = k.sb([128, NB], F32, 'bth')
                k.op('pool', lambda e: e.iota(bth[:], [[BLK, NB]], base=0, channel_multiplier=0,
                                              allow_small_or_imprecise_dtypes=True), [], [bth])
                cmpb = k.sb([128, NB, NE], F32, 'cmpb')
                k.tt('dve', cmpb[:], pad[:, 1, :].unsqueeze(1).to_broadcast([128, NB, NE]),
                     bth[:].unsqueeze(2).to_broadcast([128, NB, NE]), ALU.is_le, [pad, bth], [cmpb])
                be = k.sb([128, 4, NB], F32, 'be')
                k.op('dve', lambda e: e.reduce_sum(be[:, 0, :], cmpb[:], AX.X), [cmpb], [be])
                k.ts('dve', be[:, 1, :], be[:, 0, :], float(NE) - 0.5, BIG, ALU.is_gt, ALU.mult, [be], [be])
                k.ts('dve', be[:, 0, :], be[:, 0, :], float(NE - 1), None, ALU.min, None, [be], [be])
                iw0 = k.sb([128, 8], F32, 'iw0')
                k.op('pool', lambda e: e.iota(iw0[:], [[128, 8]], base=l * NE * 1024, channel_multiplier=1,
                                              allow_small_or_imprecise_dtypes=True), [], [iw0])
                k.ts('dve', be[:, 2, :], be[:, 0, :], 1024.0, None, ALU.mult, None, [be], [be])
                k.tt('dve', be[:, 2, :], be[:, 2, :], be[:, 1, :], ALU.add, [be], [be])
                iwf = k.sb([128, NB, 8], F32, 'iwf')
                k.tt('dve', iwf[:], be[:, 2, :].unsqueeze(2).to_broadcast([128, NB, 8]),
                     iw0[:].unsqueeze(1).to_broadcast([128, NB, 8]), ALU.add, [be, iw0], [iwf])
                k.copy('dve', IW[:], iwf[:], [iwf], [IW])
                ib0 = k.sb([16, 1], F32, 'ib0')
                k.op('pool', lambda e: e.iota(ib0[:], [[0, 1]], base=l * NE * 16, channel_multiplier=1,
                                              allow_small_or_imprecise_dtypes=True), [], [ib0])
                ibf = k.sb([16, NB], F32, 'ibf')
                k.ts('dve', ibf[:], be[0:16, 0, :], 16.0, ib0[:, 0:1], ALU.mult, ALU.add, [be, ib0], [ibf])
                k.tt('dve', ibf[:], ibf[:], be[0:16, 1, :], ALU.add, [ibf, be], [ibf])
                k.copy('dve', IB[:], ibf[:], [ibf], [IB])
                zt = k.sb([128, (NR // 128) * 16], I32, 'zt')
                k.memset('pool', zt, zt[:], 0)
                k.dma('sp', RT[:, :].rearrange("(p n) c -> p (n c)", p=128), zt[:], [zt], RT)
                RTS = T(RT.t)
                Dm = k.sb([128, NE], F32, 'Dm')
                d8 = k.sb([128, 8], F32, 'd8')
                eq = k.sb([128, NE], F32, 'eq')
                tok = k.sb([128, 16], I32, 'tok')
                for ti in range(NT):
                    k.tt('dve', Dm[:], rank[:, ti, :], pad[:, 2, :], ALU.add, [rank, pad], [Dm])
                    k.ts('dve', Dm[:], Dm[:], 1.0, None, ALU.add, None, [Dm], [Dm])
                    k.tt('dve', Dm[:], Dm[:], Mk[:, ti, :], ALU.mult, [Dm, Mk], [Dm])
                    k.ts('dve', Dm[:], Dm[:], -1.0, None, ALU.add, None, [Dm], [Dm])
                    k.op('dve', lambda e: e.max(d8[:], Dm[:]), [Dm], [d8])
                    k.copy('dve', dsel[:, ti, :], d8[:, 0:4], [d8], [dsel])
                    for kk_ in range(4):
                        k.ts('dve', eq[:], Dm[:], d8[:, kk_:kk_ + 1], None, ALU.is_equal, None, [Dm, d8], [eq])
                        k.tt('dve', eq[:], eq[:], G[:, ti, :], ALU.mult, [eq, G], [eq])
                        k.op('dve', lambda e, kk_=kk_: e.reduce_sum(gsel[:, ti, kk_:kk_ + 1], eq[:], AX.X), [eq], [gsel])
                    k.op('pool', lambda e, ti=ti: e.iota(tok[:], [[0, 16]], base=ti * 128, channel_multiplier=1), [], [tok])
                    for kk_ in range(4):
                        k.idma(RTS, RT[:, :], tok, tok[:], dsel, dsel[:, ti, kk_:kk_ + 1], scatter=True, stream=True, extra_reads=[RT])
            with k.phase():
                NW = 16
                w1p = [k.sb([128, 2 * DFF], BF16, 'w1p') for _ in range(NW)]
                w2p = [k.sb([128, D], BF16, 'w2p') for _ in range(NW)]
                xia = k.sb([128, NB, NTB], I32, 'xia')
                k.dma('sp', xia[:], RT[:, 0:1].rearrange("(b i p) c -> p b (i c)", p=128, i=NTB), [RT, RTS], xia,
                      allow_slow_non_contiguous=True)
                xgs = [k.sb([128, D], F32, 'xg') for _ in range(4)]
                xTs = [k.sb([128, 8, BLK], BF16, 'xT') for _ in range(2)]
                aTs = [k.sb([128, 8, BLK], BF16, 'aT') for _ in range(2)]
                b1gs = [k.sb([16, 128], F32, 'b1g') for _ in range(2)]
                b1bs = [k.sb([128, 16], F32, 'b1b') for _ in range(2)]
                ysb = [k.sb([128, D], F32, 'ysb') for _ in range(2)]
                g0s = [k.sb([128, BLK], F32, 'g0') for _ in range(2)]
                sgs = [k.sb([128, BLK], F32, 'sg') for _ in range(2)]
                u0s = [k.sb([128, BLK], F32, 'u0') for _ in range(2)]
                NBX = NB
                wi = 0
                ei = 0
                xgi = 0
                yi = 0
                for b in range(NBX):
                    xT, aT, b1g, b1b = xTs[b % 2], aTs[b % 2], b1gs[b % 2], b1bs[b % 2]
                    xg4 = []
                    for i in range(NTB):
                        xg = xgs[xgi % 4]
                        xgi += 1
                        k.idma(xg, xg[:], H, H[:, :], xia, xia[:, b, i:i + 1])
                        xg4.append(xg)
                    k.idma(b1g, b1g[:], self.inp['moe_b1'], b1v, IB, IB[:, b:b + 1], bounds=self.reg_b)
                    w1k, w2k = [], []
                    for kc in range(8):
                        w = w1p[wi % NW]
                        k.idma(w, w[:], self.inp['moe_w1'], w1v, IW, IW[:, b, kc:kc + 1], bounds=self.reg_w)
                        w1k.append(w)
                        wi += 1
                    wi -= 8
                    for kc in range(8):
                        w = w2p[wi % NW]
                        k.idma(w, w[:], self.inp['moe_w2'], w2v, IW, IW[:, b, kc:kc + 1], bounds=self.reg_w)
                        w2k.append(w)
                        wi += 1
                    for i in range(NTB):
                        xg = xg4[i]
                        for half in range(2):
                            bb = self.bank()
                            for j in range(4):
                                c = half * 4 + j
                                k.tr(bb[:, j * 128:(j + 1) * 128], xg[:, c * 128:(c + 1) * 128], self.ident[:],
                                     [xg, self.ident], [bb], inc=(j == 3))
                            k.copy('act' if half else 'dve', xT[:, half * 4:(half + 1) * 4, i * 128:(i + 1) * 128],
                                   bb[:].rearrange("p (c t) -> p c t", c=4), [bb], [xT])
                    bb = self.bank()
                    k.tr(bb[:, 0:16], b1g[:], self.ident[0:16, 0:16], [b1g, self.ident], [bb])
                    k.copy('dve', b1b[:], bb[:, 0:16], [bb], [b1b])
                    for p in range(8):
                        bgp, bup = self.bank(), self.bank()
                        for gu, bb in ((0, bgp), (1, bup)):
                            c0 = gu * DFF + p * 128
                            for kc in range(8):
                                k.mm(bb[:, 0:BLK], w1k[kc][:, c0:c0 + 128], xT[:, kc, :], [w1k[kc], xT], [bb],
                                     start=(kc == 0), stop=(kc == 7), inc=(kc == 7))
                        g0, sg, u0 = g0s[ei % 2], sgs[ei % 2], u0s[ei % 2]
                        ei += 1
                        k.act(g0[:], bgp[:, 0:BLK], AF.Identity, [bgp, b1b], [g0], bias=b1b[:, p:p + 1], scale=1.0)
                        k.ts('dve', g0[:], g0[:], 7.0, None, ALU.min, None, [g0], [g0])
                        k.act(sg[:], g0[:], AF.Sigmoid, [g0], [sg], scale=1.702)
                        k.tt('dve', sg[:], sg[:], g0[:], ALU.mult, [sg, g0], [sg])
                        k.act(u0[:], bup[:, 0:BLK], AF.Identity, [bup, b1b], [u0], bias=b1b[:, 8 + p:9 + p], scale=1.0)
                        k.ts('dve', u0[:], u0[:], 7.0, -7.0, ALU.min, ALU.max, [u0], [u0])
                        k.stt('dve', aT[:, p, :], u0[:], 1.0, sg[:], ALU.add, ALU.mult, [u0, sg], [aT])
                    for i in range(NTB):
                        bks = [self.bank(), self.bank()]
                        for hf in range(2):
                            for fc in range(8):
                                k.mm(bks[hf][:], aT[:, fc, i * 128:(i + 1) * 128], w2k[fc][:, hf * 512:(hf + 1) * 512],
                                     [aT, w2k[fc]], [bks[hf]], start=(fc == 0), stop=(fc == 7), inc=(fc == 7))
                        y = ysb[yi % 2]
                        yi += 1
                        k.copy('act', y[:, 0:512], bks[0][:], [bks[0]], [y])
                        k.copy('act', y[:, 512:1024], bks[1][:], [bks[1]], [y])
                        k.dma('sp', YB[b * BLK + i * 128:b * BLK + (i + 1) * 128, :], y[:], [y], YB, stream=True)
            with k.phase():
                g = self.bc_load(self.inp['ln3_g'], self.inp['ln3_g'][l], D)
                bt_ = self.bc_load(self.inp['ln3_b'], self.inp['ln3_b'][l], D)
                zs = [k.sb([128, D], F32, 'z') for _ in range(2)]
                ygs = [k.sb([128, D], F32, 'yg') for _ in range(8)]
                GTs = [k.sb([NE, 128], F32, 'GT') for _ in range(2)]
                gi = 0
                for ti in range(NT):
                    z = zs[ti % 2]
                    GT = GTs[ti % 2]
                    k.dma('sp', z[:], H[ti * 128:(ti + 1) * 128, :], [H], z)
                    bg = self.bank()
                    k.tr(bg[0:NE, 0:128], G[:, ti, :], self.ident[:], [G, self.ident], [bg])
                    k.copy('act', GT[:], bg[0:NE, 0:128], [bg], [GT])
                    for hf in range(2):
                        bb = self.bank()
                        k.mm(bb[:], GT[:], b2[:, hf * 512:(hf + 1) * 512], [GT, b2], [bb])
                        k.stt('dve', z[:, hf * 512:(hf + 1) * 512], z[:, hf * 512:(hf + 1) * 512], DN_ALPHA, bb[:],
                              ALU.mult, ALU.add, [z, bb], [z])
                    for kk_ in range(4):
                        yg = ygs[gi % 8]
                        gi += 1
                        k.idma(yg, yg[:], YB, YB[:, :], dsel, dsel[:, ti, kk_:kk_ + 1])
                        k.stt('dve', z[:], yg[:], gsel[:, ti, kk_:kk_ + 1], z[:], ALU.mult, ALU.add, [yg, gsel, z], [z])
                    self.ln_tile(z, g, bt_, ti, H, out_dram=final_out)

    def build(self):
        k = self.k
        self.consts()
        self.alloc_hT()
        H = self.scratch('H', [S, D])
        self.phase_ln0(H)
        if self.upto <= 0:
            self.finish(H)
            return self.nc
        for l in range(DEPTH):
            IF = self.scratch(f'IF{l}', [8, S])
            PT = self.scratch(f'PT{l}', [S, 1024 + R_COLS])
            MIX = self.scratch(f'MIX{l}', [S, 1024])
            with k.phase():
                qkT = k.sb([128, 8, S], BF16, 'qkT')
                self.phase_mixproj(l, qkT, IF, PT)
                if self.upto == 1 + 10 * l:
                    qd = self.scratch('QK', [1024, S], BF16)
                    for c in range(8):
                        k.dma('sp', qd[c * 128:(c + 1) * 128, :], qkT[:, c, :], [qkT], qd)
                    self.finish(H)
                    return self.nc
                self.phase_mlstm(l, qkT, IF, PT, MIX)
                if self.upto == 2 + 10 * l:
                    self.finish(H)
                    return self.nc
            self.free_hT()
            self.phase_rwkv(l, PT, MIX)
            self.alloc_hT()
            if self.upto == 3 + 10 * l:
                self.finish(H)
                return self.nc
            self.phase_outproj(l, MIX, H)
            if self.upto == 4 + 10 * l:
                self.finish(H)
                return self.nc
            self.phase_xattn(l, H)
            if self.upto == 5 + 10 * l:
                self.finish(H)
                return self.nc
            last = (l == DEPTH - 1)
            if MOE_SPARSE:
                if l == 0:
                    self.YB = self.scratch('YB', [MOE_NB * MOE_BLK, D])
                    self.RT = self.scratch('RT', [MOE_NB * MOE_BLK, 16], mybir.dt.int32)
                self.phase_moe_sparse(l, H, self.YB, self.RT, final_out=(self.out if last else None))
            else:
                self.phase_moe(l, H, final_out=(self.out if last else None))
            if self.upto == 6 + 10 * l and not last:
                self.finish(H)
                return self.nc
        k.barrier()
        return self.nc

    def finish(self, H):
        k = self.k
        with k.phase():
            z = k.sb([128, D], F32, 'fz')
            for ti in range(NT):
                k.dma('sp', z[:], H[ti * 128:(ti + 1) * 128, :], [H], z)
                k.dma('sp', self.out[ti * 128:(ti + 1) * 128, :], z[:], [z], self.out)
        k.barrier()


def make_inputs(inputs, b):
    m = {'x': np.ascontiguousarray(inputs['x'][b]), 'mem': np.ascontiguousarray(inputs['mem'][b])}
    for n, _ in WEIGHT_SPECS:
        m[n] = np.ascontiguousarray(inputs[n])
    return m


def kernel(**inputs):
    inputs = {k_: np.asarray(v) for k_, v in inputs.items()}
    prog = Prog()
    nc = prog.build()
    in_maps = [make_inputs(inputs, b) for b in range(8)]
    res = run_bass_kernel_spmd(nc, in_maps, core_ids=list(range(8)))
    return np.stack([np.asarray(r['out']) for r in res.results], axis=0).astype(np.float32)
```

```python
import numpy as np
from contextlib import ExitStack, contextmanager
import concourse.bass as bass
import concourse.mybir as mybir
from concourse.bass_utils import run_bass_kernel_spmd

F32 = mybir.dt.float32
BF16 = mybir.dt.bfloat16
AF = mybir.ActivationFunctionType
ALU = mybir.AluOpType
AX = mybir.AxisListType

S = 2048
D = 1024
NT = S // 128
DEPTH = 2
M_W = 512
M_H = 4
M_HD = 128
R_W = 512
R_H = 8
R_HD = 64
IN_COLS = 3848
M_COLS = 2056
R_COLS = 1792
NM = 256
NE = 32
DFF = 1024
DN_ALPHA = float((2 * DEPTH) ** 0.25)
LN_EPS = 1e-5
MOE_SPARSE = True
MOE_BLK = 384
MOE_NB = (S * 4 + MOE_BLK - 1) // MOE_BLK + NE

WEIGHT_SPECS = [
    ('ln0_g', (1024,)), ('ln0_b', (1024,)), ('w_in', (2, 1024, 3848)), ('m_conv_w', (2, 4, 1024)),
    ('m_conv_b', (2, 1024)), ('m_ig_b', (2, 4)), ('m_fg_b', (2, 4)), ('m_norm_g', (2, 512)),
    ('r_mu', (2, 1792)), ('r_w0', (2, 512)), ('r_w2', (2, 64, 512)), ('r_a0', (2, 512)),
    ('r_a2', (2, 64, 512)), ('r_g2', (2, 128, 512)), ('r_kk', (2, 512)), ('r_ka', (2, 512)),
    ('r_rk', (2, 512)), ('r_gn_g', (2, 512)), ('r_gn_b', (2, 512)), ('w_out', (2, 1024, 1024)),
    ('ln1_g', (2, 1024)), ('ln1_b', (2, 1024)), ('x_wq', (2, 1024, 1024)), ('x_wkv', (2, 1024, 2048)),
    ('x_wo', (2, 1024, 1024)), ('ln2_g', (2, 1024)), ('ln2_b', (2, 1024)), ('moe_wr', (2, 1024, 32)),
    ('moe_br', (2, 32)), ('moe_w1', (2, 32, 1024, 2048)), ('moe_b1', (2, 32, 2048)),
    ('moe_w2', (2, 32, 1024, 1024)), ('moe_b2', (2, 32, 1024)), ('ln3_g', (2, 1024)), ('ln3_b', (2, 1024)),
]


class T:
    _n = 0

    def __init__(self, t, key=None, excl=False):
        self.t = t
        self.excl = excl
        T._n += 1
        self.key = key if key is not None else ('t', T._n)

    def __getitem__(self, idx):
        return self.t[idx]


class K:
    def __init__(self, nc):
        self.nc = nc
        self.es = ExitStack()
        self.engs = {'pe': nc.tensor, 'act': nc.scalar, 'dve': nc.vector, 'pool': nc.gpsimd, 'sp': nc.sync}
        self.sems = {n: self.es.enter_context(nc.semaphore('s_' + n)) for n in self.engs}
        self.cnt = {n: 0 for n in self.engs}
        self.seen = {n: {} for n in self.engs}
        self.res = {}
        self.dsem = {}
        self.ssem = {}
        self.sempool = []
        self.allsems = {('e', n): (self.sems[n], 0) for n in self.engs}
        self.stack = [self.es]
        self.uid = 0

    @contextmanager
    def phase(self):
        es = ExitStack()
        self.stack.append(es)
        try:
            yield
        finally:
            self.barrier()
            self.stack.pop()
            es.close()

    def name(self, p):
        self.uid += 1
        return f"{p}_{self.uid}"

    def sb(self, shape, dt=F32, name='sb'):
        return T(self.stack[-1].enter_context(self.nc.sbuf_tensor(self.name(name), list(shape), dt)))

    def ps(self, shape, dt=F32, name='ps'):
        return T(self.stack[-1].enter_context(self.nc.psum_tensor(self.name(name), list(shape), dt)), excl=True)

    def dram(self, name, shape, dt=F32, kind='Internal'):
        return T(self.nc.dram_tensor(name, list(shape), dt, kind=kind).ap())

    def _deps(self, reads, writes):
        deps = []
        for r in reads:
            e = self.res.get(r.key)
            if e:
                if e[0]:
                    deps.append(e[0])
                deps.extend(e[2].values())
        for w in writes:
            e = self.res.get(w.key)
            if e:
                if e[0]:
                    deps.append(e[0])
                deps.extend(e[1].values())
                deps.extend(e[2].values())
        return deps

    def _wait(self, en, deps):
        best = {}
        for (sk, sh, v) in deps:
            if en == 'pe' and sk == ('e', 'pe'):
                continue
            if self.seen[en].get(sk, 0) >= v:
                continue
            if sk not in best or best[sk][1] < v:
                best[sk] = (sh, v)
        for sk, (sh, v) in best.items():
            self.engs[en].wait_ge(sh, v)
            self.seen[en][sk] = v

    def _record(self, ev, reads, writes, stream=False):
        for r in reads:
            e = self.res.setdefault(r.key, [None, {}, {}])
            old = e[1].get(ev[0])
            if old is None or old[2] < ev[2]:
                e[1][ev[0]] = ev
        for w in writes:
            if stream:
                self.res.setdefault(w.key, [None, {}, {}])[2][ev[0]] = ev
            else:
                self.res[w.key] = [ev, {}, {}]

    NSTREAM = 4

    def _dsem(self, key, stream):
        def new():
            if self.sempool:
                return self.sempool.pop()
            return [self.es.enter_context(self.nc.semaphore(self.name('sd'))), 0]
        if not stream:
            if key not in self.dsem:
                self.dsem[key] = new()
            return self.dsem[key], None
        st = self.ssem.setdefault(key, [[], 0])
        if len(st[0]) < self.NSTREAM:
            st[0].append(new())
        ds = st[0][st[1] % len(st[0])] if len(st[0]) == self.NSTREAM else st[0][-1]
        st[1] += 1
        prev = (('d', id(ds)), ds[0], ds[1]) if ds[1] > 0 else None
        return ds, prev

    def op(self, en, fn, reads=(), writes=(), inc=True):
        writes = list(writes) + [r for r in reads if r.excl]
        reads = [r for r in reads if not r.excl]
        self._wait(en, self._deps(reads, writes))
        ins = fn(self.engs[en])
        sk = ('e', en)
        ev = (sk, self.sems[en], self.cnt[en] + 1)
        if inc:
            self.cnt[en] += 1
            ins.then_inc(self.sems[en], 1)
            self.allsems[sk] = (self.sems[en], self.cnt[en])
        self._record(ev, reads, writes)
        return ins

    def dma(self, en, out, in_, reads, write, stream=False, **kw):
        ds, prev = self._dsem(write.key, stream)
        deps = self._deps(reads, [] if stream else [write])
        if prev is not None:
            deps.append(prev)
        self._wait(en, deps)
        ds[1] += 16
        self.engs[en].dma_start(out=out, in_=in_, **kw).then_inc(ds[0], 16)
        sk = ('d', id(ds))
        ev = (sk, ds[0], ds[1])
        self.allsems[sk] = (ds[0], ds[1])
        self._record(ev, reads, [write], stream=stream)

    def idma(self, out_t, out_ap, in_t, in_ap, idx_t, idx_ap, scatter=False, bounds=None, stream=False, extra_reads=()):
        en = 'pool'
        reads = [in_t, idx_t] + list(extra_reads)
        ds, prev = self._dsem(out_t.key, stream)
        deps = self._deps(reads, [] if stream else [out_t])
        if prev is not None:
            deps.append(prev)
        self._wait(en, deps)
        ds[1] += 16
        off = bass.IndirectOffsetOnAxis(ap=idx_ap, axis=0)
        kw = {}
        if bounds is not None:
            kw = dict(bounds_check=bounds, oob_is_err=False)
        if scatter:
            ins = self.nc.gpsimd.indirect_dma_start(out=out_ap, out_offset=off, in_=in_ap, in_offset=None, **kw)
        else:
            ins = self.nc.gpsimd.indirect_dma_start(out=out_ap, out_offset=None, in_=in_ap, in_offset=off, **kw)
        ins.then_inc(ds[0], 16)
        sk = ('d', id(ds))
        ev = (sk, ds[0], ds[1])
        self.allsems[sk] = (ds[0], ds[1])
        self._record(ev, reads, [out_t], stream=stream)

    def barrier(self):
        for en in self.engs:
            deps = [(sk, sh, v) for sk, (sh, v) in self.allsems.items() if v > 0 and sk != ('e', en)]
            self._wait(en, deps)
        self.res = {}
        for ds in list(self.dsem.values()) + [d for st in self.ssem.values() for d in st[0]]:
            self.sempool.append(ds)
            self.allsems.pop(('d', id(ds)), None)
        self.dsem = {}
        self.ssem = {}

    def mm(self, out, lhsT, rhs, reads, writes, start=True, stop=True, inc=True):
        return self.op('pe', lambda e: e.matmul(out, lhsT, rhs, start=start, stop=stop), reads, writes, inc)

    def tr(self, out, in_, ident, reads, writes, inc=True):
        return self.op('pe', lambda e: e.transpose(out, in_, ident), reads, writes, inc)

    def act(self, out, in_, func, reads, writes, bias=0.0, scale=1.0, accum_out=None):
        if accum_out is None:
            return self.op('act', lambda e: e.activation(out, in_, func, bias=bias, scale=scale), reads, writes)
        return self.op('act', lambda e: e.activation(out, in_, func, bias=bias, scale=scale,
                                                     accum_out=accum_out), reads, writes)

    def tt(self, en, out, in0, in1, op, reads, writes):
        return self.op(en, lambda e: e.tensor_tensor(out, in0, in1, op), reads, writes)

    def ts(self, en, out, in0, s1, s2, op0, op1, reads, writes):
        if s2 is None:
            return self.op(en, lambda e: e.tensor_scalar(out, in0, s1, None, op0), reads, writes)
        return self.op(en, lambda e: e.tensor_scalar(out, in0, s1, s2, op0, op1), reads, writes)

    def stt(self, en, out, in0, scalar, in1, op0, op1, reads, writes):
        en = 'dve'
        return self.op(en, lambda e: e.scalar_tensor_tensor(out, in0, scalar, in1, op0, op1), reads, writes)

    def copy(self, en, out, in_, reads, writes):
        if en == 'act':
            return self.op('act', lambda e: e.copy(out, in_), reads, writes)
        return self.op(en, lambda e: e.tensor_copy(out, in_), reads, writes)

    def memset(self, en, t, ap, val):
        return self.op(en, lambda e: e.memset(ap, val), (), [t])


class Prog:
    def __init__(self, dbg=(), upto=99):
        self.nc = bass.Bass("TRN2", target_bir_lowering=False)
        self.k = K(self.nc)
        self.dbg = set(dbg)
        self.upto = upto
        nc = self.nc
        self.inp = {}
        self.inp['x'] = T(nc.dram_tensor('x', [S, D], F32, kind='ExternalInput').ap())
        self.inp['mem'] = T(nc.dram_tensor('mem', [NM, D], F32, kind='ExternalInput').ap())
        for n, shp in WEIGHT_SPECS:
            self.inp[n] = T(nc.dram_tensor(n, list(shp), F32, kind='ExternalInput').ap())
        self.out = T(nc.dram_tensor('out', [S, D], F32, kind='ExternalOutput').ap())

    def scratch(self, name, shape, dt=F32):
        kind = 'ExternalOutput' if name in self.dbg else 'Internal'
        return self.k.dram(name, shape, dt, kind=kind)

    def consts(self):
        k = self.k
        self.reg_w = self.nc.gpsimd.to_reg(2 * NE * 1024 - 1)
        self.reg_b = self.nc.gpsimd.to_reg(2 * NE * 16 - 1)
        self.ident = k.sb([128, 128], F32, 'ident')
        k.memset('pool', self.ident, self.ident[:], 1.0)
        k.op('pool', lambda e: e.affine_select(self.ident[:], self.ident[:], [[-1, 128]], ALU.is_equal, 0.0,
                                               base=0, channel_multiplier=1), [self.ident], [self.ident])
        self.identb = k.sb([128, 128], BF16, 'identb')
        k.copy('pool', self.identb[:], self.ident[:], [self.ident], [self.identb])
        self.eps_ln = k.sb([128, 1], F32, 'epsln')
        k.memset('pool', self.eps_ln, self.eps_ln[:], LN_EPS)
        self.one_c = k.sb([128, 1], F32, 'onec')
        k.memset('pool', self.one_c, self.one_c[:], 1.0)
        self.mask_le = k.sb([128, 128], F32, 'maskle')
        k.memset('pool', self.mask_le, self.mask_le[:], 1.0)
        k.op('pool', lambda e: e.affine_select(self.mask_le[:], self.mask_le[:], [[1, 128]], ALU.is_ge, 0.0,
                                               base=0, channel_multiplier=-1), [self.mask_le], [self.mask_le])
        self.pb = [k.ps([128, 512], F32, 'bank') for _ in range(8)]
        self.pbi = 0
        self.hT = None
        self.hT_es = None
        k.barrier()

    def alloc_hT(self):
        k = self.k
        self.hT_es = ExitStack()
        self.hT = T(self.hT_es.enter_context(self.nc.sbuf_tensor(k.name('hT'), [128, 8, S + 1], BF16)))
        k.memset('pool', self.hT, self.hT[:, :, 0:1], 0.0)

    def free_hT(self):
        self.hT_es.close()
        self.hT = None

    def bank(self):
        b = self.pb[self.pbi % 8]
        self.pbi += 1
        return b

    def bc_load(self, dram_t, src_ap, n, name='bc'):
        k = self.k
        t = k.sb([128, n], F32, name)
        k.dma('sp', t[:], src_ap.partition_broadcast(128), [dram_t], t)
        return t

    def ln_tile(self, z, g_bc, b_bc, ti, H, out_dram=None, defer=False, store_q='pool'):
        k = self.k
        st = k.sb([128, 2, 6], F32, 'lnst')
        mv = k.sb([128, 2], F32, 'lnmv')
        for c in range(2):
            k.op('dve', lambda e, c=c: e.bn_stats(st[:, c, :], z[:, c * 512:(c + 1) * 512]), [z], [st])
        k.op('dve', lambda e: e.bn_aggr(mv[:], st[:].rearrange("p a b -> p (a b)")), [st], [mv])
        rstd = k.sb([128, 1], F32, 'lnr')
        k.act(rstd[:], mv[:, 1:2], AF.Sqrt, [mv, self.eps_ln], [rstd], bias=self.eps_ln[:], scale=1.0)
        k.op('dve', lambda e: e.reciprocal(rstd[:], rstd[:]), [rstd], [rstd])
        k.ts('dve', z[:], z[:], mv[:, 0:1], rstd[:], ALU.subtract, ALU.mult, [z, mv, rstd], [z])
        k.tt('dve', z[:], z[:], g_bc[:], ALU.mult, [z, g_bc], [z])
        k.tt('dve', z[:], z[:], b_bc[:], ALU.add, [z, b_bc], [z])
        tgt = out_dram if out_dram is not None else H
        k.dma(store_q, tgt[ti * 128:(ti + 1) * 128, :], z[:], [z], tgt, stream=True)
        if out_dram is None:
            if defer:
                return lambda: self.to_hT(z, ti)
            self.to_hT(z, ti)
        return None

    def to_hT(self, z, ti):
        k = self.k
        for half in range(2):
            b = self.bank()
            for j in range(4):
                c = half * 4 + j
                k.tr(b[:, j * 128:(j + 1) * 128], z[:, c * 128:(c + 1) * 128], self.ident[:], [z, self.ident], [b],
                     inc=(j == 3))
            en = 'act' if half == 0 else 'dve'
            k.copy(en, self.hT[:, half * 4:(half + 1) * 4, 1 + ti * 128:1 + (ti + 1) * 128],
                   b[:].rearrange("p (c t) -> p c t", c=4), [b], [self.hT])

    def phase_ln0(self, H):
        k = self.k
        with k.phase():
            g = self.bc_load(self.inp['ln0_g'], self.inp['ln0_g'][:], D)
            b = self.bc_load(self.inp['ln0_b'], self.inp['ln0_b'][:], D)
            zs = [k.sb([128, D], F32, 'z') for _ in range(2)]
            for ti in range(NT):
                z = zs[ti % 2]
                k.dma('sp', z[:], self.inp['x'][ti * 128:(ti + 1) * 128, :], [self.inp['x']], z)
                self.ln_tile(z, g, b, ti, H)

    def load_w(self, src_t, ap, n, name='w', dt=BF16):
        k = self.k
        wt = k.sb([128, 8, n], dt, name)
        eng = 'pool' if dt != F32 else 'sp'
        k.dma(eng, wt[:], ap.rearrange("(c p) n -> p c n", p=128), [src_t], wt)
        return wt

    def phase_mixproj(self, l, qkT, IF, PT):
        k = self.k
        w_in = self.inp['w_in']
        with k.phase():
            wqk = self.load_w(w_in, w_in[l, :, 0:1024], 1024, 'wqk')
            cw = k.sb([128, 4, 8], F32, 'cw')
            for j in range(4):
                k.dma('sp', cw[:, j, :], self.inp['m_conv_w'][l, j].rearrange("(c p) -> p c", p=128),
                      [self.inp['m_conv_w']], cw, allow_slow_non_contiguous=True)
            cb = k.sb([128, 8], F32, 'cb')
            k.dma('sp', cb[:], self.inp['m_conv_b'][l].rearrange("(c p) -> p c", p=128), [self.inp['m_conv_b']], cb,
                  allow_slow_non_contiguous=True)
            raws = [k.sb([128, S + 3], F32, 'raw') for _ in range(2)]
            accs = [k.sb([128, S], F32, 'acc') for _ in range(2)]
            for r in raws:
                k.memset('pool', r, r[:, 0:3], 0.0)
            for c in range(8):
                raw = raws[c % 2]
                acc = accs[c % 2]
                for tb in range(4):
                    b = self.bank()
                    for kc in range(8):
                        k.mm(b[:], wqk[:, kc, c * 128:(c + 1) * 128], self.hT[:, kc, 1 + tb * 512:1 + (tb + 1) * 512],
                             [wqk, self.hT], [b], start=(kc == 0), stop=(kc == 7), inc=(kc == 7))
                    k.copy('act', raw[:, 3 + tb * 512:3 + (tb + 1) * 512], b[:], [b], [raw])
                en = 'dve' if c % 2 == 0 else 'pool'
                k.ts(en, acc[:], raw[:, 3:3 + S], cw[:, 3, c:c + 1], cb[:, c:c + 1], ALU.mult, ALU.add, [raw, cw, cb], [acc])
                for j in range(3):
                    k.stt(en, acc[:], raw[:, j:j + S], cw[:, j, c:c + 1], acc[:], ALU.mult, ALU.add, [raw, cw, acc], [acc])
                k.act(acc[:], acc[:], AF.Silu, [acc], [acc])
                k.ts(en, qkT[:, c, :], acc[:], (1.0 if c < 4 else float(M_HD ** -0.5)), None, ALU.mult, None, [acc], [qkT])
            wif = self.load_w(w_in, w_in[l, :, 2048:2056], 8, 'wif')
            gb = k.sb([8, 1], F32, 'gb')
            k.dma('sp', gb[0:4, :], self.inp['m_ig_b'][l].rearrange("(a b) -> a b", b=1), [self.inp['m_ig_b']], gb)
            k.dma('sp', gb[4:8, :], self.inp['m_fg_b'][l].rearrange("(a b) -> a b", b=1), [self.inp['m_fg_b']], gb)
            ifs = k.sb([8, S], F32, 'ifs')
            for tb in range(4):
                b = self.bank()
                for kc in range(8):
                    k.mm(b[0:8, :], wif[:, kc, :], self.hT[:, kc, 1 + tb * 512:1 + (tb + 1) * 512],
                         [wif, self.hT], [b], start=(kc == 0), stop=(kc == 7), inc=(kc == 7))
                k.ts('dve', ifs[:, tb * 512:(tb + 1) * 512], b[0:8, :], gb[:, 0:1], None, ALU.add, None, [b, gb], [ifs])
            k.dma('sp', IF[:, :], ifs[:], [ifs], IF)
            mu = self.bc_load(self.inp['r_mu'], self.inp['r_mu'][l], R_COLS, 'mu')
            groups = [(1024, 512, 0, False), (1536, 512, 512, False)]
            off = 0
            while off < R_COLS:
                n = min(512, R_COLS - off)
                groups.append((M_COLS + off, n, 1024 + off, True))
                off += n
            outs = [k.sb([128, 512], F32, 'pto') for _ in range(3)]
            oi = 0
            for (c0, n, o0, shifted) in groups:
                with k.phase():
                    if not shifted:
                        wc = self.load_w(w_in, w_in[l, :, c0:c0 + n], n, 'wg')
                        wp = None
                    else:
                        wf = self.load_w(w_in, w_in[l, :, c0:c0 + n], n, 'wf', dt=F32)
                        wp = k.sb([128, 8, n], BF16, 'wp')
                        wc = k.sb([128, 8, n], BF16, 'wc')
                        tmp = k.sb([128, 8, n], F32, 'wtmp')
                        r0 = c0 - M_COLS
                        for kc in range(8):
                            en = 'dve' if kc % 2 == 0 else 'pool'
                            k.tt(en, tmp[:, kc, :], wf[:, kc, :], mu[:, r0:r0 + n], ALU.mult, [wf, mu], [tmp])
                            k.copy('act', wp[:, kc, :], tmp[:, kc, :], [tmp], [wp])
                            k.tt(en, wc[:, kc, :], wf[:, kc, :], tmp[:, kc, :], ALU.subtract, [wf, tmp], [wc])
                    for ti in range(NT):
                        b = self.bank()
                        nmm = 16 if shifted else 8
                        i = 0
                        for kc in range(8):
                            k.mm(b[:, 0:n], self.hT[:, kc, 1 + ti * 128:1 + (ti + 1) * 128], wc[:, kc, :],
                                 [self.hT, wc], [b], start=(i == 0), stop=(i == nmm - 1), inc=(i == nmm - 1))
                            i += 1
                        if shifted:
                            for kc in range(8):
                                k.mm(b[:, 0:n], self.hT[:, kc, ti * 128:(ti + 1) * 128], wp[:, kc, :],
                                     [self.hT, wp], [b], start=False, stop=(i == nmm - 1), inc=(i == nmm - 1))
                                i += 1
                        o = outs[oi % 3]
                        oi += 1
                        k.copy('act' if ti % 2 == 0 else 'dve', o[:, 0:n], b[:, 0:n], [b], [o])
                        k.dma('pool', PT[ti * 128:(ti + 1) * 128, o0:o0 + n], o[:, 0:n], [o], PT, stream=True)

    def phase_mlstm(self, l, qkT, IF, PT, MIX):
        k = self.k
        NCk = 16
        sc = self.scratch(f'msc{l}', [8, 64])
        with k.phase():
            Gi = k.sb([64, 128], F32, 'Gi')
            Gf = k.sb([64, 128], F32, 'Gf')
            k.dma('sp', Gi[:], IF[0:4, :].rearrange("h (c p) -> (h c) p", p=128), [IF], Gi)
            k.dma('sp', Gf[:], IF[4:8, :].rearrange("h (c p) -> (h c) p", p=128), [IF], Gf)
            ones = k.sb([64, 128], F32, 'ones')
            k.memset('pool', ones, ones[:], 1.0)
            k.act(Gf[:], Gf[:], AF.Exp, [Gf], [Gf], scale=-1.0)
            k.act(Gf[:], Gf[:], AF.Ln, [Gf, self.one_c], [Gf], bias=self.one_c[0:64, :], scale=1.0)
            csp = k.sb([64, 128], F32, 'csp')
            k.op('dve', lambda e: e.tensor_tensor_scan(csp[:], ones[:], Gf[:], 0.0, ALU.mult, ALU.add), [ones, Gf], [csp])
            u = k.sb([64, 128], F32, 'u')
            k.tt('dve', u[:], Gi[:], csp[:], ALU.add, [Gi, csp], [u])
            col = k.sb([64, 2], F32, 'col')
            k.op('dve', lambda e: e.reduce_max(col[:, 0:1], u[:], AX.X), [u], [col])
            k.ts('dve', col[:, 1:2], csp[:, 127:128], -1.0, None, ALU.mult, None, [csp], [col])
            k.dma('sp', sc[0, :].rearrange("(a b) -> a b", b=1), col[:, 0:1], [col], sc)
            k.dma('sp', sc[1, :].rearrange("(a b) -> a b", b=1), col[:, 1:2], [col], sc)
            hm = k.sb([4, 2, 16], F32, 'hm')
            k.dma('sp', hm[:, 0, :], sc[0, :].rearrange("(h c) -> h c", c=16), [sc], hm)
            k.dma('sp', hm[:, 1, :], sc[1, :].rearrange("(h c) -> h c", c=16), [sc], hm)
            mn = k.sb([4, 17], F32, 'mn')
            k.memset('dve', mn, mn[:, 0:1], 0.0)
            k.op('dve', lambda e: e.tensor_tensor_scan(mn[:, 1:17], hm[:, 0, :], hm[:, 1, :], 0.0, ALU.max, ALU.add),
                 [hm], [mn])
            ra = k.sb([4, 2, 16], F32, 'ra')
            k.tt('dve', ra[:, 0, :], mn[:, 0:16], hm[:, 0, :], ALU.max, [mn, hm], [ra])
            k.tt('dve', ra[:, 1, :], mn[:, 0:16], ra[:, 0, :], ALU.subtract, [mn, ra], [ra])
            k.ts('dve', ra[:, 0, :], ra[:, 0, :], -1.0, None, ALU.mult, None, [ra], [ra])
            k.dma('sp', sc[2, :].rearrange("(h c) -> h c", c=16), ra[:, 0, :], [ra], sc)
            k.dma('sp', sc[3, :].rearrange("(h c) -> h c", c=16), ra[:, 1, :], [ra], sc)
            negR = k.sb([64, 1], F32, 'negR')
            k.dma('sp', negR[:], sc[2, :].rearrange("(a b) -> a b", b=1), [sc], negR)
            alpha = k.sb([128, 64], F32, 'alpha')
            k.dma('sp', alpha[:], sc[3, :].partition_broadcast(128), [sc], alpha)
            k.act(alpha[:], alpha[:], AF.Exp, [alpha], [alpha])
            k.act(u[:], u[:], AF.Exp, [u, negR], [u], bias=negR[:], scale=1.0)
            k.act(csp[:], csp[:], AF.Exp, [csp, negR], [csp], bias=negR[:], scale=1.0)
            Etm = k.sb([128, 64], F32, 'Etm')
            Ttm = k.sb([128, 64], F32, 'Ttm')
            b = self.bank()
            k.tr(b[:, 0:64], u[:], self.ident[0:64, 0:64], [u, self.ident], [b])
            k.copy('dve', Etm[:], b[:, 0:64], [b], [Etm])
            b = self.bank()
            k.tr(b[:, 0:64], csp[:], self.ident[0:64, 0:64], [csp, self.ident], [b])
            k.copy('dve', Ttm[:], b[:, 0:64], [b], [Ttm])
            Cst = [k.sb([128, 129], F32, 'Cst') for _ in range(4)]
            for h in range(4):
                k.memset('pool', Cst[h], Cst[h][:], 0.0)
            Csb = [k.sb([128, 129], BF16, 'Csb') for _ in range(4)]
            Vs = [k.sb([128, 129], BF16, 'Vs') for _ in range(4)]
            Sm = [k.sb([128, 128], BF16, 'Sm') for _ in range(4)]
            kTm = [k.sb([128, 128], BF16, 'kTm') for _ in range(4)]
            vts = [k.sb([128, 1024], F32, 'vt') for _ in range(2)]
            hns = [k.sb([128, 512], F32, 'hn') for _ in range(2)]
            sml = [k.sb([128, 16], F32, 'sml') for _ in range(4)]
            for c in range(NCk):
                vt = vts[c % 2]
                hn = hns[c % 2]
                k.dma('sp', vt[:], PT[c * 128:(c + 1) * 128, 0:1024], [PT], vt)
                tsl = slice(c * 128, (c + 1) * 128)
                for h in range(4):
                    hc = h * 16 + c
                    sm = sml[h]
                    k.op('act', lambda e, h=h, hc=hc: e.activation(Vs[h][:, 0:128], vt[:, h * 128:(h + 1) * 128], AF.Copy,
                                                                 scale=Etm[:, hc:hc + 1]), [vt, Etm], [Vs[h]])
                    k.copy('dve', Vs[h][:, 128:129], Etm[:, hc:hc + 1], [Etm], [Vs[h]])
                    b1 = self.bank()
                    k.mm(b1[:, 0:128], qkT[:, 4 + h, tsl], qkT[:, h, tsl], [qkT], [b1])
                    k.tt('dve', Sm[h][:], b1[:, 0:128], self.mask_le[:], ALU.mult, [b1, self.mask_le], [Sm[h]])
                    b2 = self.bank()
                    k.mm(b2[:, 0:128], qkT[:, 4 + h, tsl], self.identb[:], [qkT, self.identb], [b2])
                    k.copy('act', kTm[h][:], b2[:, 0:128], [b2], [kTm[h]])
                    k.ts('dve', Csb[h][:], Cst[h][:], alpha[:, hc:hc + 1], None, ALU.mult, None, [Cst[h], alpha], [Csb[h]])
                    b3 = self.bank()
                    k.mm(b3[:, 0:129], Sm[h][:], Vs[h][:], [Sm[h], Vs[h]], [b3], start=True, stop=False, inc=False)
                    k.mm(b3[:, 0:129], qkT[:, h, tsl], Csb[h][:], [qkT, Csb[h]], [b3], start=False, stop=True)
                    b4 = self.bank()
                    k.mm(b4[:, 0:129], kTm[h][:], Vs[h][:], [kTm[h], Vs[h]], [b4])
                    k.stt('dve', Cst[h][:], Cst[h][:], alpha[:, hc:hc + 1], b4[:, 0:129], ALU.mult, ALU.add,
                          [Cst[h], alpha, b4], [Cst[h]])
                    k.copy('act', sm[:, 12:13], b3[:, 128:129], [b3], [sm])
                    k.stt('dve', sm[:, 13:14], sm[:, 12:13], -1.0, sm[:, 12:13], ALU.mult, ALU.max, [sm], [sm])
                    k.tt('dve', sm[:, 0:1], sm[:, 13:14], Ttm[:, hc:hc + 1], ALU.max, [sm, Ttm], [sm])
                    k.op('dve', lambda e, sm=sm: e.reciprocal(sm[:, 1:2], sm[:, 0:1]), [sm], [sm])
                    hs = hn[:, h * 128:(h + 1) * 128]
                    k.ts('dve', hs, b3[:, 0:128], sm[:, 1:2], None, ALU.mult, None, [b3, sm], [hn])
                    k.op('dve', lambda e, sm=sm, hs=hs: e.bn_stats(sm[:, 2:8], hs), [hn], [sm])
                    k.op('dve', lambda e, sm=sm: e.bn_aggr(sm[:, 8:10], sm[:, 2:8]), [sm], [sm])
                    k.act(sm[:, 10:11], sm[:, 9:10], AF.Sqrt, [sm, self.eps_ln], [sm], bias=self.eps_ln[:], scale=1.0)
                    k.op('dve', lambda e, sm=sm: e.reciprocal(sm[:, 11:12], sm[:, 10:11]), [sm], [sm])
                    k.ts('dve', hs, hs, sm[:, 8:9], sm[:, 11:12], ALU.subtract, ALU.mult, [hn, sm], [hn])
                k.act(vt[:, 512:1024], vt[:, 512:1024], AF.Sigmoid, [vt], [vt])
                k.tt('dve', hn[:], hn[:], vt[:, 512:1024], ALU.mult, [hn, vt], [hn])
                k.dma('pool', MIX[c * 128:(c + 1) * 128, 0:512], hn[:], [hn], MIX, stream=True)

    def phase_rwkv(self, l, PT, MIX):
        k = self.k
        P64 = slice(0, 64)
        with k.phase():
            def bc(n, nm):
                return self.bc_load(self.inp[n], self.inp[n][l], 512, nm)
            w0, a0, kkp, ka, rrk, gng, gnb = (bc('r_w0', 'w0'), bc('r_a0', 'a0'), bc('r_kk', 'kkp'), bc('r_ka', 'ka'),
                                               bc('r_rk', 'rrk'), bc('r_gn_g', 'gng'), bc('r_gn_b', 'gnb'))
            omk = k.sb([128, 512], F32, 'omk')
            k.ts('dve', omk[:], ka[:], -1.0, 1.0, ALU.mult, ALU.add, [ka], [omk])
            w2 = k.sb([64, 512], F32, 'w2')
            a2 = k.sb([64, 512], F32, 'a2')
            g2 = k.sb([128, 512], F32, 'g2')
            k.dma('sp', w2[:], self.inp['r_w2'][l], [self.inp['r_w2']], w2)
            k.dma('sp', a2[:], self.inp['r_a2'][l], [self.inp['r_a2']], a2)
            k.dma('sp', g2[:], self.inp['r_g2'][l], [self.inp['r_g2']], g2)

            def b2(t):
                return t[P64, :].unsqueeze(1).to_broadcast([64, 2, 512])

            def mk_mask(pattern_cm, op, base=0):
                m = k.sb([64, 8, 64], F32, 'msk')
                k.memset('pool', m, m[:], 1.0)
                k.op('pool', lambda e: e.affine_select(m[:, 0, :], m[:, 0, :], [[pattern_cm[0], 64]], op, 0.0,
                                                       base=base, channel_multiplier=pattern_cm[1]), [m], [m])
                for i in range(1, 8):
                    k.copy('pool', m[:, i, :], m[:, 0, :], [m], [m])
                return m
            mSU = mk_mask((1, -1), ALU.is_gt)
            mSL = mk_mask((-1, 1), ALU.is_gt)
            mIU = mk_mask((1, -1), ALU.is_ge)
            I8 = mk_mask((1, -1), ALU.is_equal)
            tri = mIU[:, 0, :]
            ST = k.sb([64, 8, 64], F32, 'ST')
            k.memset('pool', ST, ST[:], 0.0)

            def tm(nm):
                return k.sb([64, 2, 512], F32, nm)

            def pair(f):
                return [f(), f()]
            lw, av, gam, gprev, ginv = tm('lw'), tm('av'), tm('gam'), tm('gprev'), tm('ginv')
            kk, rk2, t1, t2, yv = tm('kk'), tm('rk2'), tm('t1'), tm('t2'), tm('yv')
            lrw = k.sb([64, 128], F32, 'lrw')
            lra = k.sb([64, 128], F32, 'lra')
            lrg = k.sb([128, 128], F32, 'lrg')
            QTb = {q: k.sb([64, 16, 64], BF16, 'QTb' + q) for q in ('a', 'b', 'k', 'r')}
            Mc, Nc, Mn, Nn, Qm = [k.sb([64, 16, 64], BF16, 'nm') for _ in range(5)]
            Wsb = k.sb([64, 8, 64], F32, 'Wsb')
            Usb = k.sb([64, 8, 64], F32, 'Usb')
            xt_p = pair(lambda: k.sb([64, 2, R_COLS], F32, 'xt'))
            gv_p, bt_p, kt_p = pair(lambda: tm('gv')), pair(lambda: tm('bt')), pair(lambda: tm('kt'))
            sm_p = pair(lambda: k.sb([64, 8, 16], F32, 'rsm'))
            gL_p = pair(lambda: k.sb([64, 16], F32, 'gL'))
            QTa_p, QTr_p = pair(lambda: k.sb([64, 16, 64], F32, 'QTa')), pair(lambda: k.sb([64, 16, 64], F32, 'QTr'))
            Pm_p = pair(lambda: k.sb([64, 16, 64], F32, 'Pm'))
            Aak_p, Arb_p, Ark_p = [pair(lambda: k.sb([64, 16, 64], F32, 'am')) for _ in range(3)]

            def h3(ap):
                return ap.rearrange("p j (h c) -> p j h c", c=64)

            def s3(ap16):
                return ap16.rearrange("p (j h) -> p j h", j=2)

            def bch(ap16):
                return s3(ap16).unsqueeze(3).to_broadcast([64, 2, 8, 64])

            def v3(bb):
                return bb[P64, :].rearrange("p (h c) -> p h c", c=64)

            def front(it):
                p = it % 2
                xt, gv, bt, kt, sm, gL = xt_p[p], gv_p[p], bt_p[p], kt_p[p], sm_p[p], gL_p[p]
                QT = {'a': QTa_p[p], 'r': QTr_p[p]}
                Pm, Aak, Arb, Ark = Pm_p[p], Aak_p[p], Arb_p[p], Ark_p[p]
                r0 = it * 128
                rr_ = xt[:, :, 0:512]
                rkx = xt[:, :, 512:1024]
                k.dma('sp', xt[:], PT[r0:r0 + 128, 1024:1024 + R_COLS].rearrange("(j p) c -> p j c", p=64), [PT], xt)
                b = self.bank()
                for j in range(2):
                    k.tr(b[P64, j * 64:(j + 1) * 64], xt[:, j, 1536:1600], self.ident[P64, P64], [xt, self.ident], [b], inc=False)
                    k.tr(b[P64, 128 + j * 64:128 + (j + 1) * 64], xt[:, j, 1600:1664], self.ident[P64, P64],
                         [xt, self.ident], [b], inc=False)
                    k.tr(b[:, 256 + j * 64:256 + (j + 1) * 64], xt[:, j, 1664:1792], self.ident[P64, P64],
                         [xt, self.ident], [b], inc=(j == 1))
                k.act(lrw[:], b[P64, 0:128], AF.Tanh, [b], [lrw])
                k.copy('dve', lra[:], b[P64, 128:256], [b], [lra])
                k.act(lrg[:], b[:, 256:384], AF.Sigmoid, [b], [lrg])
                for j in range(2):
                    js = slice(j * 64, (j + 1) * 64)
                    b = self.bank()
                    k.mm(b[P64, :], lrw[:, js], w2[:], [lrw, w2], [b])
                    k.tt('dve', lw[:, j, :], b[P64, :], w0[P64, :], ALU.add, [b, w0], [lw])
                    b = self.bank()
                    k.mm(b[P64, :], lra[:, js], a2[:], [lra, a2], [b])
                    k.tt('dve', av[:, j, :], b[P64, :], a0[P64, :], ALU.add, [b, a0], [av])
                    b = self.bank()
                    k.mm(b[P64, :], lrg[:, js], g2[:], [lrg, g2], [b])
                    k.copy('act', gv[:, j, :], b[P64, :], [b], [gv])
                k.act(lw[:], lw[:], AF.Sigmoid, [lw], [lw])
                k.ts('dve', lw[:], lw[:], -0.6065306597126334, None, ALU.mult, None, [lw], [lw])
                k.act(av[:], av[:], AF.Sigmoid, [av], [av])
                for j in range(2):
                    b = self.bank()
                    k.mm(b[P64, :], tri, lw[:, j, :], [mIU, lw], [b])
                    k.act(gam[:, j, :], b[P64, :], AF.Exp, [b], [gam])
                    k.act(ginv[:, j, :], b[P64, :], AF.Exp, [b], [ginv], scale=-1.0)
                    k.tt('dve', gprev[:, j, :], b[P64, :], lw[:, j, :], ALU.subtract, [b, lw], [gprev])
                k.act(gprev[:], gprev[:], AF.Exp, [gprev], [gprev])
                b = self.bank()
                for j in range(2):
                    for h in range(8):
                        blk = j * 8 + h
                        k.mm(b[P64, blk:blk + 1], lw[:, j, h * 64:(h + 1) * 64], self.one_c[P64, :], [lw, self.one_c], [b],
                             inc=(blk == 15))
                k.act(gL[:], b[P64, 0:16], AF.Exp, [b], [gL])
                yield
                k.tt('dve', kk[:], rkx, b2(kkp), ALU.mult, [xt, kkp], [kk])
                k.tt('dve', t1[:], kk[:], kk[:], ALU.mult, [kk], [t1])
                k.op('dve', lambda e: e.reduce_sum(s3(sm[:, 0, :]), h3(t1[:]), AX.X), [t1], [sm])
                k.act(sm[:, 1, :], sm[:, 0, :], AF.Sqrt, [sm], [sm])
                k.ts('dve', sm[:, 1, :], sm[:, 1, :], 1e-12, None, ALU.max, None, [sm], [sm])
                k.op('dve', lambda e: e.reciprocal(sm[:, 2, :], sm[:, 1, :]), [sm], [sm])
                k.tt('dve', h3(kk[:]), h3(kk[:]), bch(sm[:, 2, :]), ALU.mult, [kk, sm], [kk])
                k.tt('dve', t1[:], av[:], b2(ka), ALU.mult, [av, ka], [t1])
                k.tt('dve', t1[:], t1[:], b2(omk), ALU.add, [t1, omk], [t1])
                k.tt('dve', rk2[:], rkx, t1[:], ALU.mult, [xt, t1], [rk2])
                k.tt('dve', t1[:], rr_, rk2[:], ALU.mult, [xt, rk2], [t1])
                k.tt('dve', t1[:], t1[:], b2(rrk), ALU.mult, [t1, rrk], [t1])
                k.op('dve', lambda e: e.reduce_sum(s3(sm[:, 3, :]), h3(t1[:]), AX.X), [t1], [sm])
                k.stt('dve', gprev[:], kk[:], -1.0, gprev[:], ALU.mult, ALU.mult, [kk, gprev], [gprev])
                k.tt('dve', bt[:], kk[:], av[:], ALU.mult, [kk, av], [bt])
                k.tt('dve', bt[:], bt[:], ginv[:], ALU.mult, [bt, ginv], [bt])
                k.tt('dve', kt[:], rk2[:], ginv[:], ALU.mult, [rk2, ginv], [kt])
                k.tt('dve', gam[:], rr_, gam[:], ALU.mult, [xt, gam], [gam])
                yield
                ei = 0
                for q, src in (('a', gprev), ('b', bt), ('k', kt), ('r', gam)):
                    for j in range(2):
                        b = self.bank()
                        for h in range(8):
                            k.tr(b[P64, h * 64:(h + 1) * 64], src[:, j, h * 64:(h + 1) * 64], self.ident[P64, P64],
                                 [src, self.ident], [b], inc=(h == 7))
                        k.copy('act' if ei % 2 == 0 else 'dve', QTb[q][:, j * 8:(j + 1) * 8, :], v3(b), [b], [QTb[q]])
                        if q in QT:
                            k.copy('dve' if ei % 2 == 0 else 'act', QT[q][:, j * 8:(j + 1) * 8, :], v3(b), [b], [QT[q]])
                        ei += 1
                    if q == 'b':
                        yield
                yield
                specs = [('b', 'a', mSU, Mc), ('a', 'b', mSL, Nc), ('k', 'a', mSU, Aak), ('b', 'r', mIU, Arb),
                         ('k', 'r', mIU, Ark)]
                for si, (ql, qr, msk, dst) in enumerate(specs):
                    for j in range(2):
                        b = self.bank()
                        for h in range(8):
                            blk = j * 8 + h
                            k.mm(b[P64, h * 64:(h + 1) * 64], QTb[ql][:, blk, :], QTb[qr][:, blk, :], [QTb[ql], QTb[qr]], [b],
                                 inc=(h == 7))
                        k.tt('dve', dst[:, j * 8:(j + 1) * 8, :], v3(b), msk[:], ALU.mult, [b, msk], [dst])
                    if si == 1:
                        yield
                for j in range(2):
                    js = slice(j * 8, (j + 1) * 8)
                    k.tt('dve', Pm[:, js, :], Mc[:, js, :], I8[:], ALU.add, [Mc, I8], [Pm])
                    k.tt('dve', Qm[:, js, :], Nc[:, js, :], I8[:], ALU.add, [Nc, I8], [Qm])
                yield
                mc, ncur, mn, nn = Mc, Nc, Mn, Nn
                for lvl in range(5):
                    last = (lvl == 4)
                    bms, bns, bps, bqs = [], [], [], []
                    for j in range(2):
                        bm = self.bank()
                        for h in range(8):
                            blk = j * 8 + h
                            k.mm(bm[P64, h * 64:(h + 1) * 64], ncur[:, blk, :], mc[:, blk, :], [ncur, mc], [bm], inc=(h == 7))
                        bms.append(bm)
                        if not last:
                            bn = self.bank()
                            for h in range(8):
                                blk = j * 8 + h
                                k.mm(bn[P64, h * 64:(h + 1) * 64], mc[:, blk, :], ncur[:, blk, :], [ncur, mc], [bn],
                                     inc=(h == 7))
                            bns.append(bn)
                    for j in range(2):
                        js = slice(j * 8, (j + 1) * 8)
                        k.copy('act', mn[:, js, :], v3(bms[j]), [bms[j]], [mn])
                        if not last:
                            k.copy('dve', nn[:, js, :], v3(bns[j]), [bns[j]], [nn])
                    for j in range(2):
                        bp = self.bank()
                        for h in range(8):
                            blk = j * 8 + h
                            k.mm(bp[P64, h * 64:(h + 1) * 64], Qm[:, blk, :], mn[:, blk, :], [Qm, mn], [bp], inc=(h == 7))
                        bps.append(bp)
                        if not last:
                            bq = self.bank()
                            for h in range(8):
                                blk = j * 8 + h
                                k.mm(bq[P64, h * 64:(h + 1) * 64], mn[:, blk, :], Qm[:, blk, :], [Qm, mn], [bq],
                                     inc=(h == 7))
                            bqs.append(bq)
                    for j in range(2):
                        js = slice(j * 8, (j + 1) * 8)
                        k.tt('dve', Pm[:, js, :], Pm[:, js, :], v3(bps[j]), ALU.add, [Pm, bps[j]], [Pm])
                        if not last:
                            k.tt('dve', Qm[:, js, :], Qm[:, js, :], v3(bqs[j]), ALU.add, [Qm, bqs[j]], [Qm])
                    mc, mn = mn, mc
                    ncur, nn = nn, ncur
                    yield

            def back(it):
                p = it % 2
                xt, gv, bt, kt, sm, gL = xt_p[p], gv_p[p], bt_p[p], kt_p[p], sm_p[p], gL_p[p]
                QT = {'a': QTa_p[p], 'r': QTr_p[p]}
                Pm, Aak, Arb, Ark = Pm_p[p], Aak_p[p], Arb_p[p], Ark_p[p]
                r0 = it * 128
                rv = xt[:, :, 1024:1536]
                for j in range(2):
                    bw = self.bank()
                    for h in range(8):
                        blk = j * 8 + h
                        hs = slice(h * 64, (h + 1) * 64)
                        k.mm(bw[P64, hs], QT['a'][:, blk, :], ST[:, h, :], [QT['a'], ST], [bw], start=True, stop=False, inc=False)
                        k.mm(bw[P64, hs], Aak[:, blk, :], xt[:, j, 1024 + h * 64:1024 + (h + 1) * 64], [Aak, xt], [bw],
                             start=False, stop=True, inc=(h == 7))
                    k.copy('act', Wsb[:], v3(bw), [bw], [Wsb])
                    yield
                    bu = self.bank()
                    for h in range(8):
                        blk = j * 8 + h
                        k.mm(bu[P64, h * 64:(h + 1) * 64], Pm[:, blk, :], Wsb[:, h, :], [Pm, Wsb], [bu], inc=(h == 7))
                    k.copy('act', Usb[:], v3(bu), [bu], [Usb])
                    yield
                    by = self.bank()
                    bs_ = self.bank()
                    for h in range(8):
                        hs = slice(h * 64, (h + 1) * 64)
                        vh = xt[:, j, 1024 + h * 64:1024 + (h + 1) * 64]
                        k.mm(bs_[P64, hs], bt[:, j, hs], Usb[:, h, :], [bt, Usb], [bs_], start=True, stop=False, inc=False)
                        k.mm(bs_[P64, hs], kt[:, j, hs], vh, [kt, xt], [bs_], start=False, stop=True, inc=(h == 7))
                    for h in range(8):
                        blk = j * 8 + h
                        hs = slice(h * 64, (h + 1) * 64)
                        vh = xt[:, j, 1024 + h * 64:1024 + (h + 1) * 64]
                        k.mm(by[P64, hs], QT['r'][:, blk, :], ST[:, h, :], [QT['r'], ST], [by], start=True, stop=False, inc=False)
                        k.mm(by[P64, hs], Arb[:, blk, :], Usb[:, h, :], [Arb, Usb], [by], start=False, stop=False, inc=False)
                        k.mm(by[P64, hs], Ark[:, blk, :], vh, [Ark, xt], [by], start=False, stop=True, inc=(h == 7))
                    k.tt('dve', ST[:], ST[:], v3(bs_), ALU.add, [ST, bs_], [ST])
                    k.tt('dve', ST[:], ST[:], gL[:, j * 8:(j + 1) * 8].unsqueeze(2).to_broadcast([64, 8, 64]), ALU.mult,
                         [ST, gL], [ST])
                    k.copy('act', yv[:, j, :], by[P64, :], [by], [yv])
                    yield
                y3 = h3(yv[:])
                k.op('dve', lambda e: e.reduce_sum(s3(sm[:, 4, :]), y3, AX.X), [yv], [sm])
                k.ts('dve', sm[:, 4, :], sm[:, 4, :], 1.0 / 64, None, ALU.mult, None, [sm], [sm])
                k.tt('dve', y3, y3, bch(sm[:, 4, :]), ALU.subtract, [yv, sm], [yv])
                k.tt('dve', t2[:], yv[:], yv[:], ALU.mult, [yv], [t2])
                k.op('dve', lambda e: e.reduce_sum(s3(sm[:, 5, :]), h3(t2[:]), AX.X), [t2], [sm])
                k.ts('dve', sm[:, 5, :], sm[:, 5, :], 1.0 / 64, 64e-5, ALU.mult, ALU.add, [sm], [sm])
                k.act(sm[:, 5, :], sm[:, 5, :], AF.Sqrt, [sm], [sm])
                k.op('dve', lambda e: e.reciprocal(sm[:, 6, :], sm[:, 5, :]), [sm], [sm])
                k.tt('dve', y3, y3, bch(sm[:, 6, :]), ALU.mult, [yv, sm], [yv])
                yield
                k.tt('dve', yv[:], yv[:], b2(gng), ALU.mult, [yv, gng], [yv])
                k.tt('dve', yv[:], yv[:], b2(gnb), ALU.add, [yv, gnb], [yv])
                k.tt('dve', h3(t2[:]), h3(rv), bch(sm[:, 3, :]), ALU.mult, [xt, sm], [t2])
                k.tt('dve', yv[:], yv[:], t2[:], ALU.add, [yv, t2], [yv])
                k.tt('dve', yv[:], yv[:], gv[:], ALU.mult, [yv, gv], [yv])
                k.dma('pool', MIX[r0:r0 + 128, 512:1024].rearrange("(j p) c -> p j c", p=64), yv[:], [yv], MIX, stream=True)
                yield

            RWI = 16

            def drive(gens):
                gens = [g for g in gens if g is not None]
                while gens:
                    for g in list(gens):
                        try:
                            next(g)
                        except StopIteration:
                            gens.remove(g)
            drive([front(0)])
            for it in range(RWI):
                drive([back(it), front(it + 1) if it + 1 < RWI else None])

    def resid_ln(self, banks, ti, H, g_bc, b_bc, zs, out_dram=None, defer=False):
        k = self.k
        z = zs[ti % len(zs)]
        k.dma('sp', z[:], H[ti * 128:(ti + 1) * 128, :], [H], z)
        if isinstance(banks, (list, tuple)):
            for hf in range(2):
                k.stt('dve', z[:, hf * 512:(hf + 1) * 512], z[:, hf * 512:(hf + 1) * 512], DN_ALPHA, banks[hf][:],
                      ALU.mult, ALU.add, [z, banks[hf]], [z])
        else:
            k.stt('dve', z[:], z[:], DN_ALPHA, banks[:], ALU.mult, ALU.add, [z, banks], [z])
        return self.ln_tile(z, g_bc, b_bc, ti, H, out_dram=out_dram, defer=defer)

    def phase_outproj(self, l, MIX, H):
        k = self.k
        wo_t = self.inp['w_out']
        with k.phase():
            wo = self.load_w(wo_t, wo_t[l], 1024, 'wo')
            gcol = k.sb([128, 4], F32, 'gcol')
            k.dma('sp', gcol[:], self.inp['m_norm_g'][l].rearrange("(c p) -> p c", p=128), [self.inp['m_norm_g']], gcol,
                  allow_slow_non_contiguous=True)
            for c in range(4):
                k.ts('dve', wo[:, c, :], wo[:, c, :], gcol[:, c:c + 1], None, ALU.mult, None, [wo, gcol], [wo])
            g = self.bc_load(self.inp['ln1_g'], self.inp['ln1_g'][l], D)
            b = self.bc_load(self.inp['ln1_b'], self.inp['ln1_b'][l], D)
            zs = [k.sb([128, D], F32, 'z') for _ in range(3)]
            pend = None
            ms = [k.sb([128, D], F32, 'mx') for _ in range(2)]
            mTs = [k.sb([128, 8, 128], BF16, 'mT') for _ in range(2)]
            for ti in range(NT):
                m = ms[ti % 2]
                mT = mTs[ti % 2]
                k.dma('sp', m[:], MIX[ti * 128:(ti + 1) * 128, :], [MIX], m)
                for half in range(2):
                    bb = self.bank()
                    for j in range(4):
                        c = half * 4 + j
                        k.tr(bb[:, j * 128:(j + 1) * 128], m[:, c * 128:(c + 1) * 128], self.ident[:], [m, self.ident], [bb],
                             inc=(j == 3))
                    k.copy('act', mT[:, half * 4:(half + 1) * 4, :], bb[:].rearrange("p (c t) -> p c t", c=4), [bb], [mT])
                bks = [self.bank(), self.bank()]
                for hf in range(2):
                    for kc in range(8):
                        k.mm(bks[hf][:], mT[:, kc, :], wo[:, kc, hf * 512:(hf + 1) * 512], [mT, wo], [bks[hf]],
                             start=(kc == 0), stop=(kc == 7), inc=(kc == 7))
                if pend is not None:
                    pend()
                pend = self.resid_ln(bks, ti, H, g, b, zs, defer=True)
            pend()

    def xattn_preload(self, l):
        return (self.load_w(self.inp['x_wq'], self.inp['x_wq'][l], 1024, 'wq'),
                self.load_w(self.inp['x_wkv'], self.inp['x_wkv'][l], 2048, 'wkv'),
                self.load_w(self.inp['x_wo'], self.inp['x_wo'][l], 1024, 'xwo'))

    def phase_xattn(self, l, H, pre=None):
        k = self.k
        with k.phase():
            wq, wkv, wo = pre if pre is not None else self.xattn_preload(l)
            g = self.bc_load(self.inp['ln2_g'], self.inp['ln2_g'][l], D)
            b = self.bc_load(self.inp['ln2_b'], self.inp['ln2_b'][l], D)
            zs = [k.sb([128, D], F32, 'z') for _ in range(3)]
            pend = None
            memT = k.sb([128, 8, NM], BF16, 'memT')
            for mt in range(2):
                z = zs[mt]
                k.dma('sp', z[:], self.inp['mem'][mt * 128:(mt + 1) * 128, :], [self.inp['mem']], z)
                for half in range(2):
                    bb = self.bank()
                    for j in range(4):
                        c = half * 4 + j
                        k.tr(bb[:, j * 128:(j + 1) * 128], z[:, c * 128:(c + 1) * 128], self.ident[:], [z, self.ident], [bb],
                             inc=(j == 3))
                    k.copy('act', memT[:, half * 4:(half + 1) * 4, mt * 128:(mt + 1) * 128],
                           bb[:].rearrange("p (c t) -> p c t", c=4), [bb], [memT])
            KT = k.sb([128, 8, NM], BF16, 'KT')
            for c in range(8):
                bb = self.bank()
                for kc in range(8):
                    k.mm(bb[:, 0:NM], wkv[:, kc, c * 128:(c + 1) * 128], memT[:, kc, :], [wkv, memT], [bb],
                         start=(kc == 0), stop=(kc == 7), inc=(kc == 7))
                k.copy('act' if c % 2 else 'dve', KT[:, c, :], bb[:, 0:NM], [bb], [KT])
            Vt = k.sb([128, 2, 1024], BF16, 'Vt')
            for mt in range(2):
                for hf in range(2):
                    bb = self.bank()
                    for kc in range(8):
                        k.mm(bb[:], memT[:, kc, mt * 128:(mt + 1) * 128], wkv[:, kc, 1024 + hf * 512:1024 + (hf + 1) * 512],
                             [wkv, memT], [bb], start=(kc == 0), stop=(kc == 7), inc=(kc == 7))
                    k.copy('act' if hf else 'dve', Vt[:, mt, hf * 512:(hf + 1) * 512], bb[:], [bb], [Vt])
            QTs = [k.sb([128, 8, 128], BF16, 'QT') for _ in range(2)]
            OTs = [k.sb([128, 8, 128], BF16, 'OT') for _ in range(2)]
            Pf = [k.sb([128, NM], F32, 'Pf') for _ in range(4)]
            Pn = [k.sb([128, NM], BF16, 'Pn') for _ in range(4)]
            PTt = [k.sb([128, 2, 128], BF16, 'PTt') for _ in range(4)]
            st = [k.sb([128, 4], F32, 'xst') for _ in range(4)]
            scale = float(256 ** -0.5)

            def qproj(ti):
                QT = QTs[ti % 2]
                tsl = slice(1 + ti * 128, 1 + (ti + 1) * 128)
                for half in range(2):
                    bb = self.bank()
                    for j in range(4):
                        c = half * 4 + j
                        for kc in range(8):
                            k.mm(bb[:, j * 128:(j + 1) * 128], wq[:, kc, c * 128:(c + 1) * 128], self.hT[:, kc, tsl],
                                 [wq, self.hT], [bb], start=(kc == 0), stop=(kc == 7), inc=(kc == 7 and j == 3))
                    k.copy('act' if half else 'dve', QT[:, half * 4:(half + 1) * 4, :],
                           bb[:].rearrange("p (c t) -> p c t", c=4), [bb], [QT])

            qproj(0)
            for ti in range(NT):
                QT, OT = QTs[ti % 2], OTs[ti % 2]
                bss = []
                for h in range(4):
                    bs = self.bank()
                    for dc in range(2):
                        k.mm(bs[:, 0:NM], QT[:, 2 * h + dc, :], KT[:, 2 * h + dc, :], [QT, KT], [bs],
                             start=(dc == 0), stop=(dc == 1), inc=(dc == 1))
                    bss.append(bs)
                if ti + 1 < NT:
                    qproj(ti + 1)
                if pend is not None:
                    pend()
                    pend = None
                for h in range(4):
                    s_, bs = st[h], bss[h]
                    k.op('dve', lambda e, s_=s_, bs=bs: e.reduce_max(s_[:, 0:1], bs[:, 0:NM], AX.X), [bs], [s_])
                    k.ts('dve', s_[:, 1:2], s_[:, 0:1], -scale, None, ALU.mult, None, [s_], [s_])
                    k.act(Pf[h][:], bs[:, 0:NM], AF.Exp, [bs, s_], [Pf[h], s_], bias=s_[:, 1:2], scale=scale,
                          accum_out=s_[:, 2:3])
                for h in range(4):
                    s_ = st[h]
                    k.op('dve', lambda e, s_=s_: e.reciprocal(s_[:, 3:4], s_[:, 2:3]), [s_], [s_])
                    k.ts('dve', Pn[h][:], Pf[h][:], s_[:, 3:4], None, ALU.mult, None, [Pf[h], s_], [Pn[h]])
                bts = []
                for h in range(4):
                    bt_ = self.bank()
                    for mc in range(2):
                        k.mm(bt_[:, mc * 128:(mc + 1) * 128], Pn[h][:, mc * 128:(mc + 1) * 128], self.identb[:],
                             [Pn[h], self.identb], [bt_], inc=(mc == 1))
                    bts.append(bt_)
                for h in range(4):
                    k.copy('act' if h % 2 else 'dve', PTt[h][:], bts[h][:, 0:256].rearrange("p (c t) -> p c t", c=2),
                           [bts[h]], [PTt[h]])
                bos = []
                for h in range(4):
                    bo = self.bank()
                    for dc in range(2):
                        c = 2 * h + dc
                        for mc in range(2):
                            k.mm(bo[:, dc * 128:(dc + 1) * 128], Vt[:, mc, c * 128:(c + 1) * 128], PTt[h][:, mc, :],
                                 [Vt, PTt[h]], [bo], start=(mc == 0), stop=(mc == 1), inc=(mc == 1 and dc == 1))
                    bos.append(bo)
                for h in range(4):
                    k.copy('dve' if h % 2 else 'act', OT[:, 2 * h:2 * h + 2, :],
                           bos[h][:, 0:256].rearrange("p (c t) -> p c t", c=2), [bos[h]], [OT])
                bks = [self.bank(), self.bank()]
                for hf in range(2):
                    for kc in range(8):
                        k.mm(bks[hf][:], OT[:, kc, :], wo[:, kc, hf * 512:(hf + 1) * 512], [OT, wo], [bks[hf]],
                             start=(kc == 0), stop=(kc == 7), inc=(kc == 7))
                pend = self.resid_ln(bks, ti, H, g, b, zs, defer=True)
            pend()

    def phase_moe(self, l, H, final_out=None):
        k = self.k
        w1_t, w2_t = self.inp['moe_w1'], self.inp['moe_w2']
        NEX = NE
        DMAONLY = 0
        with k.phase():
            G = k.sb([128, NT, NE], F32, 'G')
            acc = k.sb([128, NT, D], F32, 'acc')
            with k.phase():
                wr = k.sb([128, 8, NE], F32, 'wr')
                k.dma('sp', wr[:], self.inp['moe_wr'][l].rearrange("(c p) n -> p c n", p=128), [self.inp['moe_wr']], wr)
                br = self.bc_load(self.inp['moe_br'], self.inp['moe_br'][l], NE, 'br')
                b2 = k.sb([NE, D], F32, 'b2')
                k.dma('sp', b2[:], self.inp['moe_b2'][l], [self.inp['moe_b2']], b2)
                hts = [k.sb([128, D], F32, 'ht') for _ in range(2)]
                h32s = [k.sb([128, 8, 128], F32, 'h32') for _ in range(2)]
                lgs = [k.sb([128, NE], F32, 'lg') for _ in range(2)]
                m8s = [k.sb([128, 16], F32, 'm8') for _ in range(2)]
                GTs = [k.sb([NE, 128], F32, 'GT') for _ in range(2)]
                for ti in range(NT):
                    ht, h32, lg, m8, GT = hts[ti % 2], h32s[ti % 2], lgs[ti % 2], m8s[ti % 2], GTs[ti % 2]
                    k.dma('sp', ht[:], H[ti * 128:(ti + 1) * 128, :], [H], ht)
                    for half in range(2):
                        bb = self.bank()
                        for j in range(4):
                            c = half * 4 + j
                            k.tr(bb[:, j * 128:(j + 1) * 128], ht[:, c * 128:(c + 1) * 128], self.ident[:], [ht, self.ident],
                                 [bb], inc=(j == 3))
                        k.copy('act', h32[:, half * 4:(half + 1) * 4, :], bb[:].rearrange("p (c t) -> p c t", c=4), [bb], [h32])
                    bl = self.bank()
                    for kc in range(8):
                        k.mm(bl[:, 0:NE], h32[:, kc, :], wr[:, kc, :], [h32, wr], [bl], start=(kc == 0), stop=(kc == 7),
                             inc=(kc == 7))
                    k.tt('dve', lg[:], bl[:, 0:NE], br[:], ALU.add, [bl, br], [lg])
                    k.op('dve', lambda e, m8=m8, lg=lg: e.max(m8[:, 0:8], lg[:]), [lg], [m8])
                    k.ts('dve', m8[:, 8:9], m8[:, 0:1], -1.0, None, ALU.mult, None, [m8], [m8])
                    g_ = G[:, ti, :]
                    k.act(g_, lg[:], AF.Exp, [lg, m8], [G], bias=m8[:, 8:9], scale=1.0)
                    k.ts('dve', lg[:], lg[:], m8[:, 3:4], None, ALU.is_ge, None, [lg, m8], [lg])
                    k.tt('dve', g_, g_, lg[:], ALU.mult, [G, lg], [G])
                    k.op('dve', lambda e, m8=m8, g_=g_: e.reduce_sum(m8[:, 9:10], g_, AX.X), [G], [m8])
                    k.op('dve', lambda e, m8=m8: e.reciprocal(m8[:, 10:11], m8[:, 9:10]), [m8], [m8])
                    k.ts('dve', g_, g_, m8[:, 10:11], None, ALU.mult, None, [G, m8], [G])
                    bg = self.bank()
                    k.tr(bg[0:NE, 0:128], g_, self.ident[:], [G, self.ident], [bg])
                    k.copy('act', GT[:], bg[0:NE, 0:128], [bg], [GT])
                    for hf in range(2):
                        bb = self.bank()
                        k.mm(bb[:], GT[:], b2[:, hf * 512:(hf + 1) * 512], [GT, b2], [bb])
                        k.copy('act' if hf else 'dve', acc[:, ti, hf * 512:(hf + 1) * 512], bb[:], [bb], [acc])
            with k.phase():
                b1a = k.sb([128, NE, 16], F32, 'b1a')
                for e in range(NE):
                    k.dma('sp', b1a[:, e, :], self.inp['moe_b1'][l, e].rearrange("(c p) -> p c", p=128),
                          [self.inp['moe_b1']], b1a, allow_slow_non_contiguous=True)
                actT = k.sb([128, 8, S], BF16, 'actT')
                w1r = [k.sb([128, 8, 2, 128], BF16, 'w1r') for _ in range(4)]
                w2b = k.sb([128, 8, D], BF16, 'w2b')
                g0s = [k.sb([128, 512], F32, 'g0') for _ in range(2)]
                sgs = [k.sb([128, 512], F32, 'sg') for _ in range(2)]
                u0s = [k.sb([128, 512], F32, 'u0') for _ in range(2)]
                pi = 0
                ei = 0
                for e in range(NEX):
                    for p in range(8):
                        w1 = w1r[pi % 4]
                        pi += 1
                        for gu in range(2):
                            c0 = gu * DFF + p * 128
                            k.dma('pool', w1[:, :, gu, :], w1_t[l, e, :, c0:c0 + 128].rearrange("(c p) n -> p c n", p=128),
                                  [w1_t], w1)
                        if p == 0:
                            k.dma('pool', w2b[:], w2_t[l, e].rearrange("(c p) n -> p c n", p=128), [w2_t], w2b)
                        for tb in range(4 if not DMAONLY else 0):
                            tsl = slice(1 + tb * 512, 1 + (tb + 1) * 512)
                            bgp, bup = self.bank(), self.bank()
                            for gu, bb in ((0, bgp), (1, bup)):
                                for kc in range(8):
                                    k.mm(bb[:], w1[:, kc, gu, :], self.hT[:, kc, tsl], [w1, self.hT], [bb],
                                         start=(kc == 0), stop=(kc == 7), inc=(kc == 7))
                            g0, sg, u0 = g0s[ei % 2], sgs[ei % 2], u0s[ei % 2]
                            ei += 1
                            k.act(g0[:], bgp[:], AF.Identity, [bgp, b1a], [g0], bias=b1a[:, e, p:p + 1], scale=1.0)
                            k.ts('dve', g0[:], g0[:], 7.0, None, ALU.min, None, [g0], [g0])
                            k.act(sg[:], g0[:], AF.Sigmoid, [g0], [sg], scale=1.702)
                            k.tt('dve', sg[:], sg[:], g0[:], ALU.mult, [sg, g0], [sg])
                            k.act(u0[:], bup[:], AF.Identity, [bup, b1a], [u0], bias=b1a[:, e, 8 + p:9 + p], scale=1.0)
                            k.ts('dve', u0[:], u0[:], 7.0, -7.0, ALU.min, ALU.max, [u0], [u0])
                            k.stt('dve', actT[:, p, tb * 512:(tb + 1) * 512], u0[:], 1.0, sg[:], ALU.add, ALU.mult,
                                  [u0, sg], [actT])
                    for ti in range(NT if not DMAONLY else 0):
                        bks = [self.bank(), self.bank()]
                        for hf in range(2):
                            for fc in range(8):
                                k.mm(bks[hf][:], actT[:, fc, ti * 128:(ti + 1) * 128], w2b[:, fc, hf * 512:(hf + 1) * 512],
                                     [actT, w2b], [bks[hf]], start=(fc == 0), stop=(fc == 7), inc=(fc == 7))
                        for hf in range(2):
                            k.stt('dve', acc[:, ti, hf * 512:(hf + 1) * 512], bks[hf][:], G[:, ti, e:e + 1],
                                  acc[:, ti, hf * 512:(hf + 1) * 512], ALU.mult, ALU.add, [bks[hf], G, acc], [acc])
            with k.phase():
                g = self.bc_load(self.inp['ln3_g'], self.inp['ln3_g'][l], D)
                b = self.bc_load(self.inp['ln3_b'], self.inp['ln3_b'][l], D)
                zs = [k.sb([128, D], F32, 'z') for _ in range(2)]
                for ti in range(NT):
                    z = zs[ti % 2]
                    k.dma('sp', z[:], H[ti * 128:(ti + 1) * 128, :], [H], z)
                    k.stt('dve', z[:], z[:], DN_ALPHA, acc[:, ti, :], ALU.mult, ALU.add, [z, acc], [z])
                    self.ln_tile(z, g, b, ti, H, out_dram=final_out)

    def phase_moe_sparse(self, l, H, YB, RT, final_out=None):
        k = self.k
        I32 = mybir.dt.int32
        BLK = MOE_BLK
        NTB = BLK // 128
        NB = MOE_NB
        NR = NB * BLK
        BIG = 4.0e6
        w1v = self.inp['moe_w1'][:].rearrange("l e d f -> (l e d) f")
        w2v = self.inp['moe_w2'][:].rearrange("l e f d -> (l e f) d")
        b1v = self.inp['moe_b1'][:].rearrange("l e (c f) -> (l e c) f", f=128)
        with k.phase():
            G = k.sb([128, NT, NE], F32, 'G')
            dsel = k.sb([128, NT, 4], I32, 'dsel')
            gsel = k.sb([128, NT, 4], F32, 'gsel')
            IW = k.sb([128, NB, 8], I32, 'IW')
            IB = k.sb([16, NB], I32, 'IB')
            b2 = k.sb([NE, D], F32, 'b2')
            k.dma('sp', b2[:], self.inp['moe_b2'][l], [self.inp['moe_b2']], b2)
            with k.phase():
                wr = k.sb([128, 8, NE], F32, 'wr')
                k.dma('sp', wr[:], self.inp['moe_wr'][l].rearrange("(c p) n -> p c n", p=128), [self.inp['moe_wr']], wr)
                br = self.bc_load(self.inp['moe_br'], self.inp['moe_br'][l], NE, 'br')
                Mk = k.sb([128, NT, NE], F32, 'Mk')
                rank = k.sb([128, NT, NE], F32, 'rank')
                hts = [k.sb([128, D], F32, 'ht') for _ in range(2)]
                h32s = [k.sb([128, 8, 128], F32, 'h32') for _ in range(2)]
                lgs = [k.sb([128, NE], F32, 'lg') for _ in range(2)]
                m8s = [k.sb([128, 16], F32, 'm8') for _ in range(2)]
                for ti in range(NT):
                    ht, h32, lg, m8 = hts[ti % 2], h32s[ti % 2], lgs[ti % 2], m8s[ti % 2]
                    k.dma('sp', ht[:], H[ti * 128:(ti + 1) * 128, :], [H], ht)
                    for half in range(2):
                        bb = self.bank()
                        for j in range(4):
                            c = half * 4 + j
                            k.tr(bb[:, j * 128:(j + 1) * 128], ht[:, c * 128:(c + 1) * 128], self.ident[:], [ht, self.ident],
                                 [bb], inc=(j == 3))
                        k.copy('act', h32[:, half * 4:(half + 1) * 4, :], bb[:].rearrange("p (c t) -> p c t", c=4), [bb], [h32])
                    bl = self.bank()
                    for kc in range(8):
                        k.mm(bl[:, 0:NE], h32[:, kc, :], wr[:, kc, :], [h32, wr], [bl], start=(kc == 0), stop=(kc == 7),
                             inc=(kc == 7))
                    k.tt('dve', lg[:], bl[:, 0:NE], br[:], ALU.add, [bl, br], [lg])
                    k.op('dve', lambda e, m8=m8, lg=lg: e.max(m8[:, 0:8], lg[:]), [lg], [m8])
                    k.ts('dve', m8[:, 8:9], m8[:, 0:1], -1.0, None, ALU.mult, None, [m8], [m8])
                    g_ = G[:, ti, :]
                    k.act(g_, lg[:], AF.Exp, [lg, m8], [G], bias=m8[:, 8:9], scale=1.0)
                    k.ts('dve', Mk[:, ti, :], lg[:], m8[:, 3:4], None, ALU.is_ge, None, [lg, m8], [Mk])
                    k.tt('dve', g_, g_, Mk[:, ti, :], ALU.mult, [G, Mk], [G])
                    k.op('dve', lambda e, m8=m8, g_=g_: e.reduce_sum(m8[:, 9:10], g_, AX.X), [G], [m8])
                    k.op('dve', lambda e, m8=m8: e.reciprocal(m8[:, 10:11], m8[:, 9:10]), [m8], [m8])
                    k.ts('dve', g_, g_, m8[:, 10:11], None, ALU.mult, None, [G, m8], [G])
                ones = k.sb([128, 128], F32, 'ones')
                k.memset('pool', ones, ones[:], 1.0)
                lst = k.sb([128, 128], F32, 'lst')
                k.memset('pool', lst, lst[:], 1.0)
                k.op('pool', lambda e: e.affine_select(lst[:], lst[:], [[1, 128]], ALU.is_gt, 0.0, base=0,
                                                       channel_multiplier=-1), [lst], [lst])
                for ti in range(NT):
                    bb = self.bank()
                    for tj in range(ti):
                        k.mm(bb[:, 0:NE], ones[:], Mk[:, tj, :], [ones, Mk], [bb], start=(tj == 0), stop=False, inc=False)
                    k.mm(bb[:, 0:NE], lst[:], Mk[:, ti, :], [lst, Mk], [bb], start=(ti == 0), stop=True)
                    k.copy('act' if ti % 2 else 'dve', rank[:, ti, :], bb[:, 0:NE], [bb], [rank])
                bb = self.bank()
                for tj in range(NT):
                    k.mm(bb[:, 0:NE], ones[:], Mk[:, tj, :], [ones, Mk], [bb], start=(tj == 0), stop=(tj == NT - 1),
                         inc=(tj == NT - 1))
                cnt = k.sb([128, NE], F32, 'cnt')
                k.copy('dve', cnt[:], bb[:, 0:NE], [bb], [cnt])
                thr = k.sb([128, 16], F32, 'thr')
                k.op('pool', lambda e: e.iota(thr[:], [[BLK, 16]], base=0, channel_multiplier=0,
                                              allow_small_or_imprecise_dtypes=True), [], [thr])
                cmp_ = k.sb([128, NE, 16], F32, 'cmp')
                k.tt('dve', cmp_[:], cnt[:].unsqueeze(2).to_broadcast([128, NE, 16]),
                     thr[:].unsqueeze(1).to_broadcast([128, NE, 16]), ALU.is_gt, [cnt, thr], [cmp_])
                pad = k.sb([128, 4, NE], F32, 'pad')
                k.op('dve', lambda e: e.reduce_sum(pad[:, 0, :], cmp_[:], AX.X), [cmp_], [pad])
                k.ts('dve', pad[:, 0, :], pad[:, 0, :], float(BLK), None, ALU.mult, None, [pad], [pad])
                k.op('dve', lambda e: e.tensor_tensor_scan(pad[:, 1, :], ones[:, 0:NE], pad[:, 0, :], 0.0, ALU.mult, ALU.add),
                     [ones, pad], [pad])
                k.tt('dve', pad[:, 2, :], pad[:, 1, :], pad[:, 0, :], ALU.subtract, [pad], [pad])
                bth = k.sb([128, NB], F32, 'bth')
                k.op('pool', lambda e: e.iota(bth[:], [[BLK, NB]], base=0, channel_multiplier=0,
                                              allow_small_or_imprecise_dtypes=True), [], [bth])
                cmpb = k.sb([128, NB, NE], F32, 'cmpb')
                k.tt('dve', cmpb[:], pad[:, 1, :].unsqueeze(1).to_broadcast([128, NB, NE]),
                     bth[:].unsqueeze(2).to_broadcast([128, NB, NE]), ALU.is_le, [pad, bth], [cmpb])
                be = k.sb([128, 4, NB], F32, 'be')
                k.op('dve', lambda e: e.reduce_sum(be[:, 0, :], cmpb[:], AX.X), [cmpb], [be])
                k.ts('dve', be[:, 1, :], be[:, 0, :], float(NE) - 0.5, BIG, ALU.is_gt, ALU.mult, [be], [be])
                k.ts('dve', be[:, 0, :], be[:, 0, :], float(NE - 1), None, ALU.min, None, [be], [be])
                iw0 = k.sb([128, 8], F32, 'iw0')
                k.op('pool', lambda e: e.iota(iw0[:], [[128, 8]], base=l * NE * 1024, channel_multiplier=1,
                                              allow_small_or_imprecise_dtypes=True), [], [iw0])
                k.ts('dve', be[:, 2, :], be[:, 0, :], 1024.0, None, ALU.mult, None, [be], [be])
                k.tt('dve', be[:, 2, :], be[:, 2, :], be[:, 1, :], ALU.add, [be], [be])
                iwf = k.sb([128, NB, 8], F32, 'iwf')
                k.tt('dve', iwf[:], be[:, 2, :].unsqueeze(2).to_broadcast([128, NB, 8]),
                     iw0[:].unsqueeze(1).to_broadcast([128, NB, 8]), ALU.add, [be, iw0], [iwf])
                k.copy('dve', IW[:], iwf[:], [iwf], [IW])
                ib0 = k.sb([16, 1], F32, 'ib0')
                k.op('pool', lambda e: e.iota(ib0[:], [[0, 1]], base=l * NE * 16, channel_multiplier=1,
                                              allow_small_or_imprecise_dtypes=True), [], [ib0])
                ibf = k.sb([16, NB], F32, 'ibf')
                k.ts('dve', ibf[:], be[0:16, 0, :], 16.0, ib0[:, 0:1], ALU.mult, ALU.add, [be, ib0], [ibf])
                k.tt('dve', ibf[:], ibf[:], be[0:16, 1, :], ALU.add, [ibf, be], [ibf])
                k.copy('dve', IB[:], ibf[:], [ibf], [IB])
                zt = k.sb([128, (NR // 128) * 16], I32, 'zt')
                k.memset('pool', zt, zt[:], 0)
                k.dma('sp', RT[:, :].rearrange("(p n) c -> p (n c)", p=128), zt[:], [zt], RT)
                RTS = T(RT.t)
                Dm = k.sb([128, NE], F32, 'Dm')
                d8 = k.sb([128, 8], F32, 'd8')
                eq = k.sb([128, NE], F32, 'eq')
                tok = k.sb([128, 16], I32, 'tok')
                for ti in range(NT):
                    k.tt('dve', Dm[:], rank[:, ti, :], pad[:, 2, :], ALU.add, [rank, pad], [Dm])
                    k.ts('dve', Dm[:], Dm[:], 1.0, None, ALU.add, None, [Dm], [Dm])
                    k.tt('dve', Dm[:], Dm[:], Mk[:, ti, :], ALU.mult, [Dm, Mk], [Dm])
                    k.ts('dve', Dm[:], Dm[:], -1.0, None, ALU.add, None, [Dm], [Dm])
                    k.op('dve', lambda e: e.max(d8[:], Dm[:]), [Dm], [d8])
                    k.copy('dve', dsel[:, ti, :], d8[:, 0:4], [d8], [dsel])
                    for kk_ in range(4):
                        k.ts('dve', eq[:], Dm[:], d8[:, kk_:kk_ + 1], None, ALU.is_equal, None, [Dm, d8], [eq])
                        k.tt('dve', eq[:], eq[:], G[:, ti, :], ALU.mult, [eq, G], [eq])
                        k.op('dve', lambda e, kk_=kk_: e.reduce_sum(gsel[:, ti, kk_:kk_ + 1], eq[:], AX.X), [eq], [gsel])
                    k.op('pool', lambda e, ti=ti: e.iota(tok[:], [[0, 16]], base=ti * 128, channel_multiplier=1), [], [tok])
                    for kk_ in range(4):
                        k.idma(RTS, RT[:, :], tok, tok[:], dsel, dsel[:, ti, kk_:kk_ + 1], scatter=True, stream=True, extra_reads=[RT])
            with k.phase():
                NW = 16
                w1p = [k.sb([128, 2 * DFF], BF16, 'w1p') for _ in range(NW)]
                w2p = [k.sb([128, D], BF16, 'w2p') for _ in range(NW)]
                xia = k.sb([128, NB, NTB], I32, 'xia')
                k.dma('sp', xia[:], RT[:, 0:1].rearrange("(b i p) c -> p b (i c)", p=128, i=NTB), [RT, RTS], xia,
                      allow_slow_non_contiguous=True)
                xgs = [k.sb([128, D], F32, 'xg') for _ in range(4)]
                xTs = [k.sb([128, 8, BLK], BF16, 'xT') for _ in range(2)]
                aTs = [k.sb([128, 8, BLK], BF16, 'aT') for _ in range(2)]
                b1gs = [k.sb([16, 128], F32, 'b1g') for _ in range(2)]
                b1bs = [k.sb([128, 16], F32, 'b1b') for _ in range(2)]
                ysb = [k.sb([128, D], F32, 'ysb') for _ in range(2)]
                g0s = [k.sb([128, BLK], F32, 'g0') for _ in range(2)]
                sgs = [k.sb([128, BLK], F32, 'sg') for _ in range(2)]
                u0s = [k.sb([128, BLK], F32, 'u0') for _ in range(2)]
                NBX = NB
                wi = 0
                ei = 0
                xgi = 0
                yi = 0
                lo_, hi_ = list(range(0, NE)), list(range(NE, NB))
                order = []
                while lo_ or hi_:
                    if lo_:
                        order.append(lo_.pop(0))
                    if hi_:
                        order.append(hi_.pop(0))
                for bi_, b in enumerate(order[:NBX]):
                    xT, aT, b1g, b1b = xTs[bi_ % 2], aTs[bi_ % 2], b1gs[bi_ % 2], b1bs[bi_ % 2]
                    xg4 = []
                    for i in range(NTB):
                        xg = xgs[xgi % 4]
                        xgi += 1
                        k.idma(xg, xg[:], H, H[:, :], xia, xia[:, b, i:i + 1])
                        xg4.append(xg)
                    k.idma(b1g, b1g[:], self.inp['moe_b1'], b1v, IB, IB[:, b:b + 1], bounds=self.reg_b)
                    w1k, w2k = [], []
                    for kc in range(8):
                        w = w1p[wi % NW]
                        k.idma(w, w[:], self.inp['moe_w1'], w1v, IW, IW[:, b, kc:kc + 1], bounds=self.reg_w)
                        w1k.append(w)
                        wi += 1
                    wi -= 8
                    for kc in range(8):
                        w = w2p[wi % NW]
                        k.idma(w, w[:], self.inp['moe_w2'], w2v, IW, IW[:, b, kc:kc + 1], bounds=self.reg_w)
                        w2k.append(w)
                        wi += 1
                    for i in range(NTB):
                        xg = xg4[i]
                        for half in range(2):
                            bb = self.bank()
                            for j in range(4):
                                c = half * 4 + j
                                k.tr(bb[:, j * 128:(j + 1) * 128], xg[:, c * 128:(c + 1) * 128], self.ident[:],
                                     [xg, self.ident], [bb], inc=(j == 3))
                            k.copy('act' if half else 'dve', xT[:, half * 4:(half + 1) * 4, i * 128:(i + 1) * 128],
                                   bb[:].rearrange("p (c t) -> p c t", c=4), [bb], [xT])
                    bb = self.bank()
                    k.tr(bb[:, 0:16], b1g[:], self.ident[0:16, 0:16], [b1g, self.ident], [bb])
                    k.copy('dve', b1b[:], bb[:, 0:16], [bb], [b1b])
                    for p in range(8):
                        bgp, bup = self.bank(), self.bank()
                        for gu, bb in ((0, bgp), (1, bup)):
                            c0 = gu * DFF + p * 128
                            for kc in range(8):
                                k.mm(bb[:, 0:BLK], w1k[kc][:, c0:c0 + 128], xT[:, kc, :], [w1k[kc], xT], [bb],
                                     start=(kc == 0), stop=(kc == 7), inc=(kc == 7))
                        g0, sg, u0 = g0s[ei % 2], sgs[ei % 2], u0s[ei % 2]
                        ei += 1
                        k.act(g0[:], bgp[:, 0:BLK], AF.Identity, [bgp, b1b], [g0], bias=b1b[:, p:p + 1], scale=1.0)
                        k.ts('dve', g0[:], g0[:], 7.0, None, ALU.min, None, [g0], [g0])
                        k.act(sg[:], g0[:], AF.Sigmoid, [g0], [sg], scale=1.702)
                        k.tt('dve', sg[:], sg[:], g0[:], ALU.mult, [sg, g0], [sg])
                        k.act(u0[:], bup[:, 0:BLK], AF.Identity, [bup, b1b], [u0], bias=b1b[:, 8 + p:9 + p], scale=1.0)
                        k.ts('dve', u0[:], u0[:], 7.0, -7.0, ALU.min, ALU.max, [u0], [u0])
                        k.stt('dve', aT[:, p, :], u0[:], 1.0, sg[:], ALU.add, ALU.mult, [u0, sg], [aT])
                    for i in range(NTB):
                        bks = [self.bank(), self.bank()]
                        for hf in range(2):
                            for fc in range(8):
                                k.mm(bks[hf][:], aT[:, fc, i * 128:(i + 1) * 128], w2k[fc][:, hf * 512:(hf + 1) * 512],
                                     [aT, w2k[fc]], [bks[hf]], start=(fc == 0), stop=(fc == 7), inc=(fc == 7))
                        y = ysb[yi % 2]
                        yi += 1
                        k.copy('act', y[:, 0:512], bks[0][:], [bks[0]], [y])
                        k.copy('act', y[:, 512:1024], bks[1][:], [bks[1]], [y])
                        k.dma('sp', YB[b * BLK + i * 128:b * BLK + (i + 1) * 128, :], y[:], [y], YB, stream=True)
            with k.phase():
                g = self.bc_load(self.inp['ln3_g'], self.inp['ln3_g'][l], D)
                bt_ = self.bc_load(self.inp['ln3_b'], self.inp['ln3_b'][l], D)
                zs = [k.sb([128, D], F32, 'z') for _ in range(2)]
                ygs = [k.sb([128, D], F32, 'yg') for _ in range(8)]
                GTs = [k.sb([NE, 128], F32, 'GT') for _ in range(2)]
                gi = 0
                for ti in range(NT):
                    z = zs[ti % 2]
                    GT = GTs[ti % 2]
                    k.dma('sp', z[:], H[ti * 128:(ti + 1) * 128, :], [H], z)
                    bg = self.bank()
                    k.tr(bg[0:NE, 0:128], G[:, ti, :], self.ident[:], [G, self.ident], [bg])
                    k.copy('act', GT[:], bg[0:NE, 0:128], [bg], [GT])
                    for hf in range(2):
                        bb = self.bank()
                        k.mm(bb[:], GT[:], b2[:, hf * 512:(hf + 1) * 512], [GT, b2], [bb])
                        k.stt('dve', z[:, hf * 512:(hf + 1) * 512], z[:, hf * 512:(hf + 1) * 512], DN_ALPHA, bb[:],
                              ALU.mult, ALU.add, [z, bb], [z])
                    for kk_ in range(4):
                        yg = ygs[gi % 8]
                        gi += 1
                        k.idma(yg, yg[:], YB, YB[:, :], dsel, dsel[:, ti, kk_:kk_ + 1])
                        k.stt('dve', z[:], yg[:], gsel[:, ti, kk_:kk_ + 1], z[:], ALU.mult, ALU.add, [yg, gsel, z], [z])
                    self.ln_tile(z, g, bt_, ti, H, out_dram=final_out, store_q='sp')

    def build(self):
        k = self.k
        self.consts()
        self.alloc_hT()
        H = self.scratch('H', [S, D])
        self.phase_ln0(H)
        if self.upto <= 0:
            self.finish(H)
            return self.nc
        for l in range(DEPTH):
            IF = self.scratch(f'IF{l}', [8, S])
            PT = self.scratch(f'PT{l}', [S, 1024 + R_COLS])
            MIX = self.scratch(f'MIX{l}', [S, 1024])
            with k.phase():
                qkT = k.sb([128, 8, S], BF16, 'qkT')
                self.phase_mixproj(l, qkT, IF, PT)
                if self.upto == 1 + 10 * l:
                    qd = self.scratch('QK', [1024, S], BF16)
                    for c in range(8):
                        k.dma('sp', qd[c * 128:(c + 1) * 128, :], qkT[:, c, :], [qkT], qd)
                    self.finish(H)
                    return self.nc
                self.phase_mlstm(l, qkT, IF, PT, MIX)
                if self.upto == 2 + 10 * l:
                    self.finish(H)
                    return self.nc
            self.free_hT()
            self.phase_rwkv(l, PT, MIX)
            self.alloc_hT()
            if self.upto == 3 + 10 * l:
                self.finish(H)
                return self.nc
            with k.phase():
                xpre = self.xattn_preload(l)
                self.phase_outproj(l, MIX, H)
                self.phase_xattn(l, H, pre=xpre)
            if self.upto == 5 + 10 * l:
                self.finish(H)
                return self.nc
            last = (l == DEPTH - 1)
            if MOE_SPARSE:
                if l == 0:
                    self.YB = self.scratch('YB', [MOE_NB * MOE_BLK, D])
                    self.RT = self.scratch('RT', [MOE_NB * MOE_BLK, 16], mybir.dt.int32)
                self.phase_moe_sparse(l, H, self.YB, self.RT, final_out=(self.out if last else None))
            else:
                self.phase_moe(l, H, final_out=(self.out if last else None))
            if self.upto == 6 + 10 * l and not last:
                self.finish(H)
                return self.nc
        k.barrier()
        return self.nc

    def finish(self, H):
        k = self.k
        with k.phase():
            z = k.sb([128, D], F32, 'fz')
            for ti in range(NT):
                k.dma('sp', z[:], H[ti * 128:(ti + 1) * 128, :], [H], z)
                k.dma('sp', self.out[ti * 128:(ti + 1) * 128, :], z[:], [z], self.out)
        k.barrier()


def make_inputs(inputs, b):
    m = {'x': np.ascontiguousarray(inputs['x'][b]), 'mem': np.ascontiguousarray(inputs['mem'][b])}
    for n, _ in WEIGHT_SPECS:
        m[n] = np.ascontiguousarray(inputs[n])
    return m


def kernel(**inputs):
    inputs = {k_: np.asarray(v) for k_, v in inputs.items()}
    prog = Prog()
    nc = prog.build()
    in_maps = [make_inputs(inputs, b) for b in range(8)]
    res = run_bass_kernel_spmd(nc, in_maps, core_ids=list(range(8)))
    return np.stack([np.asarray(r['out']) for r in res.results], axis=0).astype(np.float32)
```

```python
import numpy as np
from contextlib import ExitStack, contextmanager
import concourse.bass as bass
import concourse.mybir as mybir
from concourse.bass_utils import run_bass_kernel_spmd

F32 = mybir.dt.float32
BF16 = mybir.dt.bfloat16
AF = mybir.ActivationFunctionType
ALU = mybir.AluOpType
AX = mybir.AxisListType

S = 2048
D = 1024
NT = S // 128
DEPTH = 2
M_W = 512
M_H = 4
M_HD = 128
R_W = 512
R_H = 8
R_HD = 64
IN_COLS = 3848
M_COLS = 2056
R_COLS = 1792
NM = 256
NE = 32
DFF = 1024
DN_ALPHA = float((2 * DEPTH) ** 0.25)
LN_EPS = 1e-5
MOE_SPARSE = True
MOE_BLK = 384
MOE_NB = (S * 4 + MOE_BLK - 1) // MOE_BLK + NE

WEIGHT_SPECS = [
    ('ln0_g', (1024,)), ('ln0_b', (1024,)), ('w_in', (2, 1024, 3848)), ('m_conv_w', (2, 4, 1024)),
    ('m_conv_b', (2, 1024)), ('m_ig_b', (2, 4)), ('m_fg_b', (2, 4)), ('m_norm_g', (2, 512)),
    ('r_mu', (2, 1792)), ('r_w0', (2, 512)), ('r_w2', (2, 64, 512)), ('r_a0', (2, 512)),
    ('r_a2', (2, 64, 512)), ('r_g2', (2, 128, 512)), ('r_kk', (2, 512)), ('r_ka', (2, 512)),
    ('r_rk', (2, 512)), ('r_gn_g', (2, 512)), ('r_gn_b', (2, 512)), ('w_out', (2, 1024, 1024)),
    ('ln1_g', (2, 1024)), ('ln1_b', (2, 1024)), ('x_wq', (2, 1024, 1024)), ('x_wkv', (2, 1024, 2048)),
    ('x_wo', (2, 1024, 1024)), ('ln2_g', (2, 1024)), ('ln2_b', (2, 1024)), ('moe_wr', (2, 1024, 32)),
    ('moe_br', (2, 32)), ('moe_w1', (2, 32, 1024, 2048)), ('moe_b1', (2, 32, 2048)),
    ('moe_w2', (2, 32, 1024, 1024)), ('moe_b2', (2, 32, 1024)), ('ln3_g', (2, 1024)), ('ln3_b', (2, 1024)),
]


class T:
    _n = 0

    def __init__(self, t, key=None, excl=False):
        self.t = t
        self.excl = excl
        T._n += 1
        self.key = key if key is not None else ('t', T._n)

    def __getitem__(self, idx):
        return self.t[idx]


class K:
    def __init__(self, nc):
        self.nc = nc
        self.es = ExitStack()
        self.engs = {'pe': nc.tensor, 'act': nc.scalar, 'dve': nc.vector, 'pool': nc.gpsimd, 'sp': nc.sync}
        self.sems = {n: self.es.enter_context(nc.semaphore('s_' + n)) for n in self.engs}
        self.cnt = {n: 0 for n in self.engs}
        self.seen = {n: {} for n in self.engs}
        self.res = {}
        self.dsem = {}
        self.ssem = {}
        self.sempool = []
        self.allsems = {('e', n): (self.sems[n], 0) for n in self.engs}
        self.stack = [self.es]
        self.uid = 0

    @contextmanager
    def phase(self):
        es = ExitStack()
        self.stack.append(es)
        try:
            yield
        finally:
            self.barrier()
            self.stack.pop()
            es.close()

    def name(self, p):
        self.uid += 1
        return f"{p}_{self.uid}"

    def sb(self, shape, dt=F32, name='sb'):
        return T(self.stack[-1].enter_context(self.nc.sbuf_tensor(self.name(name), list(shape), dt)))

    def ps(self, shape, dt=F32, name='ps'):
        return T(self.stack[-1].enter_context(self.nc.psum_tensor(self.name(name), list(shape), dt)), excl=True)

    def dram(self, name, shape, dt=F32, kind='Internal'):
        return T(self.nc.dram_tensor(name, list(shape), dt, kind=kind).ap())

    def _deps(self, reads, writes):
        deps = []
        for r in reads:
            e = self.res.get(r.key)
            if e:
                if e[0]:
                    deps.append(e[0])
                deps.extend(e[2].values())
        for w in writes:
            e = self.res.get(w.key)
            if e:
                if e[0]:
                    deps.append(e[0])
                deps.extend(e[1].values())
                deps.extend(e[2].values())
        return deps

    def _wait(self, en, deps):
        best = {}
        for (sk, sh, v) in deps:
            if en == 'pe' and sk == ('e', 'pe'):
                continue
            if self.seen[en].get(sk, 0) >= v:
                continue
            if sk not in best or best[sk][1] < v:
                best[sk] = (sh, v)
        for sk, (sh, v) in best.items():
            self.engs[en].wait_ge(sh, v)
            self.seen[en][sk] = v

    def _record(self, ev, reads, writes, stream=False):
        for r in reads:
            e = self.res.setdefault(r.key, [None, {}, {}])
            old = e[1].get(ev[0])
            if old is None or old[2] < ev[2]:
                e[1][ev[0]] = ev
        for w in writes:
            if stream:
                self.res.setdefault(w.key, [None, {}, {}])[2][ev[0]] = ev
            else:
                self.res[w.key] = [ev, {}, {}]

    NSTREAM = 4

    def _dsem(self, key, stream):
        def new():
            if self.sempool:
                return self.sempool.pop()
            return [self.es.enter_context(self.nc.semaphore(self.name('sd'))), 0]
        if not stream:
            if key not in self.dsem:
                self.dsem[key] = new()
            return self.dsem[key], None
        st = self.ssem.setdefault(key, [[], 0])
        if len(st[0]) < self.NSTREAM:
            st[0].append(new())
        ds = st[0][st[1] % len(st[0])] if len(st[0]) == self.NSTREAM else st[0][-1]
        st[1] += 1
        prev = (('d', id(ds)), ds[0], ds[1]) if ds[1] > 0 else None
        return ds, prev

    def op(self, en, fn, reads=(), writes=(), inc=True):
        writes = list(writes) + [r for r in reads if r.excl]
        reads = [r for r in reads if not r.excl]
        self._wait(en, self._deps(reads, writes))
        ins = fn(self.engs[en])
        sk = ('e', en)
        ev = (sk, self.sems[en], self.cnt[en] + 1)
        if inc:
            self.cnt[en] += 1
            ins.then_inc(self.sems[en], 1)
            self.allsems[sk] = (self.sems[en], self.cnt[en])
        self._record(ev, reads, writes)
        return ins

    def dma(self, en, out, in_, reads, write, stream=False, **kw):
        ds, prev = self._dsem(write.key, stream)
        deps = self._deps(reads, [] if stream else [write])
        if prev is not None:
            deps.append(prev)
        self._wait(en, deps)
        ds[1] += 16
        self.engs[en].dma_start(out=out, in_=in_, **kw).then_inc(ds[0], 16)
        sk = ('d', id(ds))
        ev = (sk, ds[0], ds[1])
        self.allsems[sk] = (ds[0], ds[1])
        self._record(ev, reads, [write], stream=stream)

    def idma(self, out_t, out_ap, in_t, in_ap, idx_t, idx_ap, scatter=False, bounds=None, stream=False, extra_reads=()):
        en = 'pool'
        reads = [in_t, idx_t] + list(extra_reads)
        ds, prev = self._dsem(out_t.key, stream)
        deps = self._deps(reads, [] if stream else [out_t])
        if prev is not None:
            deps.append(prev)
        self._wait(en, deps)
        ds[1] += 16
        off = bass.IndirectOffsetOnAxis(ap=idx_ap, axis=0)
        kw = {}
        if bounds is not None:
            kw = dict(bounds_check=bounds, oob_is_err=False)
        if scatter:
            ins = self.nc.gpsimd.indirect_dma_start(out=out_ap, out_offset=off, in_=in_ap, in_offset=None, **kw)
        else:
            ins = self.nc.gpsimd.indirect_dma_start(out=out_ap, out_offset=None, in_=in_ap, in_offset=off, **kw)
        ins.then_inc(ds[0], 16)
        sk = ('d', id(ds))
        ev = (sk, ds[0], ds[1])
        self.allsems[sk] = (ds[0], ds[1])
        self._record(ev, reads, [out_t], stream=stream)

    def barrier(self):
        for en in self.engs:
            deps = [(sk, sh, v) for sk, (sh, v) in self.allsems.items() if v > 0 and sk != ('e', en)]
            self._wait(en, deps)
        self.res = {}
        for ds in list(self.dsem.values()) + [d for st in self.ssem.values() for d in st[0]]:
            self.sempool.append(ds)
            self.allsems.pop(('d', id(ds)), None)
        self.dsem = {}
        self.ssem = {}

    def mm(self, out, lhsT, rhs, reads, writes, start=True, stop=True, inc=True):
        return self.op('pe', lambda e: e.matmul(out, lhsT, rhs, start=start, stop=stop), reads, writes, inc)

    def tr(self, out, in_, ident, reads, writes, inc=True):
        return self.op('pe', lambda e: e.transpose(out, in_, ident), reads, writes, inc)

    def act(self, out, in_, func, reads, writes, bias=0.0, scale=1.0, accum_out=None):
        if accum_out is None:
            return self.op('act', lambda e: e.activation(out, in_, func, bias=bias, scale=scale), reads, writes)
        return self.op('act', lambda e: e.activation(out, in_, func, bias=bias, scale=scale,
                                                     accum_out=accum_out), reads, writes)

    def tt(self, en, out, in0, in1, op, reads, writes):
        return self.op(en, lambda e: e.tensor_tensor(out, in0, in1, op), reads, writes)

    def ts(self, en, out, in0, s1, s2, op0, op1, reads, writes):
        if s2 is None:
            return self.op(en, lambda e: e.tensor_scalar(out, in0, s1, None, op0), reads, writes)
        return self.op(en, lambda e: e.tensor_scalar(out, in0, s1, s2, op0, op1), reads, writes)

    def stt(self, en, out, in0, scalar, in1, op0, op1, reads, writes):
        en = 'dve'
        return self.op(en, lambda e: e.scalar_tensor_tensor(out, in0, scalar, in1, op0, op1), reads, writes)

    def copy(self, en, out, in_, reads, writes):
        if en == 'act':
            return self.op('act', lambda e: e.copy(out, in_), reads, writes)
        return self.op(en, lambda e: e.tensor_copy(out, in_), reads, writes)

    def memset(self, en, t, ap, val):
        return self.op(en, lambda e: e.memset(ap, val), (), [t])


class Prog:
    def __init__(self, dbg=(), upto=99):
        self.nc = bass.Bass("TRN2", target_bir_lowering=False)
        self.k = K(self.nc)
        self.dbg = set(dbg)
        self.upto = upto
        nc = self.nc
        self.inp = {}
        self.inp['x'] = T(nc.dram_tensor('x', [S, D], F32, kind='ExternalInput').ap())
        self.inp['mem'] = T(nc.dram_tensor('mem', [NM, D], F32, kind='ExternalInput').ap())
        for n, shp in WEIGHT_SPECS:
            self.inp[n] = T(nc.dram_tensor(n, list(shp), F32, kind='ExternalInput').ap())
        self.out = T(nc.dram_tensor('out', [S, D], F32, kind='ExternalOutput').ap())

    def scratch(self, name, shape, dt=F32):
        kind = 'ExternalOutput' if name in self.dbg else 'Internal'
        return self.k.dram(name, shape, dt, kind=kind)

    def consts(self):
        k = self.k
        self.reg_w = self.nc.gpsimd.to_reg(2 * NE * 1024 - 1)
        self.reg_b = self.nc.gpsimd.to_reg(2 * NE * 16 - 1)
        self.ident = k.sb([128, 128], F32, 'ident')
        k.memset('pool', self.ident, self.ident[:], 1.0)
        k.op('pool', lambda e: e.affine_select(self.ident[:], self.ident[:], [[-1, 128]], ALU.is_equal, 0.0,
                                               base=0, channel_multiplier=1), [self.ident], [self.ident])
        self.identb = k.sb([128, 128], BF16, 'identb')
        k.copy('pool', self.identb[:], self.ident[:], [self.ident], [self.identb])
        self.eps_ln = k.sb([128, 1], F32, 'epsln')
        k.memset('pool', self.eps_ln, self.eps_ln[:], LN_EPS)
        self.one_c = k.sb([128, 1], F32, 'onec')
        k.memset('pool', self.one_c, self.one_c[:], 1.0)
        self.mask_le = k.sb([128, 128], F32, 'maskle')
        k.memset('pool', self.mask_le, self.mask_le[:], 1.0)
        k.op('pool', lambda e: e.affine_select(self.mask_le[:], self.mask_le[:], [[1, 128]], ALU.is_ge, 0.0,
                                               base=0, channel_multiplier=-1), [self.mask_le], [self.mask_le])
        self.pb = [k.ps([128, 512], F32, 'bank') for _ in range(8)]
        self.pbi = 0
        self.hT = None
        self.hT_es = None
        k.barrier()

    def alloc_hT(self):
        k = self.k
        self.hT_es = ExitStack()
        self.hT = T(self.hT_es.enter_context(self.nc.sbuf_tensor(k.name('hT'), [128, 8, S + 1], BF16)))
        k.memset('pool', self.hT, self.hT[:, :, 0:1], 0.0)

    def free_hT(self):
        self.hT_es.close()
        self.hT = None

    def bank(self):
        b = self.pb[self.pbi % 8]
        self.pbi += 1
        return b

    def bc_load(self, dram_t, src_ap, n, name='bc'):
        k = self.k
        t = k.sb([128, n], F32, name)
        k.dma('sp', t[:], src_ap.partition_broadcast(128), [dram_t], t)
        return t

    def ln_tile(self, z, g_bc, b_bc, ti, H, out_dram=None, defer=False, store_q='pool'):
        k = self.k
        st = k.sb([128, 2, 6], F32, 'lnst')
        mv = k.sb([128, 2], F32, 'lnmv')
        for c in range(2):
            k.op('dve', lambda e, c=c: e.bn_stats(st[:, c, :], z[:, c * 512:(c + 1) * 512]), [z], [st])
        k.op('dve', lambda e: e.bn_aggr(mv[:], st[:].rearrange("p a b -> p (a b)")), [st], [mv])
        rstd = k.sb([128, 1], F32, 'lnr')
        k.act(rstd[:], mv[:, 1:2], AF.Sqrt, [mv, self.eps_ln], [rstd], bias=self.eps_ln[:], scale=1.0)
        k.op('dve', lambda e: e.reciprocal(rstd[:], rstd[:]), [rstd], [rstd])
        k.ts('dve', z[:], z[:], mv[:, 0:1], rstd[:], ALU.subtract, ALU.mult, [z, mv, rstd], [z])
        k.tt('dve', z[:], z[:], g_bc[:], ALU.mult, [z, g_bc], [z])
        k.tt('dve', z[:], z[:], b_bc[:], ALU.add, [z, b_bc], [z])
        tgt = out_dram if out_dram is not None else H
        k.dma(store_q, tgt[ti * 128:(ti + 1) * 128, :], z[:], [z], tgt, stream=True)
        if out_dram is None:
            if defer:
                return lambda: self.to_hT(z, ti)
            self.to_hT(z, ti)
        return None

    def to_hT(self, z, ti):
        k = self.k
        for half in range(2):
            b = self.bank()
            for j in range(4):
                c = half * 4 + j
                k.tr(b[:, j * 128:(j + 1) * 128], z[:, c * 128:(c + 1) * 128], self.ident[:], [z, self.ident], [b],
                     inc=(j == 3))
            en = 'act' if half == 0 else 'dve'
            k.copy(en, self.hT[:, half * 4:(half + 1) * 4, 1 + ti * 128:1 + (ti + 1) * 128],
                   b[:].rearrange("p (c t) -> p c t", c=4), [b], [self.hT])

    def phase_ln0(self, H):
        k = self.k
        with k.phase():
            g = self.bc_load(self.inp['ln0_g'], self.inp['ln0_g'][:], D)
            b = self.bc_load(self.inp['ln0_b'], self.inp['ln0_b'][:], D)
            zs = [k.sb([128, D], F32, 'z') for _ in range(2)]
            for ti in range(NT):
                z = zs[ti % 2]
                k.dma('sp', z[:], self.inp['x'][ti * 128:(ti + 1) * 128, :], [self.inp['x']], z)
                self.ln_tile(z, g, b, ti, H)

    def load_w(self, src_t, ap, n, name='w', dt=BF16):
        k = self.k
        wt = k.sb([128, 8, n], dt, name)
        eng = 'pool' if dt != F32 else 'sp'
        k.dma(eng, wt[:], ap.rearrange("(c p) n -> p c n", p=128), [src_t], wt)
        return wt

    def phase_mixproj(self, l, qkT, IF, PT):
        k = self.k
        w_in = self.inp['w_in']
        with k.phase():
            wqk = self.load_w(w_in, w_in[l, :, 0:1024], 1024, 'wqk')
            cw = k.sb([128, 4, 8], F32, 'cw')
            for j in range(4):
                k.dma('sp', cw[:, j, :], self.inp['m_conv_w'][l, j].rearrange("(c p) -> p c", p=128),
                      [self.inp['m_conv_w']], cw, allow_slow_non_contiguous=True)
            cb = k.sb([128, 8], F32, 'cb')
            k.dma('sp', cb[:], self.inp['m_conv_b'][l].rearrange("(c p) -> p c", p=128), [self.inp['m_conv_b']], cb,
                  allow_slow_non_contiguous=True)
            raws = [k.sb([128, S + 3], F32, 'raw') for _ in range(2)]
            accs = [k.sb([128, S], F32, 'acc') for _ in range(2)]
            for r in raws:
                k.memset('pool', r, r[:, 0:3], 0.0)
            for c in range(8):
                raw = raws[c % 2]
                acc = accs[c % 2]
                for tb in range(4):
                    b = self.bank()
                    for kc in range(8):
                        k.mm(b[:], wqk[:, kc, c * 128:(c + 1) * 128], self.hT[:, kc, 1 + tb * 512:1 + (tb + 1) * 512],
                             [wqk, self.hT], [b], start=(kc == 0), stop=(kc == 7), inc=(kc == 7))
                    k.copy('act', raw[:, 3 + tb * 512:3 + (tb + 1) * 512], b[:], [b], [raw])
                en = 'dve' if c % 2 == 0 else 'pool'
                k.ts(en, acc[:], raw[:, 3:3 + S], cw[:, 3, c:c + 1], cb[:, c:c + 1], ALU.mult, ALU.add, [raw, cw, cb], [acc])
                for j in range(3):
                    k.stt(en, acc[:], raw[:, j:j + S], cw[:, j, c:c + 1], acc[:], ALU.mult, ALU.add, [raw, cw, acc], [acc])
                k.act(acc[:], acc[:], AF.Silu, [acc], [acc])
                k.ts(en, qkT[:, c, :], acc[:], (1.0 if c < 4 else float(M_HD ** -0.5)), None, ALU.mult, None, [acc], [qkT])
            wif = self.load_w(w_in, w_in[l, :, 2048:2056], 8, 'wif')
            gb = k.sb([8, 1], F32, 'gb')
            k.dma('sp', gb[0:4, :], self.inp['m_ig_b'][l].rearrange("(a b) -> a b", b=1), [self.inp['m_ig_b']], gb)
            k.dma('sp', gb[4:8, :], self.inp['m_fg_b'][l].rearrange("(a b) -> a b", b=1), [self.inp['m_fg_b']], gb)
            ifs = k.sb([8, S], F32, 'ifs')
            for tb in range(4):
                b = self.bank()
                for kc in range(8):
                    k.mm(b[0:8, :], wif[:, kc, :], self.hT[:, kc, 1 + tb * 512:1 + (tb + 1) * 512],
                         [wif, self.hT], [b], start=(kc == 0), stop=(kc == 7), inc=(kc == 7))
                k.ts('dve', ifs[:, tb * 512:(tb + 1) * 512], b[0:8, :], gb[:, 0:1], None, ALU.add, None, [b, gb], [ifs])
            k.dma('sp', IF[:, :], ifs[:], [ifs], IF)
            mu = self.bc_load(self.inp['r_mu'], self.inp['r_mu'][l], R_COLS, 'mu')
            groups = [(1024, 512, 0, False), (1536, 512, 512, False)]
            off = 0
            while off < R_COLS:
                n = min(512, R_COLS - off)
                groups.append((M_COLS + off, n, 1024 + off, True))
                off += n
            outs = [k.sb([128, 512], F32, 'pto') for _ in range(3)]
            oi = 0
            for (c0, n, o0, shifted) in groups:
                with k.phase():
                    if not shifted:
                        wc = self.load_w(w_in, w_in[l, :, c0:c0 + n], n, 'wg')
                        wp = None
                    else:
                        wf = self.load_w(w_in, w_in[l, :, c0:c0 + n], n, 'wf', dt=F32)
                        wp = k.sb([128, 8, n], BF16, 'wp')
                        wc = k.sb([128, 8, n], BF16, 'wc')
                        tmp = k.sb([128, 8, n], F32, 'wtmp')
                        r0 = c0 - M_COLS
                        for kc in range(8):
                            en = 'dve' if kc % 2 == 0 else 'pool'
                            k.tt(en, tmp[:, kc, :], wf[:, kc, :], mu[:, r0:r0 + n], ALU.mult, [wf, mu], [tmp])
                            k.copy('act', wp[:, kc, :], tmp[:, kc, :], [tmp], [wp])
                            k.tt(en, wc[:, kc, :], wf[:, kc, :], tmp[:, kc, :], ALU.subtract, [wf, tmp], [wc])
                    for ti in range(NT):
                        b = self.bank()
                        nmm = 16 if shifted else 8
                        i = 0
                        for kc in range(8):
                            k.mm(b[:, 0:n], self.hT[:, kc, 1 + ti * 128:1 + (ti + 1) * 128], wc[:, kc, :],
                                 [self.hT, wc], [b], start=(i == 0), stop=(i == nmm - 1), inc=(i == nmm - 1))
                            i += 1
                        if shifted:
                            for kc in range(8):
                                k.mm(b[:, 0:n], self.hT[:, kc, ti * 128:(ti + 1) * 128], wp[:, kc, :],
                                     [self.hT, wp], [b], start=False, stop=(i == nmm - 1), inc=(i == nmm - 1))
                                i += 1
                        o = outs[oi % 3]
                        oi += 1
                        k.copy('act' if ti % 2 == 0 else 'dve', o[:, 0:n], b[:, 0:n], [b], [o])
                        k.dma('pool', PT[ti * 128:(ti + 1) * 128, o0:o0 + n], o[:, 0:n], [o], PT, stream=True)

    def phase_mlstm(self, l, qkT, IF, PT, MIX):
        k = self.k
        NCk = 16
        sc = self.scratch(f'msc{l}', [8, 64])
        with k.phase():
            Gi = k.sb([64, 128], F32, 'Gi')
            Gf = k.sb([64, 128], F32, 'Gf')
            k.dma('sp', Gi[:], IF[0:4, :].rearrange("h (c p) -> (h c) p", p=128), [IF], Gi)
            k.dma('sp', Gf[:], IF[4:8, :].rearrange("h (c p) -> (h c) p", p=128), [IF], Gf)
            ones = k.sb([64, 128], F32, 'ones')
            k.memset('pool', ones, ones[:], 1.0)
            k.act(Gf[:], Gf[:], AF.Exp, [Gf], [Gf], scale=-1.0)
            k.act(Gf[:], Gf[:], AF.Ln, [Gf, self.one_c], [Gf], bias=self.one_c[0:64, :], scale=1.0)
            csp = k.sb([64, 128], F32, 'csp')
            k.op('dve', lambda e: e.tensor_tensor_scan(csp[:], ones[:], Gf[:], 0.0, ALU.mult, ALU.add), [ones, Gf], [csp])
            u = k.sb([64, 128], F32, 'u')
            k.tt('dve', u[:], Gi[:], csp[:], ALU.add, [Gi, csp], [u])
            col = k.sb([64, 2], F32, 'col')
            k.op('dve', lambda e: e.reduce_max(col[:, 0:1], u[:], AX.X), [u], [col])
            k.ts('dve', col[:, 1:2], csp[:, 127:128], -1.0, None, ALU.mult, None, [csp], [col])
            k.dma('sp', sc[0, :].rearrange("(a b) -> a b", b=1), col[:, 0:1], [col], sc)
            k.dma('sp', sc[1, :].rearrange("(a b) -> a b", b=1), col[:, 1:2], [col], sc)
            hm = k.sb([4, 2, 16], F32, 'hm')
            k.dma('sp', hm[:, 0, :], sc[0, :].rearrange("(h c) -> h c", c=16), [sc], hm)
            k.dma('sp', hm[:, 1, :], sc[1, :].rearrange("(h c) -> h c", c=16), [sc], hm)
            mn = k.sb([4, 17], F32, 'mn')
            k.memset('dve', mn, mn[:, 0:1], 0.0)
            k.op('dve', lambda e: e.tensor_tensor_scan(mn[:, 1:17], hm[:, 0, :], hm[:, 1, :], 0.0, ALU.max, ALU.add),
                 [hm], [mn])
            ra = k.sb([4, 2, 16], F32, 'ra')
            k.tt('dve', ra[:, 0, :], mn[:, 0:16], hm[:, 0, :], ALU.max, [mn, hm], [ra])
            k.tt('dve', ra[:, 1, :], mn[:, 0:16], ra[:, 0, :], ALU.subtract, [mn, ra], [ra])
            k.ts('dve', ra[:, 0, :], ra[:, 0, :], -1.0, None, ALU.mult, None, [ra], [ra])
            k.dma('sp', sc[2, :].rearrange("(h c) -> h c", c=16), ra[:, 0, :], [ra], sc)
            k.dma('sp', sc[3, :].rearrange("(h c) -> h c", c=16), ra[:, 1, :], [ra], sc)
            negR = k.sb([64, 1], F32, 'negR')
            k.dma('sp', negR[:], sc[2, :].rearrange("(a b) -> a b", b=1), [sc], negR)
            alpha = k.sb([128, 64], F32, 'alpha')
            k.dma('sp', alpha[:], sc[3, :].partition_broadcast(128), [sc], alpha)
            k.act(alpha[:], alpha[:], AF.Exp, [alpha], [alpha])
            k.act(u[:], u[:], AF.Exp, [u, negR], [u], bias=negR[:], scale=1.0)
            k.act(csp[:], csp[:], AF.Exp, [csp, negR], [csp], bias=negR[:], scale=1.0)
            Etm = k.sb([128, 64], F32, 'Etm')
            Ttm = k.sb([128, 64], F32, 'Ttm')
            b = self.bank()
            k.tr(b[:, 0:64], u[:], self.ident[0:64, 0:64], [u, self.ident], [b])
            k.copy('dve', Etm[:], b[:, 0:64], [b], [Etm])
            b = self.bank()
            k.tr(b[:, 0:64], csp[:], self.ident[0:64, 0:64], [csp, self.ident], [b])
            k.copy('dve', Ttm[:], b[:, 0:64], [b], [Ttm])
            Cst = [k.sb([128, 129], F32, 'Cst') for _ in range(4)]
            for h in range(4):
                k.memset('pool', Cst[h], Cst[h][:], 0.0)
            Csb = [k.sb([128, 129], BF16, 'Csb') for _ in range(4)]
            Vs = [k.sb([128, 129], BF16, 'Vs') for _ in range(4)]
            Sm = [k.sb([128, 128], BF16, 'Sm') for _ in range(4)]
            kTm = [k.sb([128, 128], BF16, 'kTm') for _ in range(4)]
            vts = [k.sb([128, 1024], F32, 'vt') for _ in range(2)]
            hns = [k.sb([128, 512], F32, 'hn') for _ in range(2)]
            sml = [k.sb([128, 16], F32, 'sml') for _ in range(4)]
            for c in range(NCk):
                vt = vts[c % 2]
                hn = hns[c % 2]
                k.dma('sp', vt[:], PT[c * 128:(c + 1) * 128, 0:1024], [PT], vt)
                tsl = slice(c * 128, (c + 1) * 128)
                for h in range(4):
                    hc = h * 16 + c
                    sm = sml[h]
                    k.op('act', lambda e, h=h, hc=hc: e.activation(Vs[h][:, 0:128], vt[:, h * 128:(h + 1) * 128], AF.Copy,
                                                                 scale=Etm[:, hc:hc + 1]), [vt, Etm], [Vs[h]])
                    k.copy('dve', Vs[h][:, 128:129], Etm[:, hc:hc + 1], [Etm], [Vs[h]])
                    b1 = self.bank()
                    k.mm(b1[:, 0:128], qkT[:, 4 + h, tsl], qkT[:, h, tsl], [qkT], [b1])
                    k.tt('dve', Sm[h][:], b1[:, 0:128], self.mask_le[:], ALU.mult, [b1, self.mask_le], [Sm[h]])
                    b2 = self.bank()
                    k.mm(b2[:, 0:128], qkT[:, 4 + h, tsl], self.identb[:], [qkT, self.identb], [b2])
                    k.copy('act', kTm[h][:], b2[:, 0:128], [b2], [kTm[h]])
                    k.ts('dve', Csb[h][:], Cst[h][:], alpha[:, hc:hc + 1], None, ALU.mult, None, [Cst[h], alpha], [Csb[h]])
                    b3 = self.bank()
                    k.mm(b3[:, 0:129], Sm[h][:], Vs[h][:], [Sm[h], Vs[h]], [b3], start=True, stop=False, inc=False)
                    k.mm(b3[:, 0:129], qkT[:, h, tsl], Csb[h][:], [qkT, Csb[h]], [b3], start=False, stop=True)
                    b4 = self.bank()
                    k.mm(b4[:, 0:129], kTm[h][:], Vs[h][:], [kTm[h], Vs[h]], [b4])
                    k.stt('dve', Cst[h][:], Cst[h][:], alpha[:, hc:hc + 1], b4[:, 0:129], ALU.mult, ALU.add,
                          [Cst[h], alpha, b4], [Cst[h]])
                    k.copy('act', sm[:, 12:13], b3[:, 128:129], [b3], [sm])
                    k.stt('dve', sm[:, 13:14], sm[:, 12:13], -1.0, sm[:, 12:13], ALU.mult, ALU.max, [sm], [sm])
                    k.tt('dve', sm[:, 0:1], sm[:, 13:14], Ttm[:, hc:hc + 1], ALU.max, [sm, Ttm], [sm])
                    k.op('dve', lambda e, sm=sm: e.reciprocal(sm[:, 1:2], sm[:, 0:1]), [sm], [sm])
                    hs = hn[:, h * 128:(h + 1) * 128]
                    k.ts('dve', hs, b3[:, 0:128], sm[:, 1:2], None, ALU.mult, None, [b3, sm], [hn])
                    k.op('dve', lambda e, sm=sm, hs=hs: e.bn_stats(sm[:, 2:8], hs), [hn], [sm])
                    k.op('dve', lambda e, sm=sm: e.bn_aggr(sm[:, 8:10], sm[:, 2:8]), [sm], [sm])
                    k.act(sm[:, 10:11], sm[:, 9:10], AF.Sqrt, [sm, self.eps_ln], [sm], bias=self.eps_ln[:], scale=1.0)
                    k.op('dve', lambda e, sm=sm: e.reciprocal(sm[:, 11:12], sm[:, 10:11]), [sm], [sm])
                    k.ts('dve', hs, hs, sm[:, 8:9], sm[:, 11:12], ALU.subtract, ALU.mult, [hn, sm], [hn])
                k.act(vt[:, 512:1024], vt[:, 512:1024], AF.Sigmoid, [vt], [vt])
                k.tt('dve', hn[:], hn[:], vt[:, 512:1024], ALU.mult, [hn, vt], [hn])
                k.dma('pool', MIX[c * 128:(c + 1) * 128, 0:512], hn[:], [hn], MIX, stream=True)

    def phase_rwkv(self, l, PT, MIX):
        k = self.k
        P64 = slice(0, 64)
        with k.phase():
            def bc(n, nm):
                return self.bc_load(self.inp[n], self.inp[n][l], 512, nm)
            w0, a0, kkp, ka, rrk, gng, gnb = (bc('r_w0', 'w0'), bc('r_a0', 'a0'), bc('r_kk', 'kkp'), bc('r_ka', 'ka'),
                                               bc('r_rk', 'rrk'), bc('r_gn_g', 'gng'), bc('r_gn_b', 'gnb'))
            omk = k.sb([128, 512], F32, 'omk')
            k.ts('dve', omk[:], ka[:], -1.0, 1.0, ALU.mult, ALU.add, [ka], [omk])
            w2 = k.sb([64, 512], F32, 'w2')
            a2 = k.sb([64, 512], F32, 'a2')
            g2 = k.sb([128, 512], F32, 'g2')
            k.dma('sp', w2[:], self.inp['r_w2'][l], [self.inp['r_w2']], w2)
            k.dma('sp', a2[:], self.inp['r_a2'][l], [self.inp['r_a2']], a2)
            k.dma('sp', g2[:], self.inp['r_g2'][l], [self.inp['r_g2']], g2)

            def b2(t):
                return t[P64, :].unsqueeze(1).to_broadcast([64, 2, 512])

            def mk_mask(pattern_cm, op, base=0):
                m = k.sb([64, 8, 64], F32, 'msk')
                k.memset('pool', m, m[:], 1.0)
                k.op('pool', lambda e: e.affine_select(m[:, 0, :], m[:, 0, :], [[pattern_cm[0], 64]], op, 0.0,
                                                       base=base, channel_multiplier=pattern_cm[1]), [m], [m])
                for i in range(1, 8):
                    k.copy('pool', m[:, i, :], m[:, 0, :], [m], [m])
                return m
            mSU = mk_mask((1, -1), ALU.is_gt)
            mSL = mk_mask((-1, 1), ALU.is_gt)
            mIU = mk_mask((1, -1), ALU.is_ge)
            I8 = mk_mask((1, -1), ALU.is_equal)
            tri = mIU[:, 0, :]
            ST = k.sb([64, 8, 64], F32, 'ST')
            k.memset('pool', ST, ST[:], 0.0)

            def tm(nm):
                return k.sb([64, 2, 512], F32, nm)

            def pair(f):
                return [f(), f()]
            lw, av, gam, gprev, ginv = tm('lw'), tm('av'), tm('gam'), tm('gprev'), tm('ginv')
            kk, rk2, t1, t2, yv = tm('kk'), tm('rk2'), tm('t1'), tm('t2'), tm('yv')
            lrw = k.sb([64, 128], F32, 'lrw')
            lra = k.sb([64, 128], F32, 'lra')
            lrg = k.sb([128, 128], F32, 'lrg')
            QTb = {q: k.sb([64, 16, 64], BF16, 'QTb' + q) for q in ('a', 'b', 'k', 'r')}
            Mc, Nc, Mn, Nn, Qm = [k.sb([64, 16, 64], BF16, 'nm') for _ in range(5)]
            Wsb = k.sb([64, 8, 64], F32, 'Wsb')
            Usb = k.sb([64, 8, 64], F32, 'Usb')
            xt_p = pair(lambda: k.sb([64, 2, R_COLS], F32, 'xt'))
            gv_p, bt_p, kt_p = pair(lambda: tm('gv')), pair(lambda: tm('bt')), pair(lambda: tm('kt'))
            sm_p = pair(lambda: k.sb([64, 8, 16], F32, 'rsm'))
            gL_p = pair(lambda: k.sb([64, 16], F32, 'gL'))
            QTa_p, QTr_p = pair(lambda: k.sb([64, 16, 64], F32, 'QTa')), pair(lambda: k.sb([64, 16, 64], F32, 'QTr'))
            Pm_p = pair(lambda: k.sb([64, 16, 64], F32, 'Pm'))
            Aak_p, Arb_p, Ark_p = [pair(lambda: k.sb([64, 16, 64], F32, 'am')) for _ in range(3)]

            def h3(ap):
                return ap.rearrange("p j (h c) -> p j h c", c=64)

            def s3(ap16):
                return ap16.rearrange("p (j h) -> p j h", j=2)

            def bch(ap16):
                return s3(ap16).unsqueeze(3).to_broadcast([64, 2, 8, 64])

            def v3(bb):
                return bb[P64, :].rearrange("p (h c) -> p h c", c=64)

            def front(it):
                p = it % 2
                xt, gv, bt, kt, sm, gL = xt_p[p], gv_p[p], bt_p[p], kt_p[p], sm_p[p], gL_p[p]
                QT = {'a': QTa_p[p], 'r': QTr_p[p]}
                Pm, Aak, Arb, Ark = Pm_p[p], Aak_p[p], Arb_p[p], Ark_p[p]
                r0 = it * 128
                rr_ = xt[:, :, 0:512]
                rkx = xt[:, :, 512:1024]
                k.dma('sp', xt[:], PT[r0:r0 + 128, 1024:1024 + R_COLS].rearrange("(j p) c -> p j c", p=64), [PT], xt)
                b = self.bank()
                for j in range(2):
                    k.tr(b[P64, j * 64:(j + 1) * 64], xt[:, j, 1536:1600], self.ident[P64, P64], [xt, self.ident], [b], inc=False)
                    k.tr(b[P64, 128 + j * 64:128 + (j + 1) * 64], xt[:, j, 1600:1664], self.ident[P64, P64],
                         [xt, self.ident], [b], inc=False)
                    k.tr(b[:, 256 + j * 64:256 + (j + 1) * 64], xt[:, j, 1664:1792], self.ident[P64, P64],
                         [xt, self.ident], [b], inc=(j == 1))
                k.act(lrw[:], b[P64, 0:128], AF.Tanh, [b], [lrw])
                k.copy('dve', lra[:], b[P64, 128:256], [b], [lra])
                k.act(lrg[:], b[:, 256:384], AF.Sigmoid, [b], [lrg])
                for j in range(2):
                    js = slice(j * 64, (j + 1) * 64)
                    b = self.bank()
                    k.mm(b[P64, :], lrw[:, js], w2[:], [lrw, w2], [b])
                    k.tt('dve', lw[:, j, :], b[P64, :], w0[P64, :], ALU.add, [b, w0], [lw])
                    b = self.bank()
                    k.mm(b[P64, :], lra[:, js], a2[:], [lra, a2], [b])
                    k.tt('dve', av[:, j, :], b[P64, :], a0[P64, :], ALU.add, [b, a0], [av])
                    b = self.bank()
                    k.mm(b[P64, :], lrg[:, js], g2[:], [lrg, g2], [b])
                    k.copy('act', gv[:, j, :], b[P64, :], [b], [gv])
                k.act(lw[:], lw[:], AF.Sigmoid, [lw], [lw])
                k.ts('dve', lw[:], lw[:], -0.6065306597126334, None, ALU.mult, None, [lw], [lw])
                k.act(av[:], av[:], AF.Sigmoid, [av], [av])
                for j in range(2):
                    b = self.bank()
                    k.mm(b[P64, :], tri, lw[:, j, :], [mIU, lw], [b])
                    k.act(gam[:, j, :], b[P64, :], AF.Exp, [b], [gam])
                    k.act(ginv[:, j, :], b[P64, :], AF.Exp, [b], [ginv], scale=-1.0)
                    k.tt('dve', gprev[:, j, :], b[P64, :], lw[:, j, :], ALU.subtract, [b, lw], [gprev])
                k.act(gprev[:], gprev[:], AF.Exp, [gprev], [gprev])
                b = self.bank()
                for j in range(2):
                    for h in range(8):
                        blk = j * 8 + h
                        k.mm(b[P64, blk:blk + 1], lw[:, j, h * 64:(h + 1) * 64], self.one_c[P64, :], [lw, self.one_c], [b],
                             inc=(blk == 15))
                k.act(gL[:], b[P64, 0:16], AF.Exp, [b], [gL])
                yield
                k.tt('dve', kk[:], rkx, b2(kkp), ALU.mult, [xt, kkp], [kk])
                k.tt('dve', t1[:], kk[:], kk[:], ALU.mult, [kk], [t1])
                k.op('dve', lambda e: e.reduce_sum(s3(sm[:, 0, :]), h3(t1[:]), AX.X), [t1], [sm])
                k.act(sm[:, 1, :], sm[:, 0, :], AF.Sqrt, [sm], [sm])
                k.ts('dve', sm[:, 1, :], sm[:, 1, :], 1e-12, None, ALU.max, None, [sm], [sm])
                k.op('dve', lambda e: e.reciprocal(sm[:, 2, :], sm[:, 1, :]), [sm], [sm])
                k.tt('dve', h3(kk[:]), h3(kk[:]), bch(sm[:, 2, :]), ALU.mult, [kk, sm], [kk])
                k.tt('dve', t1[:], av[:], b2(ka), ALU.mult, [av, ka], [t1])
                k.tt('dve', t1[:], t1[:], b2(omk), ALU.add, [t1, omk], [t1])
                k.tt('dve', rk2[:], rkx, t1[:], ALU.mult, [xt, t1], [rk2])
                k.tt('dve', t1[:], rr_, rk2[:], ALU.mult, [xt, rk2], [t1])
                k.tt('dve', t1[:], t1[:], b2(rrk), ALU.mult, [t1, rrk], [t1])
                k.op('dve', lambda e: e.reduce_sum(s3(sm[:, 3, :]), h3(t1[:]), AX.X), [t1], [sm])
                k.stt('dve', gprev[:], kk[:], -1.0, gprev[:], ALU.mult, ALU.mult, [kk, gprev], [gprev])
                k.tt('dve', bt[:], kk[:], av[:], ALU.mult, [kk, av], [bt])
                k.tt('dve', bt[:], bt[:], ginv[:], ALU.mult, [bt, ginv], [bt])
                k.tt('dve', kt[:], rk2[:], ginv[:], ALU.mult, [rk2, ginv], [kt])
                k.tt('dve', gam[:], rr_, gam[:], ALU.mult, [xt, gam], [gam])
                yield
                ei = 0
                for q, src in (('a', gprev), ('b', bt), ('k', kt), ('r', gam)):
                    for j in range(2):
                        b = self.bank()
                        for h in range(8):
                            k.tr(b[P64, h * 64:(h + 1) * 64], src[:, j, h * 64:(h + 1) * 64], self.ident[P64, P64],
                                 [src, self.ident], [b], inc=(h == 7))
                        k.copy('act' if ei % 2 == 0 else 'dve', QTb[q][:, j * 8:(j + 1) * 8, :], v3(b), [b], [QTb[q]])
                        if q in QT:
                            k.copy('dve' if ei % 2 == 0 else 'act', QT[q][:, j * 8:(j + 1) * 8, :], v3(b), [b], [QT[q]])
                        ei += 1
                    if q == 'b':
                        yield
                yield
                specs = [('b', 'a', mSU, Mc), ('a', 'b', mSL, Nc), ('k', 'a', mSU, Aak), ('b', 'r', mIU, Arb),
                         ('k', 'r', mIU, Ark)]
                for si, (ql, qr, msk, dst) in enumerate(specs):
                    for j in range(2):
                        b = self.bank()
                        for h in range(8):
                            blk = j * 8 + h
                            k.mm(b[P64, h * 64:(h + 1) * 64], QTb[ql][:, blk, :], QTb[qr][:, blk, :], [QTb[ql], QTb[qr]], [b],
                                 inc=(h == 7))
                        k.tt('dve', dst[:, j * 8:(j + 1) * 8, :], v3(b), msk[:], ALU.mult, [b, msk], [dst])
                    if si == 1:
                        yield
                for j in range(2):
                    js = slice(j * 8, (j + 1) * 8)
                    k.tt('dve', Pm[:, js, :], Mc[:, js, :], I8[:], ALU.add, [Mc, I8], [Pm])
                    k.tt('dve', Qm[:, js, :], Nc[:, js, :], I8[:], ALU.add, [Nc, I8], [Qm])
                yield
                mc, ncur, mn, nn = Mc, Nc, Mn, Nn
                for lvl in range(5):
                    last = (lvl == 4)
                    bms, bns, bps, bqs = [], [], [], []
                    for j in range(2):
                        bm = self.bank()
                        for h in range(8):
                            blk = j * 8 + h
                            k.mm(bm[P64, h * 64:(h + 1) * 64], ncur[:, blk, :], mc[:, blk, :], [ncur, mc], [bm], inc=(h == 7))
                        bms.append(bm)
                        if not last:
                            bn = self.bank()
                            for h in range(8):
                                blk = j * 8 + h
                                k.mm(bn[P64, h * 64:(h + 1) * 64], mc[:, blk, :], ncur[:, blk, :], [ncur, mc], [bn],
                                     inc=(h == 7))
                            bns.append(bn)
                    for j in range(2):
                        js = slice(j * 8, (j + 1) * 8)
                        k.copy('act', mn[:, js, :], v3(bms[j]), [bms[j]], [mn])
                        if not last:
                            k.copy('dve', nn[:, js, :], v3(bns[j]), [bns[j]], [nn])
                    for j in range(2):
                        bp = self.bank()
                        for h in range(8):
                            blk = j * 8 + h
                            k.mm(bp[P64, h * 64:(h + 1) * 64], Qm[:, blk, :], mn[:, blk, :], [Qm, mn], [bp], inc=(h == 7))
                        bps.append(bp)
                        if not last:
                            bq = self.bank()
                            for h in range(8):
                                blk = j * 8 + h
                                k.mm(bq[P64, h * 64:(h + 1) * 64], mn[:, blk, :], Qm[:, blk, :], [Qm, mn], [bq],
                                     inc=(h == 7))
                            bqs.append(bq)
                    for j in range(2):
                        js = slice(j * 8, (j + 1) * 8)
                        k.tt('dve', Pm[:, js, :], Pm[:, js, :], v3(bps[j]), ALU.add, [Pm, bps[j]], [Pm])
                        if not last:
                            k.tt('dve', Qm[:, js, :], Qm[:, js, :], v3(bqs[j]), ALU.add, [Qm, bqs[j]], [Qm])
                    mc, mn = mn, mc
                    ncur, nn = nn, ncur
                    yield

            def back(it):
                p = it % 2
                xt, gv, bt, kt, sm, gL = xt_p[p], gv_p[p], bt_p[p], kt_p[p], sm_p[p], gL_p[p]
                QT = {'a': QTa_p[p], 'r': QTr_p[p]}
                Pm, Aak, Arb, Ark = Pm_p[p], Aak_p[p], Arb_p[p], Ark_p[p]
                r0 = it * 128
                rv = xt[:, :, 1024:1536]
                for j in range(2):
                    bw = self.bank()
                    for h in range(8):
                        blk = j * 8 + h
                        hs = slice(h * 64, (h + 1) * 64)
                        k.mm(bw[P64, hs], QT['a'][:, blk, :], ST[:, h, :], [QT['a'], ST], [bw], start=True, stop=False, inc=False)
                        k.mm(bw[P64, hs], Aak[:, blk, :], xt[:, j, 1024 + h * 64:1024 + (h + 1) * 64], [Aak, xt], [bw],
                             start=False, stop=True, inc=(h == 7))
                    k.copy('act', Wsb[:], v3(bw), [bw], [Wsb])
                    yield
                    bu = self.bank()
                    for h in range(8):
                        blk = j * 8 + h
                        k.mm(bu[P64, h * 64:(h + 1) * 64], Pm[:, blk, :], Wsb[:, h, :], [Pm, Wsb], [bu], inc=(h == 7))
                    k.copy('act', Usb[:], v3(bu), [bu], [Usb])
                    yield
                    by = self.bank()
                    bs_ = self.bank()
                    for h in range(8):
                        hs = slice(h * 64, (h + 1) * 64)
                        vh = xt[:, j, 1024 + h * 64:1024 + (h + 1) * 64]
                        k.mm(bs_[P64, hs], bt[:, j, hs], Usb[:, h, :], [bt, Usb], [bs_], start=True, stop=False, inc=False)
                        k.mm(bs_[P64, hs], kt[:, j, hs], vh, [kt, xt], [bs_], start=False, stop=True, inc=(h == 7))
                    for h in range(8):
                        blk = j * 8 + h
                        hs = slice(h * 64, (h + 1) * 64)
                        vh = xt[:, j, 1024 + h * 64:1024 + (h + 1) * 64]
                        k.mm(by[P64, hs], QT['r'][:, blk, :], ST[:, h, :], [QT['r'], ST], [by], start=True, stop=False, inc=False)
                        k.mm(by[P64, hs], Arb[:, blk, :], Usb[:, h, :], [Arb, Usb], [by], start=False, stop=False, inc=False)
                        k.mm(by[P64, hs], Ark[:, blk, :], vh, [Ark, xt], [by], start=False, stop=True, inc=(h == 7))
                    k.tt('dve', ST[:], ST[:], v3(bs_), ALU.add, [ST, bs_], [ST])
                    k.tt('dve', ST[:], ST[:], gL[:, j * 8:(j + 1) * 8].unsqueeze(2).to_broadcast([64, 8, 64]), ALU.mult,
                         [ST, gL], [ST])
                    k.copy('act', yv[:, j, :], by[P64, :], [by], [yv])
                    yield
                y3 = h3(yv[:])
                k.op('dve', lambda e: e.reduce_sum(s3(sm[:, 4, :]), y3, AX.X), [yv], [sm])
                k.ts('dve', sm[:, 4, :], sm[:, 4, :], 1.0 / 64, None, ALU.mult, None, [sm], [sm])
                k.tt('dve', y3, y3, bch(sm[:, 4, :]), ALU.subtract, [yv, sm], [yv])
                k.tt('dve', t2[:], yv[:], yv[:], ALU.mult, [yv], [t2])
                k.op('dve', lambda e: e.reduce_sum(s3(sm[:, 5, :]), h3(t2[:]), AX.X), [t2], [sm])
                k.ts('dve', sm[:, 5, :], sm[:, 5, :], 1.0 / 64, 64e-5, ALU.mult, ALU.add, [sm], [sm])
                k.act(sm[:, 5, :], sm[:, 5, :], AF.Sqrt, [sm], [sm])
                k.op('dve', lambda e: e.reciprocal(sm[:, 6, :], sm[:, 5, :]), [sm], [sm])
                k.tt('dve', y3, y3, bch(sm[:, 6, :]), ALU.mult, [yv, sm], [yv])
                yield
                k.tt('dve', yv[:], yv[:], b2(gng), ALU.mult, [yv, gng], [yv])
                k.tt('dve', yv[:], yv[:], b2(gnb), ALU.add, [yv, gnb], [yv])
                k.tt('dve', h3(t2[:]), h3(rv), bch(sm[:, 3, :]), ALU.mult, [xt, sm], [t2])
                k.tt('dve', yv[:], yv[:], t2[:], ALU.add, [yv, t2], [yv])
                k.tt('dve', yv[:], yv[:], gv[:], ALU.mult, [yv, gv], [yv])
                k.dma('pool', MIX[r0:r0 + 128, 512:1024].rearrange("(j p) c -> p j c", p=64), yv[:], [yv], MIX, stream=True)
                yield

            RWI = 16

            def drive(gens):
                gens = [g for g in gens if g is not None]
                while gens:
                    for g in list(gens):
                        try:
                            next(g)
                        except StopIteration:
                            gens.remove(g)
            drive([front(0)])
            for it in range(RWI):
                drive([back(it), front(it + 1) if it + 1 < RWI else None])

    def resid_ln(self, banks, ti, H, g_bc, b_bc, zs, out_dram=None, defer=False, preloaded=False):
        k = self.k
        z = zs[ti % len(zs)]
        if not preloaded:
            k.dma('sp', z[:], H[ti * 128:(ti + 1) * 128, :], [H], z)
        if isinstance(banks, (list, tuple)):
            for hf in range(2):
                k.stt('dve', z[:, hf * 512:(hf + 1) * 512], z[:, hf * 512:(hf + 1) * 512], DN_ALPHA, banks[hf][:],
                      ALU.mult, ALU.add, [z, banks[hf]], [z])
        else:
            k.stt('dve', z[:], z[:], DN_ALPHA, banks[:], ALU.mult, ALU.add, [z, banks], [z])
        return self.ln_tile(z, g_bc, b_bc, ti, H, out_dram=out_dram, defer=defer)

    def phase_outproj(self, l, MIX, H):
        k = self.k
        wo_t = self.inp['w_out']
        with k.phase():
            wo = self.load_w(wo_t, wo_t[l], 1024, 'wo')
            gcol = k.sb([128, 4], F32, 'gcol')
            k.dma('sp', gcol[:], self.inp['m_norm_g'][l].rearrange("(c p) -> p c", p=128), [self.inp['m_norm_g']], gcol,
                  allow_slow_non_contiguous=True)
            for c in range(4):
                k.ts('dve', wo[:, c, :], wo[:, c, :], gcol[:, c:c + 1], None, ALU.mult, None, [wo, gcol], [wo])
            g = self.bc_load(self.inp['ln1_g'], self.inp['ln1_g'][l], D)
            b = self.bc_load(self.inp['ln1_b'], self.inp['ln1_b'][l], D)
            zs = [k.sb([128, D], F32, 'z') for _ in range(3)]
            pend = None
            ms = [k.sb([128, D], F32, 'mx') for _ in range(2)]
            mTs = [k.sb([128, 8, 128], BF16, 'mT') for _ in range(2)]
            for ti in range(NT):
                m = ms[ti % 2]
                mT = mTs[ti % 2]
                k.dma('sp', m[:], MIX[ti * 128:(ti + 1) * 128, :], [MIX], m)
                k.dma('sp', zs[ti % 3][:], H[ti * 128:(ti + 1) * 128, :], [H], zs[ti % 3])
                for half in range(2):
                    bb = self.bank()
                    for j in range(4):
                        c = half * 4 + j
                        k.tr(bb[:, j * 128:(j + 1) * 128], m[:, c * 128:(c + 1) * 128], self.ident[:], [m, self.ident], [bb],
                             inc=(j == 3))
                    k.copy('act', mT[:, half * 4:(half + 1) * 4, :], bb[:].rearrange("p (c t) -> p c t", c=4), [bb], [mT])
                bks = [self.bank(), self.bank()]
                for hf in range(2):
                    for kc in range(8):
                        k.mm(bks[hf][:], mT[:, kc, :], wo[:, kc, hf * 512:(hf + 1) * 512], [mT, wo], [bks[hf]],
                             start=(kc == 0), stop=(kc == 7), inc=(kc == 7))
                if pend is not None:
                    pend()
                pend = self.resid_ln(bks, ti, H, g, b, zs, defer=True, preloaded=True)
            pend()

    def xattn_preload(self, l):
        return (self.load_w(self.inp['x_wq'], self.inp['x_wq'][l], 1024, 'wq'),
                self.load_w(self.inp['x_wkv'], self.inp['x_wkv'][l], 2048, 'wkv'),
                self.load_w(self.inp['x_wo'], self.inp['x_wo'][l], 1024, 'xwo'))

    def phase_xattn(self, l, H, pre=None):
        k = self.k
        with k.phase():
            wq, wkv, wo = pre if pre is not None else self.xattn_preload(l)
            g = self.bc_load(self.inp['ln2_g'], self.inp['ln2_g'][l], D)
            b = self.bc_load(self.inp['ln2_b'], self.inp['ln2_b'][l], D)
            zs = [k.sb([128, D], F32, 'z') for _ in range(3)]
            pend = None
            memT = k.sb([128, 8, NM], BF16, 'memT')
            for mt in range(2):
                z = zs[mt]
                k.dma('sp', z[:], self.inp['mem'][mt * 128:(mt + 1) * 128, :], [self.inp['mem']], z)
                for half in range(2):
                    bb = self.bank()
                    for j in range(4):
                        c = half * 4 + j
                        k.tr(bb[:, j * 128:(j + 1) * 128], z[:, c * 128:(c + 1) * 128], self.ident[:], [z, self.ident], [bb],
                             inc=(j == 3))
                    k.copy('act', memT[:, half * 4:(half + 1) * 4, mt * 128:(mt + 1) * 128],
                           bb[:].rearrange("p (c t) -> p c t", c=4), [bb], [memT])
            KT = k.sb([128, 8, NM], BF16, 'KT')
            for c in range(8):
                bb = self.bank()
                for kc in range(8):
                    k.mm(bb[:, 0:NM], wkv[:, kc, c * 128:(c + 1) * 128], memT[:, kc, :], [wkv, memT], [bb],
                         start=(kc == 0), stop=(kc == 7), inc=(kc == 7))
                k.copy('act' if c % 2 else 'dve', KT[:, c, :], bb[:, 0:NM], [bb], [KT])
            Vt = k.sb([128, 2, 1024], BF16, 'Vt')
            for mt in range(2):
                for hf in range(2):
                    bb = self.bank()
                    for kc in range(8):
                        k.mm(bb[:], memT[:, kc, mt * 128:(mt + 1) * 128], wkv[:, kc, 1024 + hf * 512:1024 + (hf + 1) * 512],
                             [wkv, memT], [bb], start=(kc == 0), stop=(kc == 7), inc=(kc == 7))
                    k.copy('act' if hf else 'dve', Vt[:, mt, hf * 512:(hf + 1) * 512], bb[:], [bb], [Vt])
            QTs = [k.sb([128, 8, 128], BF16, 'QT') for _ in range(2)]
            OTs = [k.sb([128, 8, 128], BF16, 'OT') for _ in range(2)]
            Pf = [k.sb([128, NM], F32, 'Pf') for _ in range(4)]
            Pn = [k.sb([128, NM], BF16, 'Pn') for _ in range(4)]
            PTt = [k.sb([128, 2, 128], BF16, 'PTt') for _ in range(4)]
            st = [k.sb([128, 4], F32, 'xst') for _ in range(4)]
            scale = float(256 ** -0.5)

            def qproj(ti):
                QT = QTs[ti % 2]
                tsl = slice(1 + ti * 128, 1 + (ti + 1) * 128)
                for half in range(2):
                    bb = self.bank()
                    for j in range(4):
                        c = half * 4 + j
                        for kc in range(8):
                            k.mm(bb[:, j * 128:(j + 1) * 128], wq[:, kc, c * 128:(c + 1) * 128], self.hT[:, kc, tsl],
                                 [wq, self.hT], [bb], start=(kc == 0), stop=(kc == 7), inc=(kc == 7 and j == 3))
                    k.copy('act' if half else 'dve', QT[:, half * 4:(half + 1) * 4, :],
                           bb[:].rearrange("p (c t) -> p c t", c=4), [bb], [QT])

            qproj(0)
            for ti in range(NT):
                QT, OT = QTs[ti % 2], OTs[ti % 2]
                k.dma('sp', zs[ti % 3][:], H[ti * 128:(ti + 1) * 128, :], [H], zs[ti % 3])
                bss = []
                for h in range(4):
                    bs = self.bank()
                    for dc in range(2):
                        k.mm(bs[:, 0:NM], QT[:, 2 * h + dc, :], KT[:, 2 * h + dc, :], [QT, KT], [bs],
                             start=(dc == 0), stop=(dc == 1), inc=(dc == 1))
                    bss.append(bs)
                if ti + 1 < NT:
                    qproj(ti + 1)
                if pend is not None:
                    pend()
                    pend = None
                for h in range(4):
                    s_, bs = st[h], bss[h]
                    k.op('dve', lambda e, s_=s_, bs=bs: e.reduce_max(s_[:, 0:1], bs[:, 0:NM], AX.X), [bs], [s_])
                    k.ts('dve', s_[:, 1:2], s_[:, 0:1], -scale, None, ALU.mult, None, [s_], [s_])
                    k.act(Pf[h][:], bs[:, 0:NM], AF.Exp, [bs, s_], [Pf[h], s_], bias=s_[:, 1:2], scale=scale,
                          accum_out=s_[:, 2:3])
                for h in range(4):
                    s_ = st[h]
                    k.op('dve', lambda e, s_=s_: e.reciprocal(s_[:, 3:4], s_[:, 2:3]), [s_], [s_])
                    k.ts('dve', Pn[h][:], Pf[h][:], s_[:, 3:4], None, ALU.mult, None, [Pf[h], s_], [Pn[h]])
                bts = []
                for h in range(4):
                    bt_ = self.bank()
                    for mc in range(2):
                        k.mm(bt_[:, mc * 128:(mc + 1) * 128], Pn[h][:, mc * 128:(mc + 1) * 128], self.identb[:],
                             [Pn[h], self.identb], [bt_], inc=(mc == 1))
                    bts.append(bt_)
                for h in range(4):
                    k.copy('act' if h % 2 else 'dve', PTt[h][:], bts[h][:, 0:256].rearrange("p (c t) -> p c t", c=2),
                           [bts[h]], [PTt[h]])
                bos = []
                for h in range(4):
                    bo = self.bank()
                    for dc in range(2):
                        c = 2 * h + dc
                        for mc in range(2):
                            k.mm(bo[:, dc * 128:(dc + 1) * 128], Vt[:, mc, c * 128:(c + 1) * 128], PTt[h][:, mc, :],
                                 [Vt, PTt[h]], [bo], start=(mc == 0), stop=(mc == 1), inc=(mc == 1 and dc == 1))
                    bos.append(bo)
                for h in range(4):
                    k.copy('dve' if h % 2 else 'act', OT[:, 2 * h:2 * h + 2, :],
                           bos[h][:, 0:256].rearrange("p (c t) -> p c t", c=2), [bos[h]], [OT])
                bks = [self.bank(), self.bank()]
                for hf in range(2):
                    for kc in range(8):
                        k.mm(bks[hf][:], OT[:, kc, :], wo[:, kc, hf * 512:(hf + 1) * 512], [OT, wo], [bks[hf]],
                             start=(kc == 0), stop=(kc == 7), inc=(kc == 7))
                pend = self.resid_ln(bks, ti, H, g, b, zs, defer=True, preloaded=True)
            pend()

    def phase_moe(self, l, H, final_out=None):
        k = self.k
        w1_t, w2_t = self.inp['moe_w1'], self.inp['moe_w2']
        NEX = NE
        DMAONLY = 0
        with k.phase():
            G = k.sb([128, NT, NE], F32, 'G')
            acc = k.sb([128, NT, D], F32, 'acc')
            with k.phase():
                wr = k.sb([128, 8, NE], F32, 'wr')
                k.dma('sp', wr[:], self.inp['moe_wr'][l].rearrange("(c p) n -> p c n", p=128), [self.inp['moe_wr']], wr)
                br = self.bc_load(self.inp['moe_br'], self.inp['moe_br'][l], NE, 'br')
                b2 = k.sb([NE, D], F32, 'b2')
                k.dma('sp', b2[:], self.inp['moe_b2'][l], [self.inp['moe_b2']], b2)
                hts = [k.sb([128, D], F32, 'ht') for _ in range(2)]
                h32s = [k.sb([128, 8, 128], F32, 'h32') for _ in range(2)]
                lgs = [k.sb([128, NE], F32, 'lg') for _ in range(2)]
                m8s = [k.sb([128, 16], F32, 'm8') for _ in range(2)]
                GTs = [k.sb([NE, 128], F32, 'GT') for _ in range(2)]
                for ti in range(NT):
                    ht, h32, lg, m8, GT = hts[ti % 2], h32s[ti % 2], lgs[ti % 2], m8s[ti % 2], GTs[ti % 2]
                    k.dma('sp', ht[:], H[ti * 128:(ti + 1) * 128, :], [H], ht)
                    for half in range(2):
                        bb = self.bank()
                        for j in range(4):
                            c = half * 4 + j
                            k.tr(bb[:, j * 128:(j + 1) * 128], ht[:, c * 128:(c + 1) * 128], self.ident[:], [ht, self.ident],
                                 [bb], inc=(j == 3))
                        k.copy('act', h32[:, half * 4:(half + 1) * 4, :], bb[:].rearrange("p (c t) -> p c t", c=4), [bb], [h32])
                    bl = self.bank()
                    for kc in range(8):
                        k.mm(bl[:, 0:NE], h32[:, kc, :], wr[:, kc, :], [h32, wr], [bl], start=(kc == 0), stop=(kc == 7),
                             inc=(kc == 7))
                    k.tt('dve', lg[:], bl[:, 0:NE], br[:], ALU.add, [bl, br], [lg])
                    k.op('dve', lambda e, m8=m8, lg=lg: e.max(m8[:, 0:8], lg[:]), [lg], [m8])
                    k.ts('dve', m8[:, 8:9], m8[:, 0:1], -1.0, None, ALU.mult, None, [m8], [m8])
                    g_ = G[:, ti, :]
                    k.act(g_, lg[:], AF.Exp, [lg, m8], [G], bias=m8[:, 8:9], scale=1.0)
                    k.ts('dve', lg[:], lg[:], m8[:, 3:4], None, ALU.is_ge, None, [lg, m8], [lg])
                    k.tt('dve', g_, g_, lg[:], ALU.mult, [G, lg], [G])
                    k.op('dve', lambda e, m8=m8, g_=g_: e.reduce_sum(m8[:, 9:10], g_, AX.X), [G], [m8])
                    k.op('dve', lambda e, m8=m8: e.reciprocal(m8[:, 10:11], m8[:, 9:10]), [m8], [m8])
                    k.ts('dve', g_, g_, m8[:, 10:11], None, ALU.mult, None, [G, m8], [G])
                    bg = self.bank()
                    k.tr(bg[0:NE, 0:128], g_, self.ident[:], [G, self.ident], [bg])
                    k.copy('act', GT[:], bg[0:NE, 0:128], [bg], [GT])
                    for hf in range(2):
                        bb = self.bank()
                        k.mm(bb[:], GT[:], b2[:, hf * 512:(hf + 1) * 512], [GT, b2], [bb])
                        k.copy('act' if hf else 'dve', acc[:, ti, hf * 512:(hf + 1) * 512], bb[:], [bb], [acc])
            with k.phase():
                b1a = k.sb([128, NE, 16], F32, 'b1a')
                for e in range(NE):
                    k.dma('sp', b1a[:, e, :], self.inp['moe_b1'][l, e].rearrange("(c p) -> p c", p=128),
                          [self.inp['moe_b1']], b1a, allow_slow_non_contiguous=True)
                actT = k.sb([128, 8, S], BF16, 'actT')
                w1r = [k.sb([128, 8, 2, 128], BF16, 'w1r') for _ in range(4)]
                w2b = k.sb([128, 8, D], BF16, 'w2b')
                g0s = [k.sb([128, 512], F32, 'g0') for _ in range(2)]
                sgs = [k.sb([128, 512], F32, 'sg') for _ in range(2)]
                u0s = [k.sb([128, 512], F32, 'u0') for _ in range(2)]
                pi = 0
                ei = 0
                for e in range(NEX):
                    for p in range(8):
                        w1 = w1r[pi % 4]
                        pi += 1
                        for gu in range(2):
                            c0 = gu * DFF + p * 128
                            k.dma('pool', w1[:, :, gu, :], w1_t[l, e, :, c0:c0 + 128].rearrange("(c p) n -> p c n", p=128),
                                  [w1_t], w1)
                        if p == 0:
                            k.dma('pool', w2b[:], w2_t[l, e].rearrange("(c p) n -> p c n", p=128), [w2_t], w2b)
                        for tb in range(4 if not DMAONLY else 0):
                            tsl = slice(1 + tb * 512, 1 + (tb + 1) * 512)
                            bgp, bup = self.bank(), self.bank()
                            for gu, bb in ((0, bgp), (1, bup)):
                                for kc in range(8):
                                    k.mm(bb[:], w1[:, kc, gu, :], self.hT[:, kc, tsl], [w1, self.hT], [bb],
                                         start=(kc == 0), stop=(kc == 7), inc=(kc == 7))
                            g0, sg, u0 = g0s[ei % 2], sgs[ei % 2], u0s[ei % 2]
                            ei += 1
                            k.act(g0[:], bgp[:], AF.Identity, [bgp, b1a], [g0], bias=b1a[:, e, p:p + 1], scale=1.0)
                            k.ts('dve', g0[:], g0[:], 7.0, None, ALU.min, None, [g0], [g0])
                            k.act(sg[:], g0[:], AF.Sigmoid, [g0], [sg], scale=1.702)
                            k.tt('dve', sg[:], sg[:], g0[:], ALU.mult, [sg, g0], [sg])
                            k.act(u0[:], bup[:], AF.Identity, [bup, b1a], [u0], bias=b1a[:, e, 8 + p:9 + p], scale=1.0)
                            k.ts('dve', u0[:], u0[:], 7.0, -7.0, ALU.min, ALU.max, [u0], [u0])
                            k.stt('dve', actT[:, p, tb * 512:(tb + 1) * 512], u0[:], 1.0, sg[:], ALU.add, ALU.mult,
                                  [u0, sg], [actT])
                    for ti in range(NT if not DMAONLY else 0):
                        bks = [self.bank(), self.bank()]
                        for hf in range(2):
                            for fc in range(8):
                                k.mm(bks[hf][:], actT[:, fc, ti * 128:(ti + 1) * 128], w2b[:, fc, hf * 512:(hf + 1) * 512],
                                     [actT, w2b], [bks[hf]], start=(fc == 0), stop=(fc == 7), inc=(fc == 7))
                        for hf in range(2):
                            k.stt('dve', acc[:, ti, hf * 512:(hf + 1) * 512], bks[hf][:], G[:, ti, e:e + 1],
                                  acc[:, ti, hf * 512:(hf + 1) * 512], ALU.mult, ALU.add, [bks[hf], G, acc], [acc])
            with k.phase():
                g = self.bc_load(self.inp['ln3_g'], self.inp['ln3_g'][l], D)
                b = self.bc_load(self.inp['ln3_b'], self.inp['ln3_b'][l], D)
                zs = [k.sb([128, D], F32, 'z') for _ in range(2)]
                for ti in range(NT):
                    z = zs[ti % 2]
                    k.dma('sp', z[:], H[ti * 128:(ti + 1) * 128, :], [H], z)
                    k.stt('dve', z[:], z[:], DN_ALPHA, acc[:, ti, :], ALU.mult, ALU.add, [z, acc], [z])
                    self.ln_tile(z, g, b, ti, H, out_dram=final_out)

    def phase_moe_sparse(self, l, H, YB, RT, final_out=None):
        k = self.k
        I32 = mybir.dt.int32
        BLK = MOE_BLK
        NTB = BLK // 128
        NB = MOE_NB
        NR = NB * BLK
        BIG = 4.0e6
        w1v = self.inp['moe_w1'][:].rearrange("l e d f -> (l e d) f")
        w2v = self.inp['moe_w2'][:].rearrange("l e f d -> (l e f) d")
        b1v = self.inp['moe_b1'][:].rearrange("l e (c f) -> (l e c) f", f=128)
        with k.phase():
            G = k.sb([128, NT, NE], F32, 'G')
            dsel = k.sb([128, NT, 4], I32, 'dsel')
            gsel = k.sb([128, NT, 4], F32, 'gsel')
            IW = k.sb([128, NB, 8], I32, 'IW')
            IB = k.sb([16, NB], I32, 'IB')
            b2 = k.sb([NE, D], F32, 'b2')
            k.dma('sp', b2[:], self.inp['moe_b2'][l], [self.inp['moe_b2']], b2)
            with k.phase():
                wr = k.sb([128, 8, NE], F32, 'wr')
                k.dma('sp', wr[:], self.inp['moe_wr'][l].rearrange("(c p) n -> p c n", p=128), [self.inp['moe_wr']], wr)
                br = self.bc_load(self.inp['moe_br'], self.inp['moe_br'][l], NE, 'br')
                Mk = k.sb([128, NT, NE], F32, 'Mk')
                rank = k.sb([128, NT, NE], F32, 'rank')
                hts = [k.sb([128, D], F32, 'ht') for _ in range(2)]
                h32s = [k.sb([128, 8, 128], F32, 'h32') for _ in range(2)]
                lgs = [k.sb([128, NE], F32, 'lg') for _ in range(2)]
                m8s = [k.sb([128, 16], F32, 'm8') for _ in range(2)]
                for ti in range(NT):
                    ht, h32, lg, m8 = hts[ti % 2], h32s[ti % 2], lgs[ti % 2], m8s[ti % 2]
                    k.dma('sp', ht[:], H[ti * 128:(ti + 1) * 128, :], [H], ht)
                    for half in range(2):
                        bb = self.bank()
                        for j in range(4):
                            c = half * 4 + j
                            k.tr(bb[:, j * 128:(j + 1) * 128], ht[:, c * 128:(c + 1) * 128], self.ident[:], [ht, self.ident],
                                 [bb], inc=(j == 3))
                        k.copy('act', h32[:, half * 4:(half + 1) * 4, :], bb[:].rearrange("p (c t) -> p c t", c=4), [bb], [h32])
                    bl = self.bank()
                    for kc in range(8):
                        k.mm(bl[:, 0:NE], h32[:, kc, :], wr[:, kc, :], [h32, wr], [bl], start=(kc == 0), stop=(kc == 7),
                             inc=(kc == 7))
                    k.tt('dve', lg[:], bl[:, 0:NE], br[:], ALU.add, [bl, br], [lg])
                    k.op('dve', lambda e, m8=m8, lg=lg: e.max(m8[:, 0:8], lg[:]), [lg], [m8])
                    k.ts('dve', m8[:, 8:9], m8[:, 0:1], -1.0, None, ALU.mult, None, [m8], [m8])
                    g_ = G[:, ti, :]
                    k.act(g_, lg[:], AF.Exp, [lg, m8], [G], bias=m8[:, 8:9], scale=1.0)
                    k.ts('dve', Mk[:, ti, :], lg[:], m8[:, 3:4], None, ALU.is_ge, None, [lg, m8], [Mk])
                    k.tt('dve', g_, g_, Mk[:, ti, :], ALU.mult, [G, Mk], [G])
                    k.op('dve', lambda e, m8=m8, g_=g_: e.reduce_sum(m8[:, 9:10], g_, AX.X), [G], [m8])
                    k.op('dve', lambda e, m8=m8: e.reciprocal(m8[:, 10:11], m8[:, 9:10]), [m8], [m8])
                    k.ts('dve', g_, g_, m8[:, 10:11], None, ALU.mult, None, [G, m8], [G])
                ones = k.sb([128, 128], F32, 'ones')
                k.memset('pool', ones, ones[:], 1.0)
                lst = k.sb([128, 128], F32, 'lst')
                k.memset('pool', lst, lst[:], 1.0)
                k.op('pool', lambda e: e.affine_select(lst[:], lst[:], [[1, 128]], ALU.is_gt, 0.0, base=0,
                                                       channel_multiplier=-1), [lst], [lst])
                for ti in range(NT):
                    bb = self.bank()
                    for tj in range(ti):
                        k.mm(bb[:, 0:NE], ones[:], Mk[:, tj, :], [ones, Mk], [bb], start=(tj == 0), stop=False, inc=False)
                    k.mm(bb[:, 0:NE], lst[:], Mk[:, ti, :], [lst, Mk], [bb], start=(ti == 0), stop=True)
                    k.copy('act' if ti % 2 else 'dve', rank[:, ti, :], bb[:, 0:NE], [bb], [rank])
                bb = self.bank()
                for tj in range(NT):
                    k.mm(bb[:, 0:NE], ones[:], Mk[:, tj, :], [ones, Mk], [bb], start=(tj == 0), stop=(tj == NT - 1),
                         inc=(tj == NT - 1))
                cnt = k.sb([128, NE], F32, 'cnt')
                k.copy('dve', cnt[:], bb[:, 0:NE], [bb], [cnt])
                thr = k.sb([128, 16], F32, 'thr')
                k.op('pool', lambda e: e.iota(thr[:], [[BLK, 16]], base=0, channel_multiplier=0,
                                              allow_small_or_imprecise_dtypes=True), [], [thr])
                cmp_ = k.sb([128, NE, 16], F32, 'cmp')
                k.tt('dve', cmp_[:], cnt[:].unsqueeze(2).to_broadcast([128, NE, 16]),
                     thr[:].unsqueeze(1).to_broadcast([128, NE, 16]), ALU.is_gt, [cnt, thr], [cmp_])
                pad = k.sb([128, 4, NE], F32, 'pad')
                k.op('dve', lambda e: e.reduce_sum(pad[:, 0, :], cmp_[:], AX.X), [cmp_], [pad])
                k.ts('dve', pad[:, 0, :], pad[:, 0, :], float(BLK), None, ALU.mult, None, [pad], [pad])
                k.op('dve', lambda e: e.tensor_tensor_scan(pad[:, 1, :], ones[:, 0:NE], pad[:, 0, :], 0.0, ALU.mult, ALU.add),
                     [ones, pad], [pad])
                k.tt('dve', pad[:, 2, :], pad[:, 1, :], pad[:, 0, :], ALU.subtract, [pad], [pad])
                bth = k.sb([128, NB], F32, 'bth')
                k.op('pool', lambda e: e.iota(bth[:], [[BLK, NB]], base=0, channel_multiplier=0,
                                              allow_small_or_imprecise_dtypes=True), [], [bth])
                cmpb = k.sb([128, NB, NE], F32, 'cmpb')
                k.tt('dve', cmpb[:], pad[:, 1, :].unsqueeze(1).to_broadcast([128, NB, NE]),
                     bth[:].unsqueeze(2).to_broadcast([128, NB, NE]), ALU.is_le, [pad, bth], [cmpb])
                be = k.sb([128, 4, NB], F32, 'be')
                k.op('dve', lambda e: e.reduce_sum(be[:, 0, :], cmpb[:], AX.X), [cmpb], [be])
                k.ts('dve', be[:, 1, :], be[:, 0, :], float(NE) - 0.5, BIG, ALU.is_gt, ALU.mult, [be], [be])
                k.ts('dve', be[:, 0, :], be[:, 0, :], float(NE - 1), None, ALU.min, None, [be], [be])
                iw0 = k.sb([128, 8], F32, 'iw0')
                k.op('pool', lambda e: e.iota(iw0[:], [[128, 8]], base=l * NE * 1024, channel_multiplier=1,
                                              allow_small_or_imprecise_dtypes=True), [], [iw0])
                k.ts('dve', be[:, 2, :], be[:, 0, :], 1024.0, None, ALU.mult, None, [be], [be])
                k.tt('dve', be[:, 2, :], be[:, 2, :], be[:, 1, :], ALU.add, [be], [be])
                iwf = k.sb([128, NB, 8], F32, 'iwf')
                k.tt('dve', iwf[:], be[:, 2, :].unsqueeze(2).to_broadcast([128, NB, 8]),
                     iw0[:].unsqueeze(1).to_broadcast([128, NB, 8]), ALU.add, [be, iw0], [iwf])
                k.copy('dve', IW[:], iwf[:], [iwf], [IW])
                ib0 = k.sb([16, 1], F32, 'ib0')
                k.op('pool', lambda e: e.iota(ib0[:], [[0, 1]], base=l * NE * 16, channel_multiplier=1,
                                              allow_small_or_imprecise_dtypes=True), [], [ib0])
                ibf = k.sb([16, NB], F32, 'ibf')
                k.ts('dve', ibf[:], be[0:16, 0, :], 16.0, ib0[:, 0:1], ALU.mult, ALU.add, [be, ib0], [ibf])
                k.tt('dve', ibf[:], ibf[:], be[0:16, 1, :], ALU.add, [ibf, be], [ibf])
                k.copy('dve', IB[:], ibf[:], [ibf], [IB])
                zt = k.sb([128, (NR // 128) * 16], I32, 'zt')
                k.memset('pool', zt, zt[:], 0)
                k.dma('sp', RT[:, :].rearrange("(p n) c -> p (n c)", p=128), zt[:], [zt], RT)
                RTS = T(RT.t)
                Dm = k.sb([128, NE], F32, 'Dm')
                d8 = k.sb([128, 8], F32, 'd8')
                eq = k.sb([128, NE], F32, 'eq')
                tok = k.sb([128, 16], I32, 'tok')
                for ti in range(NT):
                    k.tt('dve', Dm[:], rank[:, ti, :], pad[:, 2, :], ALU.add, [rank, pad], [Dm])
                    k.ts('dve', Dm[:], Dm[:], 1.0, None, ALU.add, None, [Dm], [Dm])
                    k.tt('dve', Dm[:], Dm[:], Mk[:, ti, :], ALU.mult, [Dm, Mk], [Dm])
                    k.ts('dve', Dm[:], Dm[:], -1.0, None, ALU.add, None, [Dm], [Dm])
                    k.op('dve', lambda e: e.max(d8[:], Dm[:]), [Dm], [d8])
                    k.copy('dve', dsel[:, ti, :], d8[:, 0:4], [d8], [dsel])
                    for kk_ in range(4):
                        k.ts('dve', eq[:], Dm[:], d8[:, kk_:kk_ + 1], None, ALU.is_equal, None, [Dm, d8], [eq])
                        k.tt('dve', eq[:], eq[:], G[:, ti, :], ALU.mult, [eq, G], [eq])
                        k.op('dve', lambda e, kk_=kk_: e.reduce_sum(gsel[:, ti, kk_:kk_ + 1], eq[:], AX.X), [eq], [gsel])
                    k.op('pool', lambda e, ti=ti: e.iota(tok[:], [[0, 16]], base=ti * 128, channel_multiplier=1), [], [tok])
                    for kk_ in range(4):
                        k.idma(RTS, RT[:, :], tok, tok[:], dsel, dsel[:, ti, kk_:kk_ + 1], scatter=True, stream=True, extra_reads=[RT])
            with k.phase():
                NW = 16
                w1p = [k.sb([128, 2 * DFF], BF16, 'w1p') for _ in range(NW)]
                w2p = [k.sb([128, D], BF16, 'w2p') for _ in range(NW)]
                xia = k.sb([128, NB, NTB], I32, 'xia')
                k.dma('sp', xia[:], RT[:, 0:1].rearrange("(b i p) c -> p b (i c)", p=128, i=NTB), [RT, RTS], xia,
                      allow_slow_non_contiguous=True)
                xgs = [k.sb([128, D], F32, 'xg') for _ in range(4)]
                xTs = [k.sb([128, 8, BLK], BF16, 'xT') for _ in range(2)]
                aTs = [k.sb([128, 8, BLK], BF16, 'aT') for _ in range(2)]
                b1gs = [k.sb([16, 128], F32, 'b1g') for _ in range(2)]
                b1bs = [k.sb([128, 16], F32, 'b1b') for _ in range(2)]
                ysb = [k.sb([128, D], F32, 'ysb') for _ in range(2)]
                g0s = [k.sb([128, BLK], F32, 'g0') for _ in range(2)]
                sgs = [k.sb([128, BLK], F32, 'sg') for _ in range(2)]
                u0s = [k.sb([128, BLK], F32, 'u0') for _ in range(2)]
                NBX = NB
                wi = 0
                ei = 0
                xgi = 0
                yi = 0
                lo_, hi_ = list(range(0, NE)), list(range(NE, NB))
                order = []
                while lo_ or hi_:
                    if lo_:
                        order.append(lo_.pop(0))
                    if hi_:
                        order.append(hi_.pop(0))
                for bi_, b in enumerate(order[:NBX]):
                    xT, aT, b1g, b1b = xTs[bi_ % 2], aTs[bi_ % 2], b1gs[bi_ % 2], b1bs[bi_ % 2]
                    xg4 = []
                    for i in range(NTB):
                        xg = xgs[xgi % 4]
                        xgi += 1
                        k.idma(xg, xg[:], H, H[:, :], xia, xia[:, b, i:i + 1])
                        xg4.append(xg)
                    k.idma(b1g, b1g[:], self.inp['moe_b1'], b1v, IB, IB[:, b:b + 1], bounds=self.reg_b)
                    w1k, w2k = [], []
                    for kc in range(8):
                        w = w1p[wi % NW]
                        k.idma(w, w[:], self.inp['moe_w1'], w1v, IW, IW[:, b, kc:kc + 1], bounds=self.reg_w)
                        w1k.append(w)
                        wi += 1
                    wi -= 8
                    for kc in range(8):
                        w = w2p[wi % NW]
                        k.idma(w, w[:], self.inp['moe_w2'], w2v, IW, IW[:, b, kc:kc + 1], bounds=self.reg_w)
                        w2k.append(w)
                        wi += 1
                    for i in range(NTB):
                        xg = xg4[i]
                        for half in range(2):
                            bb = self.bank()
                            for j in range(4):
                                c = half * 4 + j
                                k.tr(bb[:, j * 128:(j + 1) * 128], xg[:, c * 128:(c + 1) * 128], self.ident[:],
                                     [xg, self.ident], [bb], inc=(j == 3))
                            k.copy('act' if half else 'dve', xT[:, half * 4:(half + 1) * 4, i * 128:(i + 1) * 128],
                                   bb[:].rearrange("p (c t) -> p c t", c=4), [bb], [xT])
                    bb = self.bank()
                    k.tr(bb[:, 0:16], b1g[:], self.ident[0:16, 0:16], [b1g, self.ident], [bb])
                    k.copy('dve', b1b[:], bb[:, 0:16], [bb], [b1b])
                    for p in range(8):
                        bgp, bup = self.bank(), self.bank()
                        for gu, bb in ((0, bgp), (1, bup)):
                            c0 = gu * DFF + p * 128
                            for kc in range(8):
                                k.mm(bb[:, 0:BLK], w1k[kc][:, c0:c0 + 128], xT[:, kc, :], [w1k[kc], xT], [bb],
                                     start=(kc == 0), stop=(kc == 7), inc=(kc == 7))
                        g0, sg, u0 = g0s[ei % 2], sgs[ei % 2], u0s[ei % 2]
                        ei += 1
                        k.act(g0[:], bgp[:, 0:BLK], AF.Identity, [bgp, b1b], [g0], bias=b1b[:, p:p + 1], scale=1.0)
                        k.ts('dve', g0[:], g0[:], 7.0, None, ALU.min, None, [g0], [g0])
                        k.act(sg[:], g0[:], AF.Sigmoid, [g0], [sg], scale=1.702)
                        k.tt('dve', sg[:], sg[:], g0[:], ALU.mult, [sg, g0], [sg])
                        k.act(u0[:], bup[:, 0:BLK], AF.Identity, [bup, b1b], [u0], bias=b1b[:, 8 + p:9 + p], scale=1.0)
                        k.ts('dve', u0[:], u0[:], 7.0, -7.0, ALU.min, ALU.max, [u0], [u0])
                        k.stt('dve', aT[:, p, :], u0[:], 1.0, sg[:], ALU.add, ALU.mult, [u0, sg], [aT])
                    for i in range(NTB):
                        bks = [self.bank(), self.bank()]
                        for hf in range(2):
                            for fc in range(8):
                                k.mm(bks[hf][:], aT[:, fc, i * 128:(i + 1) * 128], w2k[fc][:, hf * 512:(hf + 1) * 512],
                                     [aT, w2k[fc]], [bks[hf]], start=(fc == 0), stop=(fc == 7), inc=(fc == 7))
                        y = ysb[yi % 2]
                        yi += 1
                        k.copy('act', y[:, 0:512], bks[0][:], [bks[0]], [y])
                        k.copy('act', y[:, 512:1024], bks[1][:], [bks[1]], [y])
                        k.dma('sp', YB[b * BLK + i * 128:b * BLK + (i + 1) * 128, :], y[:], [y], YB, stream=True)
            with k.phase():
                g = self.bc_load(self.inp['ln3_g'], self.inp['ln3_g'][l], D)
                bt_ = self.bc_load(self.inp['ln3_b'], self.inp['ln3_b'][l], D)
                zs = [k.sb([128, D], F32, 'z') for _ in range(3)]
                ygs = [k.sb([128, D], F32, 'yg') for _ in range(8)]
                GTs = [k.sb([NE, 128], F32, 'GT') for _ in range(2)]
                gi = 0
                k.dma('sp', zs[0][:], H[0:128, :], [H], zs[0])
                for ti in range(NT):
                    z = zs[ti % 3]
                    GT = GTs[ti % 2]
                    if ti + 1 < NT:
                        zn = zs[(ti + 1) % 3]
                        k.dma('sp', zn[:], H[(ti + 1) * 128:(ti + 2) * 128, :], [H], zn)
                    bg = self.bank()
                    k.tr(bg[0:NE, 0:128], G[:, ti, :], self.ident[:], [G, self.ident], [bg])
                    k.copy('act', GT[:], bg[0:NE, 0:128], [bg], [GT])
                    for hf in range(2):
                        bb = self.bank()
                        k.mm(bb[:], GT[:], b2[:, hf * 512:(hf + 1) * 512], [GT, b2], [bb])
                        k.stt('dve', z[:, hf * 512:(hf + 1) * 512], z[:, hf * 512:(hf + 1) * 512], DN_ALPHA, bb[:],
                              ALU.mult, ALU.add, [z, bb], [z])
                    for kk_ in range(4):
                        yg = ygs[gi % 8]
                        gi += 1
                        k.idma(yg, yg[:], YB, YB[:, :], dsel, dsel[:, ti, kk_:kk_ + 1])
                        k.stt('dve', z[:], yg[:], gsel[:, ti, kk_:kk_ + 1], z[:], ALU.mult, ALU.add, [yg, gsel, z], [z])
                    self.ln_tile(z, g, bt_, ti, H, out_dram=final_out, store_q='sp')

    def build(self):
        k = self.k
        self.consts()
        self.alloc_hT()
        H = self.scratch('H', [S, D])
        self.phase_ln0(H)
        if self.upto <= 0:
            self.finish(H)
            return self.nc
        for l in range(DEPTH):
            IF = self.scratch(f'IF{l}', [8, S])
            PT = self.scratch(f'PT{l}', [S, 1024 + R_COLS])
            MIX = self.scratch(f'MIX{l}', [S, 1024])
            with k.phase():
                qkT = k.sb([128, 8, S], BF16, 'qkT')
                self.phase_mixproj(l, qkT, IF, PT)
                if self.upto == 1 + 10 * l:
                    qd = self.scratch('QK', [1024, S], BF16)
                    for c in range(8):
                        k.dma('sp', qd[c * 128:(c + 1) * 128, :], qkT[:, c, :], [qkT], qd)
                    self.finish(H)
                    return self.nc
                self.phase_mlstm(l, qkT, IF, PT, MIX)
                if self.upto == 2 + 10 * l:
                    self.finish(H)
                    return self.nc
            self.free_hT()
            self.phase_rwkv(l, PT, MIX)
            self.alloc_hT()
            if self.upto == 3 + 10 * l:
                self.finish(H)
                return self.nc
            with k.phase():
                xpre = self.xattn_preload(l)
                self.phase_outproj(l, MIX, H)
                self.phase_xattn(l, H, pre=xpre)
            if self.upto == 5 + 10 * l:
                self.finish(H)
                return self.nc
            last = (l == DEPTH - 1)
            if MOE_SPARSE:
                if l == 0:
                    self.YB = self.scratch('YB', [MOE_NB * MOE_BLK, D])
                    self.RT = self.scratch('RT', [MOE_NB * MOE_BLK, 16], mybir.dt.int32)
                self.phase_moe_sparse(l, H, self.YB, self.RT, final_out=(self.out if last else None))
            else:
                self.phase_moe(l, H, final_out=(self.out if last else None))
            if self.upto == 6 + 10 * l and not last:
                self.finish(H)
                return self.nc
        k.barrier()
        return self.nc

    def finish(self, H):
        k = self.k
        with k.phase():
            z = k.sb([128, D], F32, 'fz')
            for ti in range(NT):
                k.dma('sp', z[:], H[ti * 128:(ti + 1) * 128, :], [H], z)
                k.dma('sp', self.out[ti * 128:(ti + 1) * 128, :], z[:], [z], self.out)
        k.barrier()


def make_inputs(inputs, b):
    m = {'x': np.ascontiguousarray(inputs['x'][b]), 'mem': np.ascontiguousarray(inputs['mem'][b])}
    for n, _ in WEIGHT_SPECS:
        m[n] = np.ascontiguousarray(inputs[n])
    return m


def kernel(**inputs):
    inputs = {k_: np.asarray(v) for k_, v in inputs.items()}
    prog = Prog()
    nc = prog.build()
    in_maps = [make_inputs(inputs, b) for b in range(8)]
    res = run_bass_kernel_spmd(nc, in_maps, core_ids=list(range(8)))
    return np.stack([np.asarray(r['out']) for r in res.results], axis=0).astype(np.float32)
```

```python
import numpy as np
from contextlib import ExitStack, contextmanager
import concourse.bass as bass
import concourse.mybir as mybir
from concourse.bass_utils import run_bass_kernel_spmd

F32 = mybir.dt.float32
BF16 = mybir.dt.bfloat16
AF = mybir.ActivationFunctionType
ALU = mybir.AluOpType
AX = mybir.AxisListType

S = 2048
D = 1024
NT = S // 128
DEPTH = 2
M_W = 512
M_H = 4
M_HD = 128
R_W = 512
R_H = 8
R_HD = 64
IN_COLS = 3848
M_COLS = 2056
R_COLS = 1792
NM = 256
NE = 32
DFF = 1024
DN_ALPHA = float((2 * DEPTH) ** 0.25)
LN_EPS = 1e-5
MOE_SPARSE = True
MOE_BLK = 384
MOE_NB = (S * 4 + MOE_BLK - 1) // MOE_BLK + NE

WEIGHT_SPECS = [
    ('ln0_g', (1024,)), ('ln0_b', (1024,)), ('w_in', (2, 1024, 3848)), ('m_conv_w', (2, 4, 1024)),
    ('m_conv_b', (2, 1024)), ('m_ig_b', (2, 4)), ('m_fg_b', (2, 4)), ('m_norm_g', (2, 512)),
    ('r_mu', (2, 1792)), ('r_w0', (2, 512)), ('r_w2', (2, 64, 512)), ('r_a0', (2, 512)),
    ('r_a2', (2, 64, 512)), ('r_g2', (2, 128, 512)), ('r_kk', (2, 512)), ('r_ka', (2, 512)),
    ('r_rk', (2, 512)), ('r_gn_g', (2, 512)), ('r_gn_b', (2, 512)), ('w_out', (2, 1024, 1024)),
    ('ln1_g', (2, 1024)), ('ln1_b', (2, 1024)), ('x_wq', (2, 1024, 1024)), ('x_wkv', (2, 1024, 2048)),
    ('x_wo', (2, 1024, 1024)), ('ln2_g', (2, 1024)), ('ln2_b', (2, 1024)), ('moe_wr', (2, 1024, 32)),
    ('moe_br', (2, 32)), ('moe_w1', (2, 32, 1024, 2048)), ('moe_b1', (2, 32, 2048)),
    ('moe_w2', (2, 32, 1024, 1024)), ('moe_b2', (2, 32, 1024)), ('ln3_g', (2, 1024)), ('ln3_b', (2, 1024)),
]


class T:
    _n = 0

    def __init__(self, t, key=None, excl=False):
        self.t = t
        self.excl = excl
        T._n += 1
        self.key = key if key is not None else ('t', T._n)

    def __getitem__(self, idx):
        return self.t[idx]


class K:
    def __init__(self, nc):
        self.nc = nc
        self.es = ExitStack()
        self.engs = {'pe': nc.tensor, 'act': nc.scalar, 'dve': nc.vector, 'pool': nc.gpsimd, 'sp': nc.sync}
        self.sems = {n: self.es.enter_context(nc.semaphore('s_' + n)) for n in self.engs}
        self.cnt = {n: 0 for n in self.engs}
        self.seen = {n: {} for n in self.engs}
        self.res = {}
        self.dsem = {}
        self.ssem = {}
        self.sempool = []
        self.allsems = {('e', n): (self.sems[n], 0) for n in self.engs}
        self.stack = [self.es]
        self.uid = 0

    @contextmanager
    def phase(self):
        es = ExitStack()
        self.stack.append(es)
        try:
            yield
        finally:
            self.barrier()
            self.stack.pop()
            es.close()

    def name(self, p):
        self.uid += 1
        return f"{p}_{self.uid}"

    def sb(self, shape, dt=F32, name='sb'):
        return T(self.stack[-1].enter_context(self.nc.sbuf_tensor(self.name(name), list(shape), dt)))

    def ps(self, shape, dt=F32, name='ps'):
        return T(self.stack[-1].enter_context(self.nc.psum_tensor(self.name(name), list(shape), dt)), excl=True)

    def dram(self, name, shape, dt=F32, kind='Internal'):
        return T(self.nc.dram_tensor(name, list(shape), dt, kind=kind).ap())

    def _deps(self, reads, writes):
        deps = []
        for r in reads:
            e = self.res.get(r.key)
            if e:
                if e[0]:
                    deps.append(e[0])
                deps.extend(e[2].values())
        for w in writes:
            e = self.res.get(w.key)
            if e:
                if e[0]:
                    deps.append(e[0])
                deps.extend(e[1].values())
                deps.extend(e[2].values())
        return deps

    def _wait(self, en, deps):
        best = {}
        for (sk, sh, v) in deps:
            if en == 'pe' and sk == ('e', 'pe'):
                continue
            if self.seen[en].get(sk, 0) >= v:
                continue
            if sk not in best or best[sk][1] < v:
                best[sk] = (sh, v)
        for sk, (sh, v) in best.items():
            self.engs[en].wait_ge(sh, v)
            self.seen[en][sk] = v

    def _record(self, ev, reads, writes, stream=False):
        for r in reads:
            e = self.res.setdefault(r.key, [None, {}, {}])
            old = e[1].get(ev[0])
            if old is None or old[2] < ev[2]:
                e[1][ev[0]] = ev
        for w in writes:
            if stream:
                self.res.setdefault(w.key, [None, {}, {}])[2][ev[0]] = ev
            else:
                self.res[w.key] = [ev, {}, {}]

    NSTREAM = 4

    def _dsem(self, key, stream):
        def new():
            if self.sempool:
                return self.sempool.pop()
            return [self.es.enter_context(self.nc.semaphore(self.name('sd'))), 0]
        if not stream:
            if key not in self.dsem:
                self.dsem[key] = new()
            return self.dsem[key], None
        st = self.ssem.setdefault(key, [[], 0])
        if len(st[0]) < self.NSTREAM:
            st[0].append(new())
        ds = st[0][st[1] % len(st[0])] if len(st[0]) == self.NSTREAM else st[0][-1]
        st[1] += 1
        prev = (('d', id(ds)), ds[0], ds[1]) if ds[1] > 0 else None
        return ds, prev

    def op(self, en, fn, reads=(), writes=(), inc=True):
        writes = list(writes) + [r for r in reads if r.excl]
        reads = [r for r in reads if not r.excl]
        self._wait(en, self._deps(reads, writes))
        ins = fn(self.engs[en])
        sk = ('e', en)
        ev = (sk, self.sems[en], self.cnt[en] + 1)
        if inc:
            self.cnt[en] += 1
            ins.then_inc(self.sems[en], 1)
            self.allsems[sk] = (self.sems[en], self.cnt[en])
        self._record(ev, reads, writes)
        return ins

    def dma(self, en, out, in_, reads, write, stream=False, **kw):
        ds, prev = self._dsem(write.key, stream)
        deps = self._deps(reads, [] if stream else [write])
        if prev is not None:
            deps.append(prev)
        self._wait(en, deps)
        ds[1] += 16
        self.engs[en].dma_start(out=out, in_=in_, **kw).then_inc(ds[0], 16)
        sk = ('d', id(ds))
        ev = (sk, ds[0], ds[1])
        self.allsems[sk] = (ds[0], ds[1])
        self._record(ev, reads, [write], stream=stream)

    def idma(self, out_t, out_ap, in_t, in_ap, idx_t, idx_ap, scatter=False, bounds=None, stream=False, extra_reads=()):
        en = 'pool'
        reads = [in_t, idx_t] + list(extra_reads)
        ds, prev = self._dsem(out_t.key, stream)
        deps = self._deps(reads, [] if stream else [out_t])
        if prev is not None:
            deps.append(prev)
        self._wait(en, deps)
        ds[1] += 16
        off = bass.IndirectOffsetOnAxis(ap=idx_ap, axis=0)
        kw = {}
        if bounds is not None:
            kw = dict(bounds_check=bounds, oob_is_err=False)
        if scatter:
            ins = self.nc.gpsimd.indirect_dma_start(out=out_ap, out_offset=off, in_=in_ap, in_offset=None, **kw)
        else:
            ins = self.nc.gpsimd.indirect_dma_start(out=out_ap, out_offset=None, in_=in_ap, in_offset=off, **kw)
        ins.then_inc(ds[0], 16)
        sk = ('d', id(ds))
        ev = (sk, ds[0], ds[1])
        self.allsems[sk] = (ds[0], ds[1])
        self._record(ev, reads, [out_t], stream=stream)

    def barrier(self):
        for en in self.engs:
            deps = [(sk, sh, v) for sk, (sh, v) in self.allsems.items() if v > 0 and sk != ('e', en)]
            self._wait(en, deps)
        self.res = {}
        for ds in list(self.dsem.values()) + [d for st in self.ssem.values() for d in st[0]]:
            self.sempool.append(ds)
            self.allsems.pop(('d', id(ds)), None)
        self.dsem = {}
        self.ssem = {}

    def mm(self, out, lhsT, rhs, reads, writes, start=True, stop=True, inc=True):
        return self.op('pe', lambda e: e.matmul(out, lhsT, rhs, start=start, stop=stop), reads, writes, inc)

    def tr(self, out, in_, ident, reads, writes, inc=True):
        return self.op('pe', lambda e: e.transpose(out, in_, ident), reads, writes, inc)

    def act(self, out, in_, func, reads, writes, bias=0.0, scale=1.0, accum_out=None):
        if accum_out is None:
            return self.op('act', lambda e: e.activation(out, in_, func, bias=bias, scale=scale), reads, writes)
        return self.op('act', lambda e: e.activation(out, in_, func, bias=bias, scale=scale,
                                                     accum_out=accum_out), reads, writes)

    def tt(self, en, out, in0, in1, op, reads, writes):
        return self.op(en, lambda e: e.tensor_tensor(out, in0, in1, op), reads, writes)

    def ts(self, en, out, in0, s1, s2, op0, op1, reads, writes):
        if s2 is None:
            return self.op(en, lambda e: e.tensor_scalar(out, in0, s1, None, op0), reads, writes)
        return self.op(en, lambda e: e.tensor_scalar(out, in0, s1, s2, op0, op1), reads, writes)

    def stt(self, en, out, in0, scalar, in1, op0, op1, reads, writes):
        en = 'dve'
        return self.op(en, lambda e: e.scalar_tensor_tensor(out, in0, scalar, in1, op0, op1), reads, writes)

    def copy(self, en, out, in_, reads, writes):
        if en == 'act':
            return self.op('act', lambda e: e.copy(out, in_), reads, writes)
        return self.op(en, lambda e: e.tensor_copy(out, in_), reads, writes)

    def memset(self, en, t, ap, val):
        return self.op(en, lambda e: e.memset(ap, val), (), [t])


class Prog:
    def __init__(self, dbg=(), upto=99):
        self.nc = bass.Bass("TRN2", target_bir_lowering=False)
        self.k = K(self.nc)
        self.dbg = set(dbg)
        self.upto = upto
        nc = self.nc
        self.inp = {}
        self.inp['x'] = T(nc.dram_tensor('x', [S, D], F32, kind='ExternalInput').ap())
        self.inp['mem'] = T(nc.dram_tensor('mem', [NM, D], F32, kind='ExternalInput').ap())
        for n, shp in WEIGHT_SPECS:
            self.inp[n] = T(nc.dram_tensor(n, list(shp), F32, kind='ExternalInput').ap())
        self.out = T(nc.dram_tensor('out', [S, D], F32, kind='ExternalOutput').ap())

    def scratch(self, name, shape, dt=F32):
        kind = 'ExternalOutput' if name in self.dbg else 'Internal'
        return self.k.dram(name, shape, dt, kind=kind)

    def consts(self):
        k = self.k
        self.reg_w = self.nc.gpsimd.to_reg(2 * NE * 1024 - 1)
        self.reg_b = self.nc.gpsimd.to_reg(2 * NE * 16 - 1)
        self.ident = k.sb([128, 128], F32, 'ident')
        k.memset('pool', self.ident, self.ident[:], 1.0)
        k.op('pool', lambda e: e.affine_select(self.ident[:], self.ident[:], [[-1, 128]], ALU.is_equal, 0.0,
                                               base=0, channel_multiplier=1), [self.ident], [self.ident])
        self.identb = k.sb([128, 128], BF16, 'identb')
        k.copy('pool', self.identb[:], self.ident[:], [self.ident], [self.identb])
        self.eps_ln = k.sb([128, 1], F32, 'epsln')
        k.memset('pool', self.eps_ln, self.eps_ln[:], LN_EPS)
        self.one_c = k.sb([128, 1], F32, 'onec')
        k.memset('pool', self.one_c, self.one_c[:], 1.0)
        self.mask_le = k.sb([128, 128], F32, 'maskle')
        k.memset('pool', self.mask_le, self.mask_le[:], 1.0)
        k.op('pool', lambda e: e.affine_select(self.mask_le[:], self.mask_le[:], [[1, 128]], ALU.is_ge, 0.0,
                                               base=0, channel_multiplier=-1), [self.mask_le], [self.mask_le])
        self.pb = [k.ps([128, 512], F32, 'bank') for _ in range(8)]
        self.pbi = 0
        self.hT = None
        self.hT_es = None
        k.barrier()

    def alloc_hT(self):
        k = self.k
        self.hT_es = ExitStack()
        self.hT = T(self.hT_es.enter_context(self.nc.sbuf_tensor(k.name('hT'), [128, 8, S + 1], BF16)))
        k.memset('pool', self.hT, self.hT[:, :, 0:1], 0.0)

    def free_hT(self):
        self.hT_es.close()
        self.hT = None

    def bank(self):
        b = self.pb[self.pbi % 8]
        self.pbi += 1
        return b

    def bc_load(self, dram_t, src_ap, n, name='bc'):
        k = self.k
        t = k.sb([128, n], F32, name)
        k.dma('sp', t[:], src_ap.partition_broadcast(128), [dram_t], t)
        return t

    def ln_tile(self, z, g_bc, b_bc, ti, H, out_dram=None, defer=False, store_q='pool'):
        k = self.k
        st = k.sb([128, 2, 6], F32, 'lnst')
        mv = k.sb([128, 2], F32, 'lnmv')
        for c in range(2):
            k.op('dve', lambda e, c=c: e.bn_stats(st[:, c, :], z[:, c * 512:(c + 1) * 512]), [z], [st])
        k.op('dve', lambda e: e.bn_aggr(mv[:], st[:].rearrange("p a b -> p (a b)")), [st], [mv])
        rstd = k.sb([128, 1], F32, 'lnr')
        k.act(rstd[:], mv[:, 1:2], AF.Sqrt, [mv, self.eps_ln], [rstd], bias=self.eps_ln[:], scale=1.0)
        k.op('dve', lambda e: e.reciprocal(rstd[:], rstd[:]), [rstd], [rstd])
        k.ts('dve', z[:], z[:], mv[:, 0:1], rstd[:], ALU.subtract, ALU.mult, [z, mv, rstd], [z])
        k.tt('dve', z[:], z[:], g_bc[:], ALU.mult, [z, g_bc], [z])
        k.tt('dve', z[:], z[:], b_bc[:], ALU.add, [z, b_bc], [z])
        tgt = out_dram if out_dram is not None else H
        k.dma(store_q, tgt[ti * 128:(ti + 1) * 128, :], z[:], [z], tgt, stream=True)
        if out_dram is None:
            if defer:
                return lambda: self.to_hT(z, ti)
            self.to_hT(z, ti)
        return None

    def to_hT(self, z, ti):
        k = self.k
        for half in range(2):
            b = self.bank()
            for j in range(4):
                c = half * 4 + j
                k.tr(b[:, j * 128:(j + 1) * 128], z[:, c * 128:(c + 1) * 128], self.ident[:], [z, self.ident], [b],
                     inc=(j == 3))
            en = 'act' if half == 0 else 'dve'
            k.copy(en, self.hT[:, half * 4:(half + 1) * 4, 1 + ti * 128:1 + (ti + 1) * 128],
                   b[:].rearrange("p (c t) -> p c t", c=4), [b], [self.hT])

    def phase_ln0(self, H):
        k = self.k
        with k.phase():
            g = self.bc_load(self.inp['ln0_g'], self.inp['ln0_g'][:], D)
            b = self.bc_load(self.inp['ln0_b'], self.inp['ln0_b'][:], D)
            zs = [k.sb([128, D], F32, 'z') for _ in range(2)]
            for ti in range(NT):
                z = zs[ti % 2]
                k.dma('sp', z[:], self.inp['x'][ti * 128:(ti + 1) * 128, :], [self.inp['x']], z)
                self.ln_tile(z, g, b, ti, H)

    def load_w(self, src_t, ap, n, name='w', dt=BF16):
        k = self.k
        wt = k.sb([128, 8, n], dt, name)
        eng = 'pool' if dt != F32 else 'sp'
        k.dma(eng, wt[:], ap.rearrange("(c p) n -> p c n", p=128), [src_t], wt)
        return wt

    def phase_mixproj(self, l, qkT, IF, PT):
        k = self.k
        w_in = self.inp['w_in']
        with k.phase():
            wqk = self.load_w(w_in, w_in[l, :, 0:1024], 1024, 'wqk')
            cw = k.sb([128, 4, 8], F32, 'cw')
            for j in range(4):
                k.dma('sp', cw[:, j, :], self.inp['m_conv_w'][l, j].rearrange("(c p) -> p c", p=128),
                      [self.inp['m_conv_w']], cw, allow_slow_non_contiguous=True)
            cb = k.sb([128, 8], F32, 'cb')
            k.dma('sp', cb[:], self.inp['m_conv_b'][l].rearrange("(c p) -> p c", p=128), [self.inp['m_conv_b']], cb,
                  allow_slow_non_contiguous=True)
            raws = [k.sb([128, S + 3], F32, 'raw') for _ in range(2)]
            accs = [k.sb([128, S], F32, 'acc') for _ in range(2)]
            for r in raws:
                k.memset('pool', r, r[:, 0:3], 0.0)
            for c in range(8):
                raw = raws[c % 2]
                acc = accs[c % 2]
                for tb in range(4):
                    b = self.bank()
                    for kc in range(8):
                        k.mm(b[:], wqk[:, kc, c * 128:(c + 1) * 128], self.hT[:, kc, 1 + tb * 512:1 + (tb + 1) * 512],
                             [wqk, self.hT], [b], start=(kc == 0), stop=(kc == 7), inc=(kc == 7))
                    k.copy('act', raw[:, 3 + tb * 512:3 + (tb + 1) * 512], b[:], [b], [raw])
                en = 'dve' if c % 2 == 0 else 'pool'
                k.ts(en, acc[:], raw[:, 3:3 + S], cw[:, 3, c:c + 1], cb[:, c:c + 1], ALU.mult, ALU.add, [raw, cw, cb], [acc])
                for j in range(3):
                    k.stt(en, acc[:], raw[:, j:j + S], cw[:, j, c:c + 1], acc[:], ALU.mult, ALU.add, [raw, cw, acc], [acc])
                k.act(acc[:], acc[:], AF.Silu, [acc], [acc])
                k.ts(en, qkT[:, c, :], acc[:], (1.0 if c < 4 else float(M_HD ** -0.5)), None, ALU.mult, None, [acc], [qkT])
            wif = self.load_w(w_in, w_in[l, :, 2048:2056], 8, 'wif')
            gb = k.sb([8, 1], F32, 'gb')
            k.dma('sp', gb[0:4, :], self.inp['m_ig_b'][l].rearrange("(a b) -> a b", b=1), [self.inp['m_ig_b']], gb)
            k.dma('sp', gb[4:8, :], self.inp['m_fg_b'][l].rearrange("(a b) -> a b", b=1), [self.inp['m_fg_b']], gb)
            ifs = k.sb([8, S], F32, 'ifs')
            for tb in range(4):
                b = self.bank()
                for kc in range(8):
                    k.mm(b[0:8, :], wif[:, kc, :], self.hT[:, kc, 1 + tb * 512:1 + (tb + 1) * 512],
                         [wif, self.hT], [b], start=(kc == 0), stop=(kc == 7), inc=(kc == 7))
                k.ts('dve', ifs[:, tb * 512:(tb + 1) * 512], b[0:8, :], gb[:, 0:1], None, ALU.add, None, [b, gb], [ifs])
            k.dma('sp', IF[:, :], ifs[:], [ifs], IF)
            mu = self.bc_load(self.inp['r_mu'], self.inp['r_mu'][l], R_COLS, 'mu')
            groups = [(1024, 512, 0, False), (1536, 512, 512, False)]
            off = 0
            while off < R_COLS:
                n = min(512, R_COLS - off)
                groups.append((M_COLS + off, n, 1024 + off, True))
                off += n
            outs = [k.sb([128, 512], F32, 'pto') for _ in range(4)]
            oi = 0
            for (c0, n, o0, shifted) in groups:
                with k.phase():
                    if not shifted:
                        wc = self.load_w(w_in, w_in[l, :, c0:c0 + n], n, 'wg')
                        wp = None
                    else:
                        wf = self.load_w(w_in, w_in[l, :, c0:c0 + n], n, 'wf', dt=F32)
                        wp = k.sb([128, 8, n], BF16, 'wp')
                        wc = k.sb([128, 8, n], BF16, 'wc')
                        tmp = k.sb([128, 8, n], F32, 'wtmp')
                        r0 = c0 - M_COLS
                        for kc in range(8):
                            en = 'dve' if kc % 2 == 0 else 'pool'
                            k.tt(en, tmp[:, kc, :], wf[:, kc, :], mu[:, r0:r0 + n], ALU.mult, [wf, mu], [tmp])
                            k.copy('act', wp[:, kc, :], tmp[:, kc, :], [tmp], [wp])
                            k.tt(en, wc[:, kc, :], wf[:, kc, :], tmp[:, kc, :], ALU.subtract, [wf, tmp], [wc])
                    for ti in range(NT):
                        b = self.bank()
                        nmm = 16 if shifted else 8
                        i = 0
                        for kc in range(8):
                            k.mm(b[:, 0:n], self.hT[:, kc, 1 + ti * 128:1 + (ti + 1) * 128], wc[:, kc, :],
                                 [self.hT, wc], [b], start=(i == 0), stop=(i == nmm - 1), inc=(i == nmm - 1))
                            i += 1
                        if shifted:
                            for kc in range(8):
                                k.mm(b[:, 0:n], self.hT[:, kc, ti * 128:(ti + 1) * 128], wp[:, kc, :],
                                     [self.hT, wp], [b], start=False, stop=(i == nmm - 1), inc=(i == nmm - 1))
                                i += 1
                        o = outs[oi % 4]
                        oi += 1
                        k.copy('act' if ti % 2 == 0 else 'dve', o[:, 0:n], b[:, 0:n], [b], [o])
                        k.dma('pool', PT[ti * 128:(ti + 1) * 128, o0:o0 + n], o[:, 0:n], [o], PT, stream=True)

    def phase_mlstm(self, l, qkT, IF, PT, MIX):
        k = self.k
        NCk = 16
        sc = self.scratch(f'msc{l}', [8, 64])
        with k.phase():
            Gi = k.sb([64, 128], F32, 'Gi')
            Gf = k.sb([64, 128], F32, 'Gf')
            k.dma('sp', Gi[:], IF[0:4, :].rearrange("h (c p) -> (h c) p", p=128), [IF], Gi)
            k.dma('sp', Gf[:], IF[4:8, :].rearrange("h (c p) -> (h c) p", p=128), [IF], Gf)
            ones = k.sb([64, 128], F32, 'ones')
            k.memset('pool', ones, ones[:], 1.0)
            k.act(Gf[:], Gf[:], AF.Exp, [Gf], [Gf], scale=-1.0)
            k.act(Gf[:], Gf[:], AF.Ln, [Gf, self.one_c], [Gf], bias=self.one_c[0:64, :], scale=1.0)
            csp = k.sb([64, 128], F32, 'csp')
            k.op('dve', lambda e: e.tensor_tensor_scan(csp[:], ones[:], Gf[:], 0.0, ALU.mult, ALU.add), [ones, Gf], [csp])
            u = k.sb([64, 128], F32, 'u')
            k.tt('dve', u[:], Gi[:], csp[:], ALU.add, [Gi, csp], [u])
            col = k.sb([64, 2], F32, 'col')
            k.op('dve', lambda e: e.reduce_max(col[:, 0:1], u[:], AX.X), [u], [col])
            k.ts('dve', col[:, 1:2], csp[:, 127:128], -1.0, None, ALU.mult, None, [csp], [col])
            k.dma('sp', sc[0, :].rearrange("(a b) -> a b", b=1), col[:, 0:1], [col], sc)
            k.dma('sp', sc[1, :].rearrange("(a b) -> a b", b=1), col[:, 1:2], [col], sc)
            hm = k.sb([4, 2, 16], F32, 'hm')
            k.dma('sp', hm[:, 0, :], sc[0, :].rearrange("(h c) -> h c", c=16), [sc], hm)
            k.dma('sp', hm[:, 1, :], sc[1, :].rearrange("(h c) -> h c", c=16), [sc], hm)
            mn = k.sb([4, 17], F32, 'mn')
            k.memset('dve', mn, mn[:, 0:1], 0.0)
            k.op('dve', lambda e: e.tensor_tensor_scan(mn[:, 1:17], hm[:, 0, :], hm[:, 1, :], 0.0, ALU.max, ALU.add),
                 [hm], [mn])
            ra = k.sb([4, 2, 16], F32, 'ra')
            k.tt('dve', ra[:, 0, :], mn[:, 0:16], hm[:, 0, :], ALU.max, [mn, hm], [ra])
            k.tt('dve', ra[:, 1, :], mn[:, 0:16], ra[:, 0, :], ALU.subtract, [mn, ra], [ra])
            k.ts('dve', ra[:, 0, :], ra[:, 0, :], -1.0, None, ALU.mult, None, [ra], [ra])
            k.dma('sp', sc[2, :].rearrange("(h c) -> h c", c=16), ra[:, 0, :], [ra], sc)
            k.dma('sp', sc[3, :].rearrange("(h c) -> h c", c=16), ra[:, 1, :], [ra], sc)
            negR = k.sb([64, 1], F32, 'negR')
            k.dma('sp', negR[:], sc[2, :].rearrange("(a b) -> a b", b=1), [sc], negR)
            alpha = k.sb([128, 64], F32, 'alpha')
            k.dma('sp', alpha[:], sc[3, :].partition_broadcast(128), [sc], alpha)
            k.act(alpha[:], alpha[:], AF.Exp, [alpha], [alpha])
            k.act(u[:], u[:], AF.Exp, [u, negR], [u], bias=negR[:], scale=1.0)
            k.act(csp[:], csp[:], AF.Exp, [csp, negR], [csp], bias=negR[:], scale=1.0)
            Etm = k.sb([128, 64], F32, 'Etm')
            Ttm = k.sb([128, 64], F32, 'Ttm')
            b = self.bank()
            k.tr(b[:, 0:64], u[:], self.ident[0:64, 0:64], [u, self.ident], [b])
            k.copy('dve', Etm[:], b[:, 0:64], [b], [Etm])
            b = self.bank()
            k.tr(b[:, 0:64], csp[:], self.ident[0:64, 0:64], [csp, self.ident], [b])
            k.copy('dve', Ttm[:], b[:, 0:64], [b], [Ttm])
            Cst = [k.sb([128, 129], F32, 'Cst') for _ in range(4)]
            for h in range(4):
                k.memset('pool', Cst[h], Cst[h][:], 0.0)
            Csb = [k.sb([128, 129], BF16, 'Csb') for _ in range(4)]
            Vs = [k.sb([128, 129], BF16, 'Vs') for _ in range(4)]
            Sm = [k.sb([128, 128], BF16, 'Sm') for _ in range(4)]
            kTm = [k.sb([128, 128], BF16, 'kTm') for _ in range(4)]
            vts = [k.sb([128, 1024], F32, 'vt') for _ in range(2)]
            hns = [k.sb([128, 512], F32, 'hn') for _ in range(3)]
            sml = [k.sb([128, 16], F32, 'sml') for _ in range(4)]
            for c in range(NCk):
                vt = vts[c % 2]
                hn = hns[c % 3]
                k.dma('sp', vt[:], PT[c * 128:(c + 1) * 128, 0:1024], [PT], vt)
                tsl = slice(c * 128, (c + 1) * 128)
                for h in range(4):
                    hc = h * 16 + c
                    sm = sml[h]
                    k.op('act', lambda e, h=h, hc=hc: e.activation(Vs[h][:, 0:128], vt[:, h * 128:(h + 1) * 128], AF.Copy,
                                                                 scale=Etm[:, hc:hc + 1]), [vt, Etm], [Vs[h]])
                    k.copy('dve', Vs[h][:, 128:129], Etm[:, hc:hc + 1], [Etm], [Vs[h]])
                    b1 = self.bank()
                    k.mm(b1[:, 0:128], qkT[:, 4 + h, tsl], qkT[:, h, tsl], [qkT], [b1])
                    k.tt('dve', Sm[h][:], b1[:, 0:128], self.mask_le[:], ALU.mult, [b1, self.mask_le], [Sm[h]])
                    b2 = self.bank()
                    k.mm(b2[:, 0:128], qkT[:, 4 + h, tsl], self.identb[:], [qkT, self.identb], [b2])
                    k.copy('act', kTm[h][:], b2[:, 0:128], [b2], [kTm[h]])
                    k.ts('dve', Csb[h][:], Cst[h][:], alpha[:, hc:hc + 1], None, ALU.mult, None, [Cst[h], alpha], [Csb[h]])
                    b3 = self.bank()
                    k.mm(b3[:, 0:129], Sm[h][:], Vs[h][:], [Sm[h], Vs[h]], [b3], start=True, stop=False, inc=False)
                    k.mm(b3[:, 0:129], qkT[:, h, tsl], Csb[h][:], [qkT, Csb[h]], [b3], start=False, stop=True)
                    b4 = self.bank()
                    k.mm(b4[:, 0:129], kTm[h][:], Vs[h][:], [kTm[h], Vs[h]], [b4])
                    k.stt('dve', Cst[h][:], Cst[h][:], alpha[:, hc:hc + 1], b4[:, 0:129], ALU.mult, ALU.add,
                          [Cst[h], alpha, b4], [Cst[h]])
                    k.copy('act', sm[:, 12:13], b3[:, 128:129], [b3], [sm])
                    k.stt('dve', sm[:, 13:14], sm[:, 12:13], -1.0, sm[:, 12:13], ALU.mult, ALU.max, [sm], [sm])
                    k.tt('dve', sm[:, 0:1], sm[:, 13:14], Ttm[:, hc:hc + 1], ALU.max, [sm, Ttm], [sm])
                    k.op('dve', lambda e, sm=sm: e.reciprocal(sm[:, 1:2], sm[:, 0:1]), [sm], [sm])
                    hs = hn[:, h * 128:(h + 1) * 128]
                    k.ts('dve', hs, b3[:, 0:128], sm[:, 1:2], None, ALU.mult, None, [b3, sm], [hn])
                    k.op('dve', lambda e, sm=sm, hs=hs: e.bn_stats(sm[:, 2:8], hs), [hn], [sm])
                    k.op('dve', lambda e, sm=sm: e.bn_aggr(sm[:, 8:10], sm[:, 2:8]), [sm], [sm])
                    k.act(sm[:, 10:11], sm[:, 9:10], AF.Sqrt, [sm, self.eps_ln], [sm], bias=self.eps_ln[:], scale=1.0)
                    k.op('dve', lambda e, sm=sm: e.reciprocal(sm[:, 11:12], sm[:, 10:11]), [sm], [sm])
                    k.ts('dve', hs, hs, sm[:, 8:9], sm[:, 11:12], ALU.subtract, ALU.mult, [hn, sm], [hn])
                k.act(vt[:, 512:1024], vt[:, 512:1024], AF.Sigmoid, [vt], [vt])
                k.tt('dve', hn[:], hn[:], vt[:, 512:1024], ALU.mult, [hn, vt], [hn])
                k.dma('pool', MIX[c * 128:(c + 1) * 128, 0:512], hn[:], [hn], MIX, stream=True)

    def phase_rwkv(self, l, PT, MIX):
        k = self.k
        P64 = slice(0, 64)
        with k.phase():
            def bc(n, nm):
                return self.bc_load(self.inp[n], self.inp[n][l], 512, nm)
            w0, a0, kkp, ka, rrk, gng, gnb = (bc('r_w0', 'w0'), bc('r_a0', 'a0'), bc('r_kk', 'kkp'), bc('r_ka', 'ka'),
                                               bc('r_rk', 'rrk'), bc('r_gn_g', 'gng'), bc('r_gn_b', 'gnb'))
            omk = k.sb([128, 512], F32, 'omk')
            k.ts('dve', omk[:], ka[:], -1.0, 1.0, ALU.mult, ALU.add, [ka], [omk])
            w2 = k.sb([64, 512], F32, 'w2')
            a2 = k.sb([64, 512], F32, 'a2')
            g2 = k.sb([128, 512], F32, 'g2')
            k.dma('sp', w2[:], self.inp['r_w2'][l], [self.inp['r_w2']], w2)
            k.dma('sp', a2[:], self.inp['r_a2'][l], [self.inp['r_a2']], a2)
            k.dma('sp', g2[:], self.inp['r_g2'][l], [self.inp['r_g2']], g2)

            def b2(t):
                return t[P64, :].unsqueeze(1).to_broadcast([64, 2, 512])

            def mk_mask(pattern_cm, op, base=0):
                m = k.sb([64, 8, 64], F32, 'msk')
                k.memset('pool', m, m[:], 1.0)
                k.op('pool', lambda e: e.affine_select(m[:, 0, :], m[:, 0, :], [[pattern_cm[0], 64]], op, 0.0,
                                                       base=base, channel_multiplier=pattern_cm[1]), [m], [m])
                for i in range(1, 8):
                    k.copy('pool', m[:, i, :], m[:, 0, :], [m], [m])
                return m
            mSU = mk_mask((1, -1), ALU.is_gt)
            mSL = mk_mask((-1, 1), ALU.is_gt)
            mIU = mk_mask((1, -1), ALU.is_ge)
            I8 = mk_mask((1, -1), ALU.is_equal)
            tri = mIU[:, 0, :]
            ST = k.sb([64, 8, 64], F32, 'ST')
            k.memset('pool', ST, ST[:], 0.0)

            def tm(nm):
                return k.sb([64, 2, 512], F32, nm)

            def pair(f):
                return [f(), f()]
            lw, av, gam, gprev, ginv = tm('lw'), tm('av'), tm('gam'), tm('gprev'), tm('ginv')
            kk, rk2, t1, t2, yv = tm('kk'), tm('rk2'), tm('t1'), tm('t2'), tm('yv')
            lrw = k.sb([64, 128], F32, 'lrw')
            lra = k.sb([64, 128], F32, 'lra')
            lrg = k.sb([128, 128], F32, 'lrg')
            QTb = {q: k.sb([64, 16, 64], BF16, 'QTb' + q) for q in ('a', 'b', 'k', 'r')}
            Mc, Nc, Mn, Nn, Qm = [k.sb([64, 16, 64], BF16, 'nm') for _ in range(5)]
            Wsb = k.sb([64, 8, 64], F32, 'Wsb')
            Usb = k.sb([64, 8, 64], F32, 'Usb')
            xt_p = pair(lambda: k.sb([64, 2, R_COLS], F32, 'xt'))
            gv_p, bt_p, kt_p = pair(lambda: tm('gv')), pair(lambda: tm('bt')), pair(lambda: tm('kt'))
            sm_p = pair(lambda: k.sb([64, 8, 16], F32, 'rsm'))
            gL_p = pair(lambda: k.sb([64, 16], F32, 'gL'))
            QTa_p, QTr_p = pair(lambda: k.sb([64, 16, 64], F32, 'QTa')), pair(lambda: k.sb([64, 16, 64], F32, 'QTr'))
            Pm_p = pair(lambda: k.sb([64, 16, 64], F32, 'Pm'))
            Aak_p, Arb_p, Ark_p = [pair(lambda: k.sb([64, 16, 64], F32, 'am')) for _ in range(3)]

            def h3(ap):
                return ap.rearrange("p j (h c) -> p j h c", c=64)

            def s3(ap16):
                return ap16.rearrange("p (j h) -> p j h", j=2)

            def bch(ap16):
                return s3(ap16).unsqueeze(3).to_broadcast([64, 2, 8, 64])

            def v3(bb):
                return bb[P64, :].rearrange("p (h c) -> p h c", c=64)

            def front(it):
                p = it % 2
                xt, gv, bt, kt, sm, gL = xt_p[p], gv_p[p], bt_p[p], kt_p[p], sm_p[p], gL_p[p]
                QT = {'a': QTa_p[p], 'r': QTr_p[p]}
                Pm, Aak, Arb, Ark = Pm_p[p], Aak_p[p], Arb_p[p], Ark_p[p]
                r0 = it * 128
                rr_ = xt[:, :, 0:512]
                rkx = xt[:, :, 512:1024]
                k.dma('sp', xt[:], PT[r0:r0 + 128, 1024:1024 + R_COLS].rearrange("(j p) c -> p j c", p=64), [PT], xt)
                b = self.bank()
                for j in range(2):
                    k.tr(b[P64, j * 64:(j + 1) * 64], xt[:, j, 1536:1600], self.ident[P64, P64], [xt, self.ident], [b], inc=False)
                    k.tr(b[P64, 128 + j * 64:128 + (j + 1) * 64], xt[:, j, 1600:1664], self.ident[P64, P64],
                         [xt, self.ident], [b], inc=False)
                    k.tr(b[:, 256 + j * 64:256 + (j + 1) * 64], xt[:, j, 1664:1792], self.ident[P64, P64],
                         [xt, self.ident], [b], inc=(j == 1))
                k.act(lrw[:], b[P64, 0:128], AF.Tanh, [b], [lrw])
                k.copy('dve', lra[:], b[P64, 128:256], [b], [lra])
                k.act(lrg[:], b[:, 256:384], AF.Sigmoid, [b], [lrg])
                for j in range(2):
                    js = slice(j * 64, (j + 1) * 64)
                    b = self.bank()
                    k.mm(b[P64, :], lrw[:, js], w2[:], [lrw, w2], [b])
                    k.tt('dve', lw[:, j, :], b[P64, :], w0[P64, :], ALU.add, [b, w0], [lw])
                    b = self.bank()
                    k.mm(b[P64, :], lra[:, js], a2[:], [lra, a2], [b])
                    k.tt('dve', av[:, j, :], b[P64, :], a0[P64, :], ALU.add, [b, a0], [av])
                    b = self.bank()
                    k.mm(b[P64, :], lrg[:, js], g2[:], [lrg, g2], [b])
                    k.copy('act', gv[:, j, :], b[P64, :], [b], [gv])
                k.act(lw[:], lw[:], AF.Sigmoid, [lw], [lw])
                k.ts('dve', lw[:], lw[:], -0.6065306597126334, None, ALU.mult, None, [lw], [lw])
                k.act(av[:], av[:], AF.Sigmoid, [av], [av])
                for j in range(2):
                    b = self.bank()
                    k.mm(b[P64, :], tri, lw[:, j, :], [mIU, lw], [b])
                    k.act(gam[:, j, :], b[P64, :], AF.Exp, [b], [gam])
                    k.act(ginv[:, j, :], b[P64, :], AF.Exp, [b], [ginv], scale=-1.0)
                    k.tt('dve', gprev[:, j, :], b[P64, :], lw[:, j, :], ALU.subtract, [b, lw], [gprev])
                k.act(gprev[:], gprev[:], AF.Exp, [gprev], [gprev])
                b = self.bank()
                for j in range(2):
                    for h in range(8):
                        blk = j * 8 + h
                        k.mm(b[P64, blk:blk + 1], lw[:, j, h * 64:(h + 1) * 64], self.one_c[P64, :], [lw, self.one_c], [b],
                             inc=(blk == 15))
                k.act(gL[:], b[P64, 0:16], AF.Exp, [b], [gL])
                yield
                k.tt('dve', kk[:], rkx, b2(kkp), ALU.mult, [xt, kkp], [kk])
                k.tt('dve', t1[:], kk[:], kk[:], ALU.mult, [kk], [t1])
                k.op('dve', lambda e: e.reduce_sum(s3(sm[:, 0, :]), h3(t1[:]), AX.X), [t1], [sm])
                k.act(sm[:, 1, :], sm[:, 0, :], AF.Sqrt, [sm], [sm])
                k.ts('dve', sm[:, 1, :], sm[:, 1, :], 1e-12, None, ALU.max, None, [sm], [sm])
                k.op('dve', lambda e: e.reciprocal(sm[:, 2, :], sm[:, 1, :]), [sm], [sm])
                k.tt('dve', h3(kk[:]), h3(kk[:]), bch(sm[:, 2, :]), ALU.mult, [kk, sm], [kk])
                k.tt('dve', t1[:], av[:], b2(ka), ALU.mult, [av, ka], [t1])
                k.tt('dve', t1[:], t1[:], b2(omk), ALU.add, [t1, omk], [t1])
                k.tt('dve', rk2[:], rkx, t1[:], ALU.mult, [xt, t1], [rk2])
                k.tt('dve', t1[:], rr_, rk2[:], ALU.mult, [xt, rk2], [t1])
                k.tt('dve', t1[:], t1[:], b2(rrk), ALU.mult, [t1, rrk], [t1])
                k.op('dve', lambda e: e.reduce_sum(s3(sm[:, 3, :]), h3(t1[:]), AX.X), [t1], [sm])
                k.stt('dve', gprev[:], kk[:], -1.0, gprev[:], ALU.mult, ALU.mult, [kk, gprev], [gprev])
                k.tt('dve', bt[:], kk[:], av[:], ALU.mult, [kk, av], [bt])
                k.tt('dve', bt[:], bt[:], ginv[:], ALU.mult, [bt, ginv], [bt])
                k.tt('dve', kt[:], rk2[:], ginv[:], ALU.mult, [rk2, ginv], [kt])
                k.tt('dve', gam[:], rr_, gam[:], ALU.mult, [xt, gam], [gam])
                yield
                ei = 0
                for q, src in (('a', gprev), ('b', bt), ('k', kt), ('r', gam)):
                    for j in range(2):
                        b = self.bank()
                        for h in range(8):
                            k.tr(b[P64, h * 64:(h + 1) * 64], src[:, j, h * 64:(h + 1) * 64], self.ident[P64, P64],
                                 [src, self.ident], [b], inc=(h == 7))
                        k.copy('act' if ei % 2 == 0 else 'dve', QTb[q][:, j * 8:(j + 1) * 8, :], v3(b), [b], [QTb[q]])
                        if q in QT:
                            k.copy('dve' if ei % 2 == 0 else 'act', QT[q][:, j * 8:(j + 1) * 8, :], v3(b), [b], [QT[q]])
                        ei += 1
                    if q == 'b':
                        yield
                yield
                specs = [('b', 'a', mSU, Mc), ('a', 'b', mSL, Nc), ('k', 'a', mSU, Aak), ('b', 'r', mIU, Arb),
                         ('k', 'r', mIU, Ark)]
                for si, (ql, qr, msk, dst) in enumerate(specs):
                    for j in range(2):
                        b = self.bank()
                        for h in range(8):
                            blk = j * 8 + h
                            k.mm(b[P64, h * 64:(h + 1) * 64], QTb[ql][:, blk, :], QTb[qr][:, blk, :], [QTb[ql], QTb[qr]], [b],
                                 inc=(h == 7))
                        k.tt('dve', dst[:, j * 8:(j + 1) * 8, :], v3(b), msk[:], ALU.mult, [b, msk], [dst])
                    if si == 1:
                        yield
                for j in range(2):
                    js = slice(j * 8, (j + 1) * 8)
                    k.tt('dve', Pm[:, js, :], Mc[:, js, :], I8[:], ALU.add, [Mc, I8], [Pm])
                    k.tt('dve', Qm[:, js, :], Nc[:, js, :], I8[:], ALU.add, [Nc, I8], [Qm])
                yield
                mc, ncur, mn, nn = Mc, Nc, Mn, Nn
                for lvl in range(5):
                    last = (lvl == 4)
                    bms, bns, bps, bqs = [], [], [], []
                    for j in range(2):
                        bm = self.bank()
                        for h in range(8):
                            blk = j * 8 + h
                            k.mm(bm[P64, h * 64:(h + 1) * 64], ncur[:, blk, :], mc[:, blk, :], [ncur, mc], [bm], inc=(h == 7))
                        bms.append(bm)
                        if not last:
                            bn = self.bank()
                            for h in range(8):
                                blk = j * 8 + h
                                k.mm(bn[P64, h * 64:(h + 1) * 64], mc[:, blk, :], ncur[:, blk, :], [ncur, mc], [bn],
                                     inc=(h == 7))
                            bns.append(bn)
                    for j in range(2):
                        js = slice(j * 8, (j + 1) * 8)
                        k.copy('act', mn[:, js, :], v3(bms[j]), [bms[j]], [mn])
                        if not last:
                            k.copy('dve', nn[:, js, :], v3(bns[j]), [bns[j]], [nn])
                    for j in range(2):
                        bp = self.bank()
                        for h in range(8):
                            blk = j * 8 + h
                            k.mm(bp[P64, h * 64:(h + 1) * 64], Qm[:, blk, :], mn[:, blk, :], [Qm, mn], [bp], inc=(h == 7))
                        bps.append(bp)
                        if not last:
                            bq = self.bank()
                            for h in range(8):
                                blk = j * 8 + h
                                k.mm(bq[P64, h * 64:(h + 1) * 64], mn[:, blk, :], Qm[:, blk, :], [Qm, mn], [bq],
                                     inc=(h == 7))
                            bqs.append(bq)
                    for j in range(2):
                        js = slice(j * 8, (j + 1) * 8)
                        k.tt('dve', Pm[:, js, :], Pm[:, js, :], v3(bps[j]), ALU.add, [Pm, bps[j]], [Pm])
                        if not last:
                            k.tt('dve', Qm[:, js, :], Qm[:, js, :], v3(bqs[j]), ALU.add, [Qm, bqs[j]], [Qm])
                    mc, mn = mn, mc
                    ncur, nn = nn, ncur
                    yield

            def back(it):
                p = it % 2
                xt, gv, bt, kt, sm, gL = xt_p[p], gv_p[p], bt_p[p], kt_p[p], sm_p[p], gL_p[p]
                QT = {'a': QTa_p[p], 'r': QTr_p[p]}
                Pm, Aak, Arb, Ark = Pm_p[p], Aak_p[p], Arb_p[p], Ark_p[p]
                r0 = it * 128
                rv = xt[:, :, 1024:1536]
                for j in range(2):
                    bw = self.bank()
                    for h in range(8):
                        blk = j * 8 + h
                        hs = slice(h * 64, (h + 1) * 64)
                        k.mm(bw[P64, hs], QT['a'][:, blk, :], ST[:, h, :], [QT['a'], ST], [bw], start=True, stop=False, inc=False)
                        k.mm(bw[P64, hs], Aak[:, blk, :], xt[:, j, 1024 + h * 64:1024 + (h + 1) * 64], [Aak, xt], [bw],
                             start=False, stop=True, inc=(h == 7))
                    k.copy('act', Wsb[:], v3(bw), [bw], [Wsb])
                    yield
                    bu = self.bank()
                    for h in range(8):
                        blk = j * 8 + h
                        k.mm(bu[P64, h * 64:(h + 1) * 64], Pm[:, blk, :], Wsb[:, h, :], [Pm, Wsb], [bu], inc=(h == 7))
                    k.copy('act', Usb[:], v3(bu), [bu], [Usb])
                    yield
                    by = self.bank()
                    bs_ = self.bank()
                    for h in range(8):
                        hs = slice(h * 64, (h + 1) * 64)
                        vh = xt[:, j, 1024 + h * 64:1024 + (h + 1) * 64]
                        k.mm(bs_[P64, hs], bt[:, j, hs], Usb[:, h, :], [bt, Usb], [bs_], start=True, stop=False, inc=False)
                        k.mm(bs_[P64, hs], kt[:, j, hs], vh, [kt, xt], [bs_], start=False, stop=True, inc=(h == 7))
                    for h in range(8):
                        blk = j * 8 + h
                        hs = slice(h * 64, (h + 1) * 64)
                        vh = xt[:, j, 1024 + h * 64:1024 + (h + 1) * 64]
                        k.mm(by[P64, hs], QT['r'][:, blk, :], ST[:, h, :], [QT['r'], ST], [by], start=True, stop=False, inc=False)
                        k.mm(by[P64, hs], Arb[:, blk, :], Usb[:, h, :], [Arb, Usb], [by], start=False, stop=False, inc=False)
                        k.mm(by[P64, hs], Ark[:, blk, :], vh, [Ark, xt], [by], start=False, stop=True, inc=(h == 7))
                    k.tt('dve', ST[:], ST[:], v3(bs_), ALU.add, [ST, bs_], [ST])
                    k.tt('dve', ST[:], ST[:], gL[:, j * 8:(j + 1) * 8].unsqueeze(2).to_broadcast([64, 8, 64]), ALU.mult,
                         [ST, gL], [ST])
                    k.copy('act', yv[:, j, :], by[P64, :], [by], [yv])
                    yield
                y3 = h3(yv[:])
                k.op('dve', lambda e: e.reduce_sum(s3(sm[:, 4, :]), y3, AX.X), [yv], [sm])
                k.ts('dve', sm[:, 4, :], sm[:, 4, :], 1.0 / 64, None, ALU.mult, None, [sm], [sm])
                k.tt('dve', y3, y3, bch(sm[:, 4, :]), ALU.subtract, [yv, sm], [yv])
                k.tt('dve', t2[:], yv[:], yv[:], ALU.mult, [yv], [t2])
                k.op('dve', lambda e: e.reduce_sum(s3(sm[:, 5, :]), h3(t2[:]), AX.X), [t2], [sm])
                k.ts('dve', sm[:, 5, :], sm[:, 5, :], 1.0 / 64, 64e-5, ALU.mult, ALU.add, [sm], [sm])
                k.act(sm[:, 5, :], sm[:, 5, :], AF.Sqrt, [sm], [sm])
                k.op('dve', lambda e: e.reciprocal(sm[:, 6, :], sm[:, 5, :]), [sm], [sm])
                k.tt('dve', y3, y3, bch(sm[:, 6, :]), ALU.mult, [yv, sm], [yv])
                yield
                k.tt('dve', yv[:], yv[:], b2(gng), ALU.mult, [yv, gng], [yv])
                k.tt('dve', yv[:], yv[:], b2(gnb), ALU.add, [yv, gnb], [yv])
                k.tt('dve', h3(t2[:]), h3(rv), bch(sm[:, 3, :]), ALU.mult, [xt, sm], [t2])
                k.tt('dve', yv[:], yv[:], t2[:], ALU.add, [yv, t2], [yv])
                k.tt('dve', yv[:], yv[:], gv[:], ALU.mult, [yv, gv], [yv])
                k.dma('pool', MIX[r0:r0 + 128, 512:1024].rearrange("(j p) c -> p j c", p=64), yv[:], [yv], MIX, stream=True)
                yield

            RWI = 16

            def drive(gens):
                gens = [g for g in gens if g is not None]
                while gens:
                    for g in list(gens):
                        try:
                            next(g)
                        except StopIteration:
                            gens.remove(g)
            drive([front(0)])
            for it in range(RWI):
                drive([back(it), front(it + 1) if it + 1 < RWI else None])

    def resid_ln(self, banks, ti, H, g_bc, b_bc, zs, out_dram=None, defer=False, preloaded=False):
        k = self.k
        z = zs[ti % len(zs)]
        if not preloaded:
            k.dma('sp', z[:], H[ti * 128:(ti + 1) * 128, :], [H], z)
        if isinstance(banks, (list, tuple)):
            for hf in range(2):
                k.stt('dve', z[:, hf * 512:(hf + 1) * 512], z[:, hf * 512:(hf + 1) * 512], DN_ALPHA, banks[hf][:],
                      ALU.mult, ALU.add, [z, banks[hf]], [z])
        else:
            k.stt('dve', z[:], z[:], DN_ALPHA, banks[:], ALU.mult, ALU.add, [z, banks], [z])
        return self.ln_tile(z, g_bc, b_bc, ti, H, out_dram=out_dram, defer=defer)

    def phase_outproj(self, l, MIX, H):
        k = self.k
        wo_t = self.inp['w_out']
        with k.phase():
            wo = self.load_w(wo_t, wo_t[l], 1024, 'wo')
            gcol = k.sb([128, 4], F32, 'gcol')
            k.dma('sp', gcol[:], self.inp['m_norm_g'][l].rearrange("(c p) -> p c", p=128), [self.inp['m_norm_g']], gcol,
                  allow_slow_non_contiguous=True)
            for c in range(4):
                k.ts('dve', wo[:, c, :], wo[:, c, :], gcol[:, c:c + 1], None, ALU.mult, None, [wo, gcol], [wo])
            g = self.bc_load(self.inp['ln1_g'], self.inp['ln1_g'][l], D)
            b = self.bc_load(self.inp['ln1_b'], self.inp['ln1_b'][l], D)
            zs = [k.sb([128, D], F32, 'z') for _ in range(3)]
            pend = None
            ms = [k.sb([128, D], F32, 'mx') for _ in range(2)]
            mTs = [k.sb([128, 8, 128], BF16, 'mT') for _ in range(2)]
            for ti in range(NT):
                m = ms[ti % 2]
                mT = mTs[ti % 2]
                k.dma('sp', m[:], MIX[ti * 128:(ti + 1) * 128, :], [MIX], m)
                k.dma('sp', zs[ti % 3][:], H[ti * 128:(ti + 1) * 128, :], [H], zs[ti % 3])
                for half in range(2):
                    bb = self.bank()
                    for j in range(4):
                        c = half * 4 + j
                        k.tr(bb[:, j * 128:(j + 1) * 128], m[:, c * 128:(c + 1) * 128], self.ident[:], [m, self.ident], [bb],
                             inc=(j == 3))
                    k.copy('act', mT[:, half * 4:(half + 1) * 4, :], bb[:].rearrange("p (c t) -> p c t", c=4), [bb], [mT])
                bks = [self.bank(), self.bank()]
                for hf in range(2):
                    for kc in range(8):
                        k.mm(bks[hf][:], mT[:, kc, :], wo[:, kc, hf * 512:(hf + 1) * 512], [mT, wo], [bks[hf]],
                             start=(kc == 0), stop=(kc == 7), inc=(kc == 7))
                if pend is not None:
                    pend()
                pend = self.resid_ln(bks, ti, H, g, b, zs, defer=True, preloaded=True)
            pend()

    def xattn_preload(self, l):
        return (self.load_w(self.inp['x_wq'], self.inp['x_wq'][l], 1024, 'wq'),
                self.load_w(self.inp['x_wkv'], self.inp['x_wkv'][l], 2048, 'wkv'),
                self.load_w(self.inp['x_wo'], self.inp['x_wo'][l], 1024, 'xwo'))

    def phase_xattn(self, l, H, pre=None):
        k = self.k
        with k.phase():
            wq, wkv, wo = pre if pre is not None else self.xattn_preload(l)
            g = self.bc_load(self.inp['ln2_g'], self.inp['ln2_g'][l], D)
            b = self.bc_load(self.inp['ln2_b'], self.inp['ln2_b'][l], D)
            zs = [k.sb([128, D], F32, 'z') for _ in range(3)]
            pend = None
            memT = k.sb([128, 8, NM], BF16, 'memT')
            for mt in range(2):
                z = zs[mt]
                k.dma('sp', z[:], self.inp['mem'][mt * 128:(mt + 1) * 128, :], [self.inp['mem']], z)
                for half in range(2):
                    bb = self.bank()
                    for j in range(4):
                        c = half * 4 + j
                        k.tr(bb[:, j * 128:(j + 1) * 128], z[:, c * 128:(c + 1) * 128], self.ident[:], [z, self.ident], [bb],
                             inc=(j == 3))
                    k.copy('act', memT[:, half * 4:(half + 1) * 4, mt * 128:(mt + 1) * 128],
                           bb[:].rearrange("p (c t) -> p c t", c=4), [bb], [memT])
            KT = k.sb([128, 8, NM], BF16, 'KT')
            for c in range(8):
                bb = self.bank()
                for kc in range(8):
                    k.mm(bb[:, 0:NM], wkv[:, kc, c * 128:(c + 1) * 128], memT[:, kc, :], [wkv, memT], [bb],
                         start=(kc == 0), stop=(kc == 7), inc=(kc == 7))
                k.copy('act' if c % 2 else 'dve', KT[:, c, :], bb[:, 0:NM], [bb], [KT])
            Vt = k.sb([128, 2, 1024], BF16, 'Vt')
            for mt in range(2):
                for hf in range(2):
                    bb = self.bank()
                    for kc in range(8):
                        k.mm(bb[:], memT[:, kc, mt * 128:(mt + 1) * 128], wkv[:, kc, 1024 + hf * 512:1024 + (hf + 1) * 512],
                             [wkv, memT], [bb], start=(kc == 0), stop=(kc == 7), inc=(kc == 7))
                    k.copy('act' if hf else 'dve', Vt[:, mt, hf * 512:(hf + 1) * 512], bb[:], [bb], [Vt])
            QTs = [k.sb([128, 8, 128], BF16, 'QT') for _ in range(2)]
            OTs = [k.sb([128, 8, 128], BF16, 'OT') for _ in range(2)]
            Pf = [k.sb([128, NM], F32, 'Pf') for _ in range(4)]
            Pn = [k.sb([128, NM], BF16, 'Pn') for _ in range(4)]
            PTt = [k.sb([128, 2, 128], BF16, 'PTt') for _ in range(4)]
            st = [k.sb([128, 4], F32, 'xst') for _ in range(4)]
            scale = float(256 ** -0.5)

            def qproj(ti):
                QT = QTs[ti % 2]
                tsl = slice(1 + ti * 128, 1 + (ti + 1) * 128)
                for half in range(2):
                    bb = self.bank()
                    for j in range(4):
                        c = half * 4 + j
                        for kc in range(8):
                            k.mm(bb[:, j * 128:(j + 1) * 128], wq[:, kc, c * 128:(c + 1) * 128], self.hT[:, kc, tsl],
                                 [wq, self.hT], [bb], start=(kc == 0), stop=(kc == 7), inc=(kc == 7 and j == 3))
                    k.copy('act' if half else 'dve', QT[:, half * 4:(half + 1) * 4, :],
                           bb[:].rearrange("p (c t) -> p c t", c=4), [bb], [QT])

            qproj(0)
            for ti in range(NT):
                QT, OT = QTs[ti % 2], OTs[ti % 2]
                k.dma('sp', zs[ti % 3][:], H[ti * 128:(ti + 1) * 128, :], [H], zs[ti % 3])
                bss = []
                for h in range(4):
                    bs = self.bank()
                    for dc in range(2):
                        k.mm(bs[:, 0:NM], QT[:, 2 * h + dc, :], KT[:, 2 * h + dc, :], [QT, KT], [bs],
                             start=(dc == 0), stop=(dc == 1), inc=(dc == 1))
                    bss.append(bs)
                if ti + 1 < NT:
                    qproj(ti + 1)
                if pend is not None:
                    pend()
                    pend = None
                for h in range(4):
                    s_, bs = st[h], bss[h]
                    k.op('dve', lambda e, s_=s_, bs=bs: e.reduce_max(s_[:, 0:1], bs[:, 0:NM], AX.X), [bs], [s_])
                    k.ts('dve', s_[:, 1:2], s_[:, 0:1], -scale, None, ALU.mult, None, [s_], [s_])
                    k.act(Pf[h][:], bs[:, 0:NM], AF.Exp, [bs, s_], [Pf[h], s_], bias=s_[:, 1:2], scale=scale,
                          accum_out=s_[:, 2:3])
                for h in range(4):
                    s_ = st[h]
                    k.op('dve', lambda e, s_=s_: e.reciprocal(s_[:, 3:4], s_[:, 2:3]), [s_], [s_])
                    k.ts('dve', Pn[h][:], Pf[h][:], s_[:, 3:4], None, ALU.mult, None, [Pf[h], s_], [Pn[h]])
                bts = []
                for h in range(4):
                    bt_ = self.bank()
                    for mc in range(2):
                        k.mm(bt_[:, mc * 128:(mc + 1) * 128], Pn[h][:, mc * 128:(mc + 1) * 128], self.identb[:],
                             [Pn[h], self.identb], [bt_], inc=(mc == 1))
                    bts.append(bt_)
                for h in range(4):
                    k.copy('act' if h % 2 else 'dve', PTt[h][:], bts[h][:, 0:256].rearrange("p (c t) -> p c t", c=2),
                           [bts[h]], [PTt[h]])
                bos = []
                for h in range(4):
                    bo = self.bank()
                    for dc in range(2):
                        c = 2 * h + dc
                        for mc in range(2):
                            k.mm(bo[:, dc * 128:(dc + 1) * 128], Vt[:, mc, c * 128:(c + 1) * 128], PTt[h][:, mc, :],
                                 [Vt, PTt[h]], [bo], start=(mc == 0), stop=(mc == 1), inc=(mc == 1 and dc == 1))
                    bos.append(bo)
                for h in range(4):
                    k.copy('dve' if h % 2 else 'act', OT[:, 2 * h:2 * h + 2, :],
                           bos[h][:, 0:256].rearrange("p (c t) -> p c t", c=2), [bos[h]], [OT])
                bks = [self.bank(), self.bank()]
                for hf in range(2):
                    for kc in range(8):
                        k.mm(bks[hf][:], OT[:, kc, :], wo[:, kc, hf * 512:(hf + 1) * 512], [OT, wo], [bks[hf]],
                             start=(kc == 0), stop=(kc == 7), inc=(kc == 7))
                pend = self.resid_ln(bks, ti, H, g, b, zs, defer=True, preloaded=True)
            pend()

    def phase_moe(self, l, H, final_out=None):
        k = self.k
        w1_t, w2_t = self.inp['moe_w1'], self.inp['moe_w2']
        NEX = NE
        DMAONLY = 0
        with k.phase():
            G = k.sb([128, NT, NE], F32, 'G')
            acc = k.sb([128, NT, D], F32, 'acc')
            with k.phase():
                wr = k.sb([128, 8, NE], F32, 'wr')
                k.dma('sp', wr[:], self.inp['moe_wr'][l].rearrange("(c p) n -> p c n", p=128), [self.inp['moe_wr']], wr)
                br = self.bc_load(self.inp['moe_br'], self.inp['moe_br'][l], NE, 'br')
                b2 = k.sb([NE, D], F32, 'b2')
                k.dma('sp', b2[:], self.inp['moe_b2'][l], [self.inp['moe_b2']], b2)
                hts = [k.sb([128, D], F32, 'ht') for _ in range(2)]
                h32s = [k.sb([128, 8, 128], F32, 'h32') for _ in range(2)]
                lgs = [k.sb([128, NE], F32, 'lg') for _ in range(2)]
                m8s = [k.sb([128, 16], F32, 'm8') for _ in range(2)]
                GTs = [k.sb([NE, 128], F32, 'GT') for _ in range(2)]
                for ti in range(NT):
                    ht, h32, lg, m8, GT = hts[ti % 2], h32s[ti % 2], lgs[ti % 2], m8s[ti % 2], GTs[ti % 2]
                    k.dma('sp', ht[:], H[ti * 128:(ti + 1) * 128, :], [H], ht)
                    for half in range(2):
                        bb = self.bank()
                        for j in range(4):
                            c = half * 4 + j
                            k.tr(bb[:, j * 128:(j + 1) * 128], ht[:, c * 128:(c + 1) * 128], self.ident[:], [ht, self.ident],
                                 [bb], inc=(j == 3))
                        k.copy('act', h32[:, half * 4:(half + 1) * 4, :], bb[:].rearrange("p (c t) -> p c t", c=4), [bb], [h32])
                    bl = self.bank()
                    for kc in range(8):
                        k.mm(bl[:, 0:NE], h32[:, kc, :], wr[:, kc, :], [h32, wr], [bl], start=(kc == 0), stop=(kc == 7),
                             inc=(kc == 7))
                    k.tt('dve', lg[:], bl[:, 0:NE], br[:], ALU.add, [bl, br], [lg])
                    k.op('dve', lambda e, m8=m8, lg=lg: e.max(m8[:, 0:8], lg[:]), [lg], [m8])
                    k.ts('dve', m8[:, 8:9], m8[:, 0:1], -1.0, None, ALU.mult, None, [m8], [m8])
                    g_ = G[:, ti, :]
                    k.act(g_, lg[:], AF.Exp, [lg, m8], [G], bias=m8[:, 8:9], scale=1.0)
                    k.ts('dve', lg[:], lg[:], m8[:, 3:4], None, ALU.is_ge, None, [lg, m8], [lg])
                    k.tt('dve', g_, g_, lg[:], ALU.mult, [G, lg], [G])
                    k.op('dve', lambda e, m8=m8, g_=g_: e.reduce_sum(m8[:, 9:10], g_, AX.X), [G], [m8])
                    k.op('dve', lambda e, m8=m8: e.reciprocal(m8[:, 10:11], m8[:, 9:10]), [m8], [m8])
                    k.ts('dve', g_, g_, m8[:, 10:11], None, ALU.mult, None, [G, m8], [G])
                    bg = self.bank()
                    k.tr(bg[0:NE, 0:128], g_, self.ident[:], [G, self.ident], [bg])
                    k.copy('act', GT[:], bg[0:NE, 0:128], [bg], [GT])
                    for hf in range(2):
                        bb = self.bank()
                        k.mm(bb[:], GT[:], b2[:, hf * 512:(hf + 1) * 512], [GT, b2], [bb])
                        k.copy('act' if hf else 'dve', acc[:, ti, hf * 512:(hf + 1) * 512], bb[:], [bb], [acc])
            with k.phase():
                b1a = k.sb([128, NE, 16], F32, 'b1a')
                for e in range(NE):
                    k.dma('sp', b1a[:, e, :], self.inp['moe_b1'][l, e].rearrange("(c p) -> p c", p=128),
                          [self.inp['moe_b1']], b1a, allow_slow_non_contiguous=True)
                actT = k.sb([128, 8, S], BF16, 'actT')
                w1r = [k.sb([128, 8, 2, 128], BF16, 'w1r') for _ in range(4)]
                w2b = k.sb([128, 8, D], BF16, 'w2b')
                g0s = [k.sb([128, 512], F32, 'g0') for _ in range(2)]
                sgs = [k.sb([128, 512], F32, 'sg') for _ in range(2)]
                u0s = [k.sb([128, 512], F32, 'u0') for _ in range(2)]
                pi = 0
                ei = 0
                for e in range(NEX):
                    for p in range(8):
                        w1 = w1r[pi % 4]
                        pi += 1
                        for gu in range(2):
                            c0 = gu * DFF + p * 128
                            k.dma('pool', w1[:, :, gu, :], w1_t[l, e, :, c0:c0 + 128].rearrange("(c p) n -> p c n", p=128),
                                  [w1_t], w1)
                        if p == 0:
                            k.dma('pool', w2b[:], w2_t[l, e].rearrange("(c p) n -> p c n", p=128), [w2_t], w2b)
                        for tb in range(4 if not DMAONLY else 0):
                            tsl = slice(1 + tb * 512, 1 + (tb + 1) * 512)
                            bgp, bup = self.bank(), self.bank()
                            for gu, bb in ((0, bgp), (1, bup)):
                                for kc in range(8):
                                    k.mm(bb[:], w1[:, kc, gu, :], self.hT[:, kc, tsl], [w1, self.hT], [bb],
                                         start=(kc == 0), stop=(kc == 7), inc=(kc == 7))
                            g0, sg, u0 = g0s[ei % 2], sgs[ei % 2], u0s[ei % 2]
                            ei += 1
                            k.act(g0[:], bgp[:], AF.Identity, [bgp, b1a], [g0], bias=b1a[:, e, p:p + 1], scale=1.0)
                            k.ts('dve', g0[:], g0[:], 7.0, None, ALU.min, None, [g0], [g0])
                            k.act(sg[:], g0[:], AF.Sigmoid, [g0], [sg], scale=1.702)
                            k.tt('dve', sg[:], sg[:], g0[:], ALU.mult, [sg, g0], [sg])
                            k.act(u0[:], bup[:], AF.Identity, [bup, b1a], [u0], bias=b1a[:, e, 8 + p:9 + p], scale=1.0)
                            k.ts('dve', u0[:], u0[:], 7.0, -7.0, ALU.min, ALU.max, [u0], [u0])
                            k.stt('dve', actT[:, p, tb * 512:(tb + 1) * 512], u0[:], 1.0, sg[:], ALU.add, ALU.mult,
                                  [u0, sg], [actT])
                    for ti in range(NT if not DMAONLY else 0):
                        bks = [self.bank(), self.bank()]
                        for hf in range(2):
                            for fc in range(8):
                                k.mm(bks[hf][:], actT[:, fc, ti * 128:(ti + 1) * 128], w2b[:, fc, hf * 512:(hf + 1) * 512],
                                     [actT, w2b], [bks[hf]], start=(fc == 0), stop=(fc == 7), inc=(fc == 7))
                        for hf in range(2):
                            k.stt('dve', acc[:, ti, hf * 512:(hf + 1) * 512], bks[hf][:], G[:, ti, e:e + 1],
                                  acc[:, ti, hf * 512:(hf + 1) * 512], ALU.mult, ALU.add, [bks[hf], G, acc], [acc])
            with k.phase():
                g = self.bc_load(self.inp['ln3_g'], self.inp['ln3_g'][l], D)
                b = self.bc_load(self.inp['ln3_b'], self.inp['ln3_b'][l], D)
                zs = [k.sb([128, D], F32, 'z') for _ in range(2)]
                for ti in range(NT):
                    z = zs[ti % 2]
                    k.dma('sp', z[:], H[ti * 128:(ti + 1) * 128, :], [H], z)
                    k.stt('dve', z[:], z[:], DN_ALPHA, acc[:, ti, :], ALU.mult, ALU.add, [z, acc], [z])
                    self.ln_tile(z, g, b, ti, H, out_dram=final_out)

    def phase_moe_sparse(self, l, H, YB, RT, final_out=None):
        k = self.k
        I32 = mybir.dt.int32
        BLK = MOE_BLK
        NTB = BLK // 128
        NB = MOE_NB
        NR = NB * BLK
        BIG = 4.0e6
        w1v = self.inp['moe_w1'][:].rearrange("l e d f -> (l e d) f")
        w2v = self.inp['moe_w2'][:].rearrange("l e f d -> (l e f) d")
        b1v = self.inp['moe_b1'][:].rearrange("l e (c f) -> (l e c) f", f=128)
        with k.phase():
            G = k.sb([128, NT, NE], F32, 'G')
            dsel = k.sb([128, NT, 4], I32, 'dsel')
            gsel = k.sb([128, NT, 4], F32, 'gsel')
            IW = k.sb([128, NB, 8], I32, 'IW')
            IB = k.sb([16, NB], I32, 'IB')
            b2 = k.sb([NE, D], F32, 'b2')
            k.dma('sp', b2[:], self.inp['moe_b2'][l], [self.inp['moe_b2']], b2)
            with k.phase():
                wr = k.sb([128, 8, NE], F32, 'wr')
                k.dma('sp', wr[:], self.inp['moe_wr'][l].rearrange("(c p) n -> p c n", p=128), [self.inp['moe_wr']], wr)
                br = self.bc_load(self.inp['moe_br'], self.inp['moe_br'][l], NE, 'br')
                Mk = k.sb([128, NT, NE], F32, 'Mk')
                rank = k.sb([128, NT, NE], F32, 'rank')
                hts = [k.sb([128, D], F32, 'ht') for _ in range(2)]
                h32s = [k.sb([128, 8, 128], F32, 'h32') for _ in range(2)]
                lgs = [k.sb([128, NE], F32, 'lg') for _ in range(2)]
                m8s = [k.sb([128, 16], F32, 'm8') for _ in range(2)]
                for ti in range(NT):
                    ht, h32, lg, m8 = hts[ti % 2], h32s[ti % 2], lgs[ti % 2], m8s[ti % 2]
                    k.dma('sp', ht[:], H[ti * 128:(ti + 1) * 128, :], [H], ht)
                    for half in range(2):
                        bb = self.bank()
                        for j in range(4):
                            c = half * 4 + j
                            k.tr(bb[:, j * 128:(j + 1) * 128], ht[:, c * 128:(c + 1) * 128], self.ident[:], [ht, self.ident],
                                 [bb], inc=(j == 3))
                        k.copy('act', h32[:, half * 4:(half + 1) * 4, :], bb[:].rearrange("p (c t) -> p c t", c=4), [bb], [h32])
                    bl = self.bank()
                    for kc in range(8):
                        k.mm(bl[:, 0:NE], h32[:, kc, :], wr[:, kc, :], [h32, wr], [bl], start=(kc == 0), stop=(kc == 7),
                             inc=(kc == 7))
                    k.tt('dve', lg[:], bl[:, 0:NE], br[:], ALU.add, [bl, br], [lg])
                    k.op('dve', lambda e, m8=m8, lg=lg: e.max(m8[:, 0:8], lg[:]), [lg], [m8])
                    k.ts('dve', m8[:, 8:9], m8[:, 0:1], -1.0, None, ALU.mult, None, [m8], [m8])
                    g_ = G[:, ti, :]
                    k.act(g_, lg[:], AF.Exp, [lg, m8], [G], bias=m8[:, 8:9], scale=1.0)
                    k.ts('dve', Mk[:, ti, :], lg[:], m8[:, 3:4], None, ALU.is_ge, None, [lg, m8], [Mk])
                    k.tt('dve', g_, g_, Mk[:, ti, :], ALU.mult, [G, Mk], [G])
                    k.op('dve', lambda e, m8=m8, g_=g_: e.reduce_sum(m8[:, 9:10], g_, AX.X), [G], [m8])
                    k.op('dve', lambda e, m8=m8: e.reciprocal(m8[:, 10:11], m8[:, 9:10]), [m8], [m8])
                    k.ts('dve', g_, g_, m8[:, 10:11], None, ALU.mult, None, [G, m8], [G])
                ones = k.sb([128, 128], F32, 'ones')
                k.memset('pool', ones, ones[:], 1.0)
                lst = k.sb([128, 128], F32, 'lst')
                k.memset('pool', lst, lst[:], 1.0)
                k.op('pool', lambda e: e.affine_select(lst[:], lst[:], [[1, 128]], ALU.is_gt, 0.0, base=0,
                                                       channel_multiplier=-1), [lst], [lst])
                for ti in range(NT):
                    bb = self.bank()
                    for tj in range(ti):
                        k.mm(bb[:, 0:NE], ones[:], Mk[:, tj, :], [ones, Mk], [bb], start=(tj == 0), stop=False, inc=False)
                    k.mm(bb[:, 0:NE], lst[:], Mk[:, ti, :], [lst, Mk], [bb], start=(ti == 0), stop=True)
                    k.copy('act' if ti % 2 else 'dve', rank[:, ti, :], bb[:, 0:NE], [bb], [rank])
                bb = self.bank()
                for tj in range(NT):
                    k.mm(bb[:, 0:NE], ones[:], Mk[:, tj, :], [ones, Mk], [bb], start=(tj == 0), stop=(tj == NT - 1),
                         inc=(tj == NT - 1))
                cnt = k.sb([128, NE], F32, 'cnt')
                k.copy('dve', cnt[:], bb[:, 0:NE], [bb], [cnt])
                thr = k.sb([128, 16], F32, 'thr')
                k.op('pool', lambda e: e.iota(thr[:], [[BLK, 16]], base=0, channel_multiplier=0,
                                              allow_small_or_imprecise_dtypes=True), [], [thr])
                cmp_ = k.sb([128, NE, 16], F32, 'cmp')
                k.tt('dve', cmp_[:], cnt[:].unsqueeze(2).to_broadcast([128, NE, 16]),
                     thr[:].unsqueeze(1).to_broadcast([128, NE, 16]), ALU.is_gt, [cnt, thr], [cmp_])
                pad = k.sb([128, 4, NE], F32, 'pad')
                k.op('dve', lambda e: e.reduce_sum(pad[:, 0, :], cmp_[:], AX.X), [cmp_], [pad])
                k.ts('dve', pad[:, 0, :], pad[:, 0, :], float(BLK), None, ALU.mult, None, [pad], [pad])
                k.op('dve', lambda e: e.tensor_tensor_scan(pad[:, 1, :], ones[:, 0:NE], pad[:, 0, :], 0.0, ALU.mult, ALU.add),
                     [ones, pad], [pad])
                k.tt('dve', pad[:, 2, :], pad[:, 1, :], pad[:, 0, :], ALU.subtract, [pad], [pad])
                bth = k.sb([128, NB], F32, 'bth')
                k.op('pool', lambda e: e.iota(bth[:], [[BLK, NB]], base=0, channel_multiplier=0,
                                              allow_small_or_imprecise_dtypes=True), [], [bth])
                cmpb = k.sb([128, NB, NE], F32, 'cmpb')
                k.tt('dve', cmpb[:], pad[:, 1, :].unsqueeze(1).to_broadcast([128, NB, NE]),
                     bth[:].unsqueeze(2).to_broadcast([128, NB, NE]), ALU.is_le, [pad, bth], [cmpb])
                be = k.sb([128, 4, NB], F32, 'be')
                k.op('dve', lambda e: e.reduce_sum(be[:, 0, :], cmpb[:], AX.X), [cmpb], [be])
                k.ts('dve', be[:, 1, :], be[:, 0, :], float(NE) - 0.5, BIG, ALU.is_gt, ALU.mult, [be], [be])
                k.ts('dve', be[:, 0, :], be[:, 0, :], float(NE - 1), None, ALU.min, None, [be], [be])
                iw0 = k.sb([128, 8], F32, 'iw0')
                k.op('pool', lambda e: e.iota(iw0[:], [[128, 8]], base=l * NE * 1024, channel_multiplier=1,
                                              allow_small_or_imprecise_dtypes=True), [], [iw0])
                k.ts('dve', be[:, 2, :], be[:, 0, :], 1024.0, None, ALU.mult, None, [be], [be])
                k.tt('dve', be[:, 2, :], be[:, 2, :], be[:, 1, :], ALU.add, [be], [be])
                iwf = k.sb([128, NB, 8], F32, 'iwf')
                k.tt('dve', iwf[:], be[:, 2, :].unsqueeze(2).to_broadcast([128, NB, 8]),
                     iw0[:].unsqueeze(1).to_broadcast([128, NB, 8]), ALU.add, [be, iw0], [iwf])
                k.copy('dve', IW[:], iwf[:], [iwf], [IW])
                ib0 = k.sb([16, 1], F32, 'ib0')
                k.op('pool', lambda e: e.iota(ib0[:], [[0, 1]], base=l * NE * 16, channel_multiplier=1,
                                              allow_small_or_imprecise_dtypes=True), [], [ib0])
                ibf = k.sb([16, NB], F32, 'ibf')
                k.ts('dve', ibf[:], be[0:16, 0, :], 16.0, ib0[:, 0:1], ALU.mult, ALU.add, [be, ib0], [ibf])
                k.tt('dve', ibf[:], ibf[:], be[0:16, 1, :], ALU.add, [ibf, be], [ibf])
                k.copy('dve', IB[:], ibf[:], [ibf], [IB])
                zt = k.sb([128, (NR // 128) * 16], I32, 'zt')
                k.memset('pool', zt, zt[:], 0)
                k.dma('sp', RT[:, :].rearrange("(p n) c -> p (n c)", p=128), zt[:], [zt], RT)
                RTS = T(RT.t)
                Dm = k.sb([128, NE], F32, 'Dm')
                d8 = k.sb([128, 8], F32, 'd8')
                eq = k.sb([128, NE], F32, 'eq')
                tok = k.sb([128, 16], I32, 'tok')
                for ti in range(NT):
                    k.tt('dve', Dm[:], rank[:, ti, :], pad[:, 2, :], ALU.add, [rank, pad], [Dm])
                    k.ts('dve', Dm[:], Dm[:], 1.0, None, ALU.add, None, [Dm], [Dm])
                    k.tt('dve', Dm[:], Dm[:], Mk[:, ti, :], ALU.mult, [Dm, Mk], [Dm])
                    k.ts('dve', Dm[:], Dm[:], -1.0, None, ALU.add, None, [Dm], [Dm])
                    k.op('dve', lambda e: e.max(d8[:], Dm[:]), [Dm], [d8])
                    k.copy('dve', dsel[:, ti, :], d8[:, 0:4], [d8], [dsel])
                    for kk_ in range(4):
                        k.ts('dve', eq[:], Dm[:], d8[:, kk_:kk_ + 1], None, ALU.is_equal, None, [Dm, d8], [eq])
                        k.tt('dve', eq[:], eq[:], G[:, ti, :], ALU.mult, [eq, G], [eq])
                        k.op('dve', lambda e, kk_=kk_: e.reduce_sum(gsel[:, ti, kk_:kk_ + 1], eq[:], AX.X), [eq], [gsel])
                    k.op('pool', lambda e, ti=ti: e.iota(tok[:], [[0, 16]], base=ti * 128, channel_multiplier=1), [], [tok])
                    for kk_ in range(4):
                        k.idma(RTS, RT[:, :], tok, tok[:], dsel, dsel[:, ti, kk_:kk_ + 1], scatter=True, stream=True, extra_reads=[RT])
            with k.phase():
                NW = 16
                w1p = [k.sb([128, 2 * DFF], BF16, 'w1p') for _ in range(NW)]
                w2p = [k.sb([128, D], BF16, 'w2p') for _ in range(NW)]
                xia = k.sb([128, NB, NTB], I32, 'xia')
                k.dma('sp', xia[:], RT[:, 0:1].rearrange("(b i p) c -> p b (i c)", p=128, i=NTB), [RT, RTS], xia,
                      allow_slow_non_contiguous=True)
                xgs = [k.sb([128, D], F32, 'xg') for _ in range(4)]
                xTs = [k.sb([128, 8, BLK], BF16, 'xT') for _ in range(2)]
                aTs = [k.sb([128, 8, BLK], BF16, 'aT') for _ in range(2)]
                b1gs = [k.sb([16, 128], F32, 'b1g') for _ in range(2)]
                b1bs = [k.sb([128, 16], F32, 'b1b') for _ in range(2)]
                ysb = [k.sb([128, D], F32, 'ysb') for _ in range(3)]
                g0s = [k.sb([128, BLK], F32, 'g0') for _ in range(2)]
                sgs = [k.sb([128, BLK], F32, 'sg') for _ in range(2)]
                u0s = [k.sb([128, BLK], F32, 'u0') for _ in range(2)]
                NBX = NB
                wi = 0
                ei = 0
                xgi = 0
                yi = 0
                lo_, hi_ = list(range(0, NE)), list(range(NE, NB))
                order = []
                while lo_ or hi_:
                    if lo_:
                        order.append(lo_.pop(0))
                    if hi_:
                        order.append(hi_.pop(0))
                for bi_, b in enumerate(order[:NBX]):
                    xT, aT, b1g, b1b = xTs[bi_ % 2], aTs[bi_ % 2], b1gs[bi_ % 2], b1bs[bi_ % 2]
                    xg4 = []
                    for i in range(NTB):
                        xg = xgs[xgi % 4]
                        xgi += 1
                        k.idma(xg, xg[:], H, H[:, :], xia, xia[:, b, i:i + 1])
                        xg4.append(xg)
                    k.idma(b1g, b1g[:], self.inp['moe_b1'], b1v, IB, IB[:, b:b + 1], bounds=self.reg_b)
                    w1k, w2k = [], []
                    for kc in range(8):
                        w = w1p[wi % NW]
                        k.idma(w, w[:], self.inp['moe_w1'], w1v, IW, IW[:, b, kc:kc + 1], bounds=self.reg_w)
                        w1k.append(w)
                        wi += 1
                    wi -= 8
                    for kc in range(8):
                        w = w2p[wi % NW]
                        k.idma(w, w[:], self.inp['moe_w2'], w2v, IW, IW[:, b, kc:kc + 1], bounds=self.reg_w)
                        w2k.append(w)
                        wi += 1
                    for i in range(NTB):
                        xg = xg4[i]
                        for half in range(2):
                            bb = self.bank()
                            for j in range(4):
                                c = half * 4 + j
                                k.tr(bb[:, j * 128:(j + 1) * 128], xg[:, c * 128:(c + 1) * 128], self.ident[:],
                                     [xg, self.ident], [bb], inc=(j == 3))
                            k.copy('act' if half else 'dve', xT[:, half * 4:(half + 1) * 4, i * 128:(i + 1) * 128],
                                   bb[:].rearrange("p (c t) -> p c t", c=4), [bb], [xT])
                    bb = self.bank()
                    k.tr(bb[:, 0:16], b1g[:], self.ident[0:16, 0:16], [b1g, self.ident], [bb])
                    k.copy('dve', b1b[:], bb[:, 0:16], [bb], [b1b])
                    for p in range(8):
                        bgp, bup = self.bank(), self.bank()
                        for gu, bb in ((0, bgp), (1, bup)):
                            c0 = gu * DFF + p * 128
                            for kc in range(8):
                                k.mm(bb[:, 0:BLK], w1k[kc][:, c0:c0 + 128], xT[:, kc, :], [w1k[kc], xT], [bb],
                                     start=(kc == 0), stop=(kc == 7), inc=(kc == 7))
                        g0, sg, u0 = g0s[ei % 2], sgs[ei % 2], u0s[ei % 2]
                        ei += 1
                        k.act(g0[:], bgp[:, 0:BLK], AF.Identity, [bgp, b1b], [g0], bias=b1b[:, p:p + 1], scale=1.0)
                        k.ts('dve', g0[:], g0[:], 7.0, None, ALU.min, None, [g0], [g0])
                        k.act(sg[:], g0[:], AF.Sigmoid, [g0], [sg], scale=1.702)
                        k.tt('dve', sg[:], sg[:], g0[:], ALU.mult, [sg, g0], [sg])
                        k.act(u0[:], bup[:, 0:BLK], AF.Identity, [bup, b1b], [u0], bias=b1b[:, 8 + p:9 + p], scale=1.0)
                        k.ts('dve', u0[:], u0[:], 7.0, -7.0, ALU.min, ALU.max, [u0], [u0])
                        k.stt('dve', aT[:, p, :], u0[:], 1.0, sg[:], ALU.add, ALU.mult, [u0, sg], [aT])
                    for i in range(NTB):
                        bks = [self.bank(), self.bank()]
                        for hf in range(2):
                            for fc in range(8):
                                k.mm(bks[hf][:], aT[:, fc, i * 128:(i + 1) * 128], w2k[fc][:, hf * 512:(hf + 1) * 512],
                                     [aT, w2k[fc]], [bks[hf]], start=(fc == 0), stop=(fc == 7), inc=(fc == 7))
                        y = ysb[yi % 3]
                        yi += 1
                        k.copy('act', y[:, 0:512], bks[0][:], [bks[0]], [y])
                        k.copy('act', y[:, 512:1024], bks[1][:], [bks[1]], [y])
                        k.dma('sp', YB[b * BLK + i * 128:b * BLK + (i + 1) * 128, :], y[:], [y], YB, stream=True)
            with k.phase():
                g = self.bc_load(self.inp['ln3_g'], self.inp['ln3_g'][l], D)
                bt_ = self.bc_load(self.inp['ln3_b'], self.inp['ln3_b'][l], D)
                zs = [k.sb([128, D], F32, 'z') for _ in range(3)]
                ygs = [k.sb([128, D], F32, 'yg') for _ in range(8)]
                GTs = [k.sb([NE, 128], F32, 'GT') for _ in range(2)]
                gi = 0
                k.dma('sp', zs[0][:], H[0:128, :], [H], zs[0])
                for ti in range(NT):
                    z = zs[ti % 3]
                    GT = GTs[ti % 2]
                    if ti + 1 < NT:
                        zn = zs[(ti + 1) % 3]
                        k.dma('sp', zn[:], H[(ti + 1) * 128:(ti + 2) * 128, :], [H], zn)
                    bg = self.bank()
                    k.tr(bg[0:NE, 0:128], G[:, ti, :], self.ident[:], [G, self.ident], [bg])
                    k.copy('act', GT[:], bg[0:NE, 0:128], [bg], [GT])
                    for hf in range(2):
                        bb = self.bank()
                        k.mm(bb[:], GT[:], b2[:, hf * 512:(hf + 1) * 512], [GT, b2], [bb])
                        k.stt('dve', z[:, hf * 512:(hf + 1) * 512], z[:, hf * 512:(hf + 1) * 512], DN_ALPHA, bb[:],
                              ALU.mult, ALU.add, [z, bb], [z])
                    for kk_ in range(4):
                        yg = ygs[gi % 8]
                        gi += 1
                        k.idma(yg, yg[:], YB, YB[:, :], dsel, dsel[:, ti, kk_:kk_ + 1])
                        k.stt('dve', z[:], yg[:], gsel[:, ti, kk_:kk_ + 1], z[:], ALU.mult, ALU.add, [yg, gsel, z], [z])
                    self.ln_tile(z, g, bt_, ti, H, out_dram=final_out, store_q='sp')

    def build(self):
        k = self.k
        self.consts()
        self.alloc_hT()
        H = self.scratch('H', [S, D])
        self.phase_ln0(H)
        if self.upto <= 0:
            self.finish(H)
            return self.nc
        for l in range(DEPTH):
            IF = self.scratch(f'IF{l}', [8, S])
            PT = self.scratch(f'PT{l}', [S, 1024 + R_COLS])
            MIX = self.scratch(f'MIX{l}', [S, 1024])
            with k.phase():
                qkT = k.sb([128, 8, S], BF16, 'qkT')
                self.phase_mixproj(l, qkT, IF, PT)
                if self.upto == 1 + 10 * l:
                    qd = self.scratch('QK', [1024, S], BF16)
                    for c in range(8):
                        k.dma('sp', qd[c * 128:(c + 1) * 128, :], qkT[:, c, :], [qkT], qd)
                    self.finish(H)
                    return self.nc
                self.phase_mlstm(l, qkT, IF, PT, MIX)
                if self.upto == 2 + 10 * l:
                    self.finish(H)
                    return self.nc
            self.free_hT()
            self.phase_rwkv(l, PT, MIX)
            self.alloc_hT()
            if self.upto == 3 + 10 * l:
                self.finish(H)
                return self.nc
            with k.phase():
                xpre = self.xattn_preload(l)
                self.phase_outproj(l, MIX, H)
                self.phase_xattn(l, H, pre=xpre)
            if self.upto == 5 + 10 * l:
                self.finish(H)
                return self.nc
            last = (l == DEPTH - 1)
            if MOE_SPARSE:
                if l == 0:
                    self.YB = self.scratch('YB', [MOE_NB * MOE_BLK, D])
                    self.RT = self.scratch('RT', [MOE_NB * MOE_BLK, 16], mybir.dt.int32)
                self.phase_moe_sparse(l, H, self.YB, self.RT, final_out=(self.out if last else None))
            else:
                self.phase_moe(l, H, final_out=(self.out if last else None))
            if self.upto == 6 + 10 * l and not last:
                self.finish(H)
                return self.nc
        k.barrier()
        return self.nc

    def finish(self, H):
        k = self.k
        with k.phase():
            z = k.sb([128, D], F32, 'fz')
            for ti in range(NT):
                k.dma('sp', z[:], H[ti * 128:(ti + 1) * 128, :], [H], z)
                k.dma('sp', self.out[ti * 128:(ti + 1) * 128, :], z[:], [z], self.out)
        k.barrier()


def make_inputs(inputs, b):
    m = {'x': np.ascontiguousarray(inputs['x'][b]), 'mem': np.ascontiguousarray(inputs['mem'][b])}
    for n, _ in WEIGHT_SPECS:
        m[n] = np.ascontiguousarray(inputs[n])
    return m


def kernel(**inputs):
    inputs = {k_: np.asarray(v) for k_, v in inputs.items()}
    prog = Prog()
    nc = prog.build()
    in_maps = [make_inputs(inputs, b) for b in range(8)]
    res = run_bass_kernel_spmd(nc, in_maps, core_ids=list(range(8)))
    return np.stack([np.asarray(r['out']) for r in res.results], axis=0).astype(np.float32)
```
